# Optimizing a Trainium2 kernel written in Bass

```python
import jax, jax.numpy as jnp
from jax import lax
import numpy as np

D_MODEL = 1024
BATCH = 8
SEQ = 2048
DEPTH = 4

CHUNK = 64
QBLK = 128
N_MIXERS = 4
N_MEM = 256

ATT_HEADS = 16
ATT_HD = D_MODEL // ATT_HEADS
ML_HEADS = 4
ML_DK = D_MODEL // 2 // ML_HEADS
ML_DV = D_MODEL // ML_HEADS
GLA_HEADS = 4
GLA_DK = D_MODEL // 2 // GLA_HEADS
GLA_DV = D_MODEL // GLA_HEADS
GLA_RANK = 16
GLA_TAU = 16.0
XA_HEADS = 4
XA_HD = D_MODEL // XA_HEADS
D_FF = 2816
CONV_W = 3

DN_ALPHA = (2.0 * DEPTH) ** 0.25
DN_BETA = (8.0 * DEPTH) ** -0.25
LN_EPS = 1e-5
RMS_EPS = 1e-6
FOX_F_BIAS = 2.0
ML_F_BIAS = 3.0

SB_IN = 3 * D_MODEL
FOX_IN = 3 * D_MODEL + ATT_HEADS
ML_WIDTHS = [ML_HEADS * ML_DK, ML_HEADS * ML_DK, ML_HEADS * ML_DV, ML_HEADS * ML_DV, ML_HEADS, ML_HEADS]
GLA_WIDTHS = [GLA_HEADS * GLA_DK, GLA_HEADS * GLA_DK, GLA_HEADS * GLA_DV, GLA_HEADS * GLA_DV, GLA_RANK]

kernel_name = 'hybrid_sb_fox_mlstm_gla_trunk'

F32 = jnp.float32


def _split_cols(t, widths):
    idx = [int(i) for i in np.cumsum(widths)[:-1]]
    return jnp.split(t, idx, axis=-1)


def _heads(t, n):
    b, s, _ = t.shape
    return t.reshape(b, s, n, -1).transpose(0, 2, 1, 3)


def _merge(t):
    b, n, s, d = t.shape
    return t.transpose(0, 2, 1, 3).reshape(b, s, n * d)


def _to_chunks(t):
    b, hh, s = t.shape[:3]
    return jnp.moveaxis(t.reshape(b, hh, s // CHUNK, CHUNK, *t.shape[3:]), 2, 0)


def _from_chunks(t):
    t = jnp.moveaxis(t, 0, 2)
    return t.reshape(t.shape[0], t.shape[1], -1, *t.shape[4:])


def layer_norm(x, g, b):
    xf = x.astype(F32)
    mu = jnp.mean(xf, -1, keepdims=True)
    var = jnp.mean(jnp.square(xf - mu), -1, keepdims=True)
    return ((xf - mu) * lax.rsqrt(var + LN_EPS) * g + b).astype(x.dtype)


def stick_breaking_core(q, k, v):
    seq = q.shape[2]
    scale = q.shape[-1] ** -0.5
    qf, kf, vf = q.astype(F32), k.astype(F32), v.astype(F32)
    outs = []
    for start in range(0, seq, QBLK):
        end = start + QBLK
        z = jnp.einsum('bhtd,bhsd->bhts', qf[:, :, start:end], kf[:, :, :end]) * scale
        earlier = jnp.arange(end)[None, :] < jnp.arange(start, end)[:, None]
        log_keep = jnp.where(earlier, jax.nn.log_sigmoid(-z), 0.0)
        csum = jnp.cumsum(log_keep, axis=-1)
        log_w = jax.nn.log_sigmoid(z) + (csum[..., -1:] - csum)
        w = jnp.where(earlier, jnp.exp(log_w), 0.0)
        outs.append(jnp.einsum('bhts,bhsd->bhtd', w, vf[:, :, :end]))
    return jnp.concatenate(outs, axis=2).astype(v.dtype)


def mixer_stick_breaking(h, w_in):
    q, k, v = jnp.split(h @ w_in, 3, axis=-1)
    o = stick_breaking_core(_heads(q, ATT_HEADS), _heads(k, ATT_HEADS), _heads(v, ATT_HEADS))
    return _merge(o)


def forgetting_core(q, k, v, log_f):
    seq = q.shape[2]
    scale = q.shape[-1] ** -0.5
    qf, kf, vf = q.astype(F32), k.astype(F32), v.astype(F32)
    cum = jnp.cumsum(log_f.astype(F32), axis=-1)
    outs = []
    for start in range(0, seq, QBLK):
        end = start + QBLK
        z = jnp.einsum('bhtd,bhsd->bhts', qf[:, :, start:end], kf[:, :, :end]) * scale
        z = z + cum[:, :, start:end, None] - cum[:, :, None, :end]
        allowed = jnp.arange(end)[None, :] <= jnp.arange(start, end)[:, None]
        p = jax.nn.softmax(jnp.where(allowed, z, -jnp.inf), axis=-1)
        outs.append(jnp.einsum('bhts,bhsd->bhtd', p, vf[:, :, :end]))
    return jnp.concatenate(outs, axis=2).astype(v.dtype)


def mixer_forgetting(h, w_in, b_f):
    qkv, f_pre = _split_cols(h @ w_in, [3 * D_MODEL, ATT_HEADS])
    q, k, v = jnp.split(qkv, 3, axis=-1)
    log_f = jnp.swapaxes(jax.nn.log_sigmoid((f_pre + b_f).astype(F32)), 1, 2)
    o = forgetting_core(_heads(q, ATT_HEADS), _heads(k, ATT_HEADS), _heads(v, ATT_HEADS), log_f)
    return _merge(o)


def mlstm_core(q, k, v, i_pre, f_pre):
    b, nh, _, dk = q.shape
    dv = v.shape[-1]
    causal = jnp.tril(jnp.ones((CHUNK, CHUNK), dtype=bool))
    xs = (_to_chunks(q.astype(F32)), _to_chunks(k.astype(F32)), _to_chunks(v.astype(F32)),
          _to_chunks(i_pre.astype(F32)), _to_chunks(jax.nn.log_sigmoid(f_pre.astype(F32))))

    def step(carry, blk):
        c_st, n_st, m_st = carry
        qb, kb, vb, ib, lfb = blk
        bcum = jnp.cumsum(lfb, axis=-1)
        d = jnp.where(causal, bcum[..., :, None] - bcum[..., None, :] + ib[..., None, :], -jnp.inf)
        inter = bcum + m_st[..., None]
        m_t = jnp.maximum(inter, jnp.max(d, axis=-1))
        w_intra = jnp.exp(d - m_t[..., None])
        w_inter = jnp.exp(inter - m_t)
        qk = jnp.einsum('bhtd,bhsd->bhts', qb, kb) * w_intra
        num = w_inter[..., None] * jnp.einsum('bhtd,bhde->bhte', qb, c_st) + jnp.einsum('bhts,bhse->bhte', qk, vb)
        den = w_inter * jnp.einsum('bhtd,bhd->bht', qb, n_st) + jnp.sum(qk, axis=-1)
        h_out = num / jnp.maximum(jnp.abs(den), jnp.exp(-m_t))[..., None]
        m_new = m_t[..., -1]
        decay = jnp.exp(bcum[..., -1] + m_st - m_new)
        kw = kb * jnp.exp(bcum[..., -1:] - bcum + ib - m_new[..., None])[..., None]
        c_new = decay[..., None, None] * c_st + jnp.einsum('bhsd,bhse->bhde', kw, vb)
        n_new = decay[..., None] * n_st + jnp.sum(kw, axis=2)
        return (c_new, n_new, m_new), h_out

    init = (jnp.zeros((b, nh, dk, dv), F32), jnp.zeros((b, nh, dk), F32), jnp.zeros((b, nh), F32))
    _, hs = lax.scan(step, init, xs)
    return _from_chunks(hs).astype(v.dtype)


def mixer_mlstm(h, w_in, b_i, b_f):
    q, k, v, o, i_pre, f_pre = _split_cols(h @ w_in, ML_WIDTHS)
    i_pre = jnp.swapaxes((i_pre + b_i).astype(F32), 1, 2)
    f_pre = jnp.swapaxes((f_pre + b_f).astype(F32), 1, 2)
    hh = mlstm_core(_heads(q, ML_HEADS), _heads(k, ML_HEADS) * (ML_DK ** -0.5), _heads(v, ML_HEADS), i_pre, f_pre)
    return _merge(hh) * jax.nn.sigmoid(o)


def gla_core(q, k, v, log_a):
    b, nh, _, dk = q.shape
    dv = v.shape[-1]
    causal = jnp.tril(jnp.ones((CHUNK, CHUNK), dtype=bool))[:, :, None]
    xs = (_to_chunks(q.astype(F32)), _to_chunks(k.astype(F32)), _to_chunks(v.astype(F32)), _to_chunks(log_a.astype(F32)))

    def step(state, blk):
        qb, kb, vb, lab = blk
        bcum = jnp.cumsum(lab, axis=2)
        rel = jnp.where(causal, bcum[:, :, :, None, :] - bcum[:, :, None, :, :], -jnp.inf)
        att = jnp.einsum('bhtd,bhsd,bhtsd->bhts', qb, kb, jnp.exp(rel))
        o = jnp.einsum('bhts,bhse->bhte', att, vb) + jnp.einsum('bhtd,bhde->bhte', qb * jnp.exp(bcum), state)
        blast = bcum[:, :, -1:, :]
        new_state = jnp.exp(blast[:, :, 0, :])[..., None] * state + jnp.einsum('bhsd,bhse->bhde', kb * jnp.exp(blast - bcum), vb)
        return new_state, o

    _, os_ = lax.scan(step, jnp.zeros((b, nh, dk, dv), F32), xs)
    return _from_chunks(os_)


def mixer_gla(h, w_in, w_a2, b_a, norm_g):
    q, k, v, r, a_low = _split_cols(h @ w_in, GLA_WIDTHS)
    log_a = jax.nn.log_sigmoid((a_low @ w_a2 + b_a).astype(F32)) / GLA_TAU
    o = gla_core(_heads(q, GLA_HEADS) * (GLA_DK ** -0.5), _heads(k, GLA_HEADS), _heads(v, GLA_HEADS), _heads(log_a, GLA_HEADS))
    o = o * lax.rsqrt(jnp.mean(jnp.square(o), -1, keepdims=True) + RMS_EPS) * norm_g
    return _merge(o.astype(h.dtype)) * jax.nn.silu(r)


def mem_cross_attn(h, mem, w_q, w_kv, w_o):
    q = _heads(h @ w_q, XA_HEADS).astype(F32)
    k, v = jnp.split(mem @ w_kv, 2, axis=-1)
    k = _heads(k, XA_HEADS).astype(F32)
    v = _heads(v, XA_HEADS).astype(F32)
    p = jax.nn.softmax(jnp.einsum('bhtd,bhmd->bhtm', q, k) * (XA_HD ** -0.5), axis=-1)
    o = jnp.einsum('bhtm,bhmd->bhtd', p, v).astype(h.dtype)
    return _merge(o) @ w_o


def conv_ffn(h, w_up, conv_w, conv_b, w_down):
    u = h @ w_up
    ch = u.shape[-1]
    u = lax.conv_general_dilated(u, conv_w, window_strides=(1,), padding=((CONV_W - 1, 0),),
                                 dimension_numbers=('NWC', 'WIO', 'NWC'), feature_group_count=ch) + conv_b
    g, val = jnp.split(u, 2, axis=-1)
    return (jax.nn.gelu(g) * val) @ w_down


def _dense(key, shape, fan_in, gain=1.0):
    return jax.random.normal(key, shape, F32) * (gain * fan_in ** -0.5)


def setup_inputs(seed: int = 0) -> dict:
    key = jax.random.key(seed)
    ks = iter(jax.random.split(key, 32))
    D = D_MODEL
    n_occ = [len(range(m, DEPTH, N_MIXERS)) for m in range(N_MIXERS)]
    nrm = lambda shape, s: jax.random.normal(next(ks), shape, F32) * s
    return {
        'x': jax.random.normal(next(ks), (BATCH, SEQ, D), F32),
        'mem': jax.random.normal(next(ks), (BATCH, N_MEM, D), F32),
        'ln_g': 1.0 + nrm((DEPTH, 3, D), 0.02),
        'ln_b': nrm((DEPTH, 3, D), 0.02),
        'mix_wo': _dense(next(ks), (DEPTH, D, D), D, DN_BETA),
        'sb_win': _dense(next(ks), (n_occ[0], D, SB_IN), D),
        'fox_win': _dense(next(ks), (n_occ[1], D, FOX_IN), D),
        'fox_bf': FOX_F_BIAS + nrm((n_occ[1], ATT_HEADS), 0.1),
        'ml_win': _dense(next(ks), (n_occ[2], D, sum(ML_WIDTHS)), D),
        'ml_bi': nrm((n_occ[2], ML_HEADS), 0.1),
        'ml_bf': ML_F_BIAS + nrm((n_occ[2], ML_HEADS), 0.1),
        'gla_win': _dense(next(ks), (n_occ[3], D, sum(GLA_WIDTHS)), D),
        'gla_wa2': _dense(next(ks), (n_occ[3], GLA_RANK, GLA_HEADS * GLA_DK), GLA_RANK),
        'gla_ba': nrm((n_occ[3], GLA_HEADS * GLA_DK), 0.1),
        'gla_norm_g': 1.0 + nrm((n_occ[3], GLA_DV), 0.02),
        'xa_wq': _dense(next(ks), (DEPTH, D, D), D),
        'xa_wkv': _dense(next(ks), (DEPTH, D, 2 * D), D),
        'xa_wo': _dense(next(ks), (DEPTH, D, D), D, DN_BETA),
        'ffn_up': _dense(next(ks), (DEPTH, D, 2 * D_FF), D),
        'ffn_conv': _dense(next(ks), (DEPTH, CONV_W, 1, 2 * D_FF), CONV_W),
        'ffn_conv_b': nrm((DEPTH, 2 * D_FF), 0.02),
        'ffn_down': _dense(next(ks), (DEPTH, D_FF, D), D_FF, DN_BETA),
    }


def reference(x, mem, ln_g, ln_b, mix_wo, sb_win, fox_win, fox_bf, ml_win, ml_bi, ml_bf,
              gla_win, gla_wa2, gla_ba, gla_norm_g, xa_wq, xa_wkv, xa_wo,
              ffn_up, ffn_conv, ffn_conv_b, ffn_down):
    h = x
    for layer in range(DEPTH):
        kind = layer % N_MIXERS
        occ = layer // N_MIXERS
        if kind == 0:
            y = mixer_stick_breaking(h, sb_win[occ])
        elif kind == 1:
            y = mixer_forgetting(h, fox_win[occ], fox_bf[occ])
        elif kind == 2:
            y = mixer_mlstm(h, ml_win[occ], ml_bi[occ], ml_bf[occ])
        else:
            y = mixer_gla(h, gla_win[occ], gla_wa2[occ], gla_ba[occ], gla_norm_g[occ])
        h = layer_norm(DN_ALPHA * h + y @ mix_wo[layer], ln_g[layer, 0], ln_b[layer, 0])
        h = layer_norm(DN_ALPHA * h + mem_cross_attn(h, mem, xa_wq[layer], xa_wkv[layer], xa_wo[layer]),
                       ln_g[layer, 1], ln_b[layer, 1])
        h = layer_norm(DN_ALPHA * h + conv_ffn(h, ffn_up[layer], ffn_conv[layer], ffn_conv_b[layer], ffn_down[layer]),
                       ln_g[layer, 2], ln_b[layer, 2])
    return h
```

```python
import numpy as np
import concourse.bass as bass
import concourse.mybir as mybir
from concourse.bass_utils import run_bass_kernel_spmd
from contextlib import ExitStack

F32 = mybir.dt.float32
BF16 = mybir.dt.bfloat16
AF = mybir.ActivationFunctionType
ALU = mybir.AluOpType
AX = mybir.AxisListType

S = 2048
D = 1024
NT = 16
ALPHA = 8.0 ** 0.25
LN_EPS = 1e-5
DFF = 2816
NCH = 22


class Eng:
    def __init__(s, name, h, sem):
        s.name, s.h, s.sem, s.cnt, s.seen = name, h, sem, 0, {}


class DSem:
    def __init__(s, key, sem):
        s.key, s.sem, s.cnt = key, sem, 0


class TT:
    def __init__(s, t, name):
        s.t, s.name, s.w, s.r = t, name, None, {}


class Ctx:
    def __init__(s, nc, es):
        s.nc = nc
        s.stacks = [es]
        s.E = {}
        for name, h in (("pe", nc.tensor), ("act", nc.scalar), ("dve", nc.vector),
                        ("pool", nc.gpsimd), ("sp", nc.sync)):
            s.E[name] = Eng(name, h, es.enter_context(nc.semaphore("s_" + name)))
        s.dsems = {}
        s.nalloc = 0
        s.same_eng_sync = True

    def push(s):
        st = ExitStack()
        st.__enter__()
        s.stacks.append(st)

    def pop(s):
        s.barrier()
        st = s.stacks.pop()
        st.__exit__(None, None, None)

    def sb(s, shape, dt, name="t"):
        s.nalloc += 1
        name = f"{name}_{s.nalloc}"
        return TT(s.stacks[-1].enter_context(s.nc.sbuf_tensor(name, list(shape), dt)), name)

    def ps(s, shape, dt, name="p"):
        s.nalloc += 1
        name = f"{name}_{s.nalloc}"
        t = TT(s.stacks[-1].enter_context(s.nc.psum_tensor(name, list(shape), dt)), name)
        t.psum = True
        return t

    def dsem(s, key):
        if key not in s.dsems:
            s.dsems[key] = DSem("dma_" + key, s.stacks[0].enter_context(s.nc.semaphore("d_" + key)))
        return s.dsems[key]

    def _wait(s, E, key, sem, val):
        if E.seen.get(key, 0) >= val:
            return
        E.h.wait_ge(sem, val)
        E.seen[key] = val

    def _waits(s, E, reads, writes):
        deps = []
        for t in reads:
            if t.w: deps.append(t.w)
            if getattr(t, "psum", False):
                deps.extend(t.r.values())
        for t in writes:
            if t.w: deps.append(t.w)
            deps.extend(t.r.values())
        for (key, sem, val, ds) in deps:
            if key == E.name and (E.name == "pe" or not s.same_eng_sync):
                continue
            if ds is not None:
                val = max(val, ds.cnt)
            s._wait(E, key, sem, val)

    def op(s, eng, fn, reads=(), writes=()):
        E = s.E[eng]
        s._waits(E, reads, writes)
        ins = fn(E.h)
        E.cnt += 1
        ins.then_inc(E.sem, 1)
        tok = (E.name, E.sem, E.cnt, None)
        for t in reads: t.r[E.name] = tok
        for t in writes:
            t.w = tok; t.r = {}
        return ins

    def dma(s, q, out, in_, reads=(), writes=(), key="g"):
        E = s.E[q]
        s._waits(E, reads, writes)
        ds = s.dsem(key)
        ins = E.h.dma_start(out=out, in_=in_)
        ds.cnt += 16
        ins.then_inc(ds.sem, 16)
        tok = (ds.key, ds.sem, ds.cnt, ds)
        for t in reads: t.r[ds.key] = tok
        for t in writes:
            t.w = tok; t.r = {}
        return ins

    def barrier(s):
        engs = list(s.E.values())
        for E in engs:
            for F in engs:
                if F.cnt > 0:
                    s._wait(E, F.name, F.sem, F.cnt)
            for ds in s.dsems.values():
                if ds.cnt > 0:
                    s._wait(E, ds.key, ds.sem, ds.cnt)


class Rot:
    def __init__(s, items):
        s.items, s.i = list(items), 0

    def next(s):
        x = s.items[s.i % len(s.items)]
        s.i += 1
        return x


C_ID, C_LE, C_LT, C_UN, C_GT, NCON = 0, 128, 256, 2304, 2432, 2560


def make_consts():
    con = np.zeros((128, NCON), np.float32)
    p = np.arange(128)[:, None]
    f = np.arange(128)[None, :]
    con[:, C_ID:C_ID + 128] = (p == f)
    con[:, C_LE:C_LE + 128] = (p <= f)
    t = np.arange(512)[None, :]
    for o in range(4):
        con[:, C_LT + 512 * o:C_LT + 512 * (o + 1)] = ((o * 128 + p) < t)
    con[:, C_UN:C_UN + 128] = -1.0 * (p >= f)
    con[:, C_GT:C_GT + 128] = (p > f)
    con4 = np.zeros((4, 4 * 128 + 4 + 128), np.float32)
    for h in range(4):
        con4[h, h * 128:(h + 1) * 128] = 1.0
        con4[h, 512 + h] = 1.0
    con4[:, 516:644] = 1.0
    rmask = np.ones((128, S), np.float32)
    rmask[:, ::128] = 0.0
    return con, con4, rmask


def build(nlayers=4, stop=None, first=0):
    nc = bass.Bass("TRN2", target_bir_lowering=False)
    Dm = {}

    def din(name, shape):
        Dm[name] = nc.dram_tensor(name, list(shape), F32, kind="ExternalInput").ap()

    din("x", [S, D]); din("mem", [256, D]); din("ln_g", [12, D]); din("ln_b", [12, D])
    din("mix_wo", [4, D, D]); din("sb_win", [D, 3072]); din("fox_win", [D, 3088]); din("fox_bf", [1, 16])
    din("ml_win", [D, 3080]); din("ml_bi", [4, 1]); din("ml_bf", [4, 1])
    din("gla_win", [D, 3088]); din("gla_wa2", [16, 512]); din("gla_ba", [128, 4]); din("gla_norm_g", [1, 256])
    din("xa_wq", [4, D, D]); din("xa_wkv", [4, D, 2 * D]); din("xa_wo", [4, D, D])
    din("ffn_up", [4, D, 2 * DFF]); din("ffn_conv", [4, 128, 44 * 3]); din("ffn_conv_b", [4, 128, 44])
    din("ffn_down", [4, DFF, D])
    din("con", [128, NCON]); din("con4", [4, 644]); din("rmask", [128, S])
    out = nc.dram_tensor("out", [S, D], F32, kind="ExternalOutput").ap()

    with ExitStack() as es:
        c = Ctx(nc, es)
        H = [c.sb([128, D], F32, f"H{i}") for i in range(NT)]
        HT = [c.sb([128, 8, 512], BF16, f"HT{q}") for q in range(4)]
        memT = c.sb([128, 8, 256], BF16, "memT")
        CON = c.sb([128, NCON], BF16, "CON")
        ONEN = c.sb([128, 128], BF16, "ONEN")
        ident = CON.t[:, C_ID:C_ID + 128]
        maskLE = CON.t[:, C_LE:C_LE + 128]
        uneg = CON.t[:, C_UN:C_UN + 128]

        def maskLT(o):
            return CON.t[:, C_LT + 512 * o:C_LT + 512 * (o + 1)]

        c.dma("pool", CON.t[:], Dm["con"][:, :], writes=[CON], key="con")
        c.op("dve", lambda e: e.memset(ONEN.t[:], -1.0), [], [ONEN])
        for i in range(NT):
            c.dma("sp", H[i].t[:], Dm["x"][i * 128:(i + 1) * 128, :], writes=[H[i]], key="x")

        wcount = [0]

        def load_w(slot, ap_dst, src):
            wcount[0] += 1
            c.dma("pool", ap_dst, src.rearrange("(kc p) n -> p kc n", p=128), writes=[slot], key=slot.key)

        def wslot(shape, key, name="w"):
            t = c.sb(shape, BF16, name)
            t.key = key
            return t

        def to_ht(i, hb_rot, pT_rot):
            hb = hb_rot.next()
            c.op("act", lambda e: e.activation(out=hb.t[:], in_=H[i].t[:], func=AF.Copy), [H[i]], [hb])
            pT = pT_rot.next()
            for k in range(8):
                c.op("pe", lambda e: e.transpose(out=pT.t[:, k, :], in_=hb.t[:, k * 128:(k + 1) * 128], identity=ident),
                     [hb, CON], [pT])
            q, j = divmod(i, 4)
            c.op("dve", lambda e: e.tensor_copy(out=HT[q].t[:, :, j * 128:(j + 1) * 128], in_=pT.t[:]), [pT], [HT[q]])

        def ln_tile(i, Sx, gB, bB, R):
            st = R["st"].next(); mv = R["mv"].next(); sm = R["sm"].next(); T1 = R["T1"].next()
            for hh in range(2):
                c.op("dve", lambda e: e.bn_stats(out=st.t[:, hh, :], in_=Sx.t[:, hh * 512:(hh + 1) * 512]), [Sx], [st])
            c.op("dve", lambda e: e.bn_aggr(out=mv.t[:], in_=st.t[:]), [st], [mv])
            c.op("act", lambda e: e.activation(out=sm.t[:, 0:1], in_=mv.t[:, 1:2], func=AF.Ln, bias=R["eps"].t[:, 0:1]), [mv, R["eps"]], [sm])
            c.op("act", lambda e: e.activation(out=sm.t[:, 1:2], in_=sm.t[:, 0:1], func=AF.Exp, scale=-0.5), [sm], [sm])
            c.op("dve", lambda e: e.tensor_scalar(out=sm.t[:, 2:3], in0=mv.t[:, 0:1], scalar1=sm.t[:, 1:2], scalar2=-1.0,
                                                  op0=ALU.mult, op1=ALU.mult), [mv, sm], [sm])
            c.op("act", lambda e: e.activation(out=T1.t[:], in_=Sx.t[:], func=AF.Identity, bias=sm.t[:, 2:3], scale=sm.t[:, 1:2]),
                 [Sx, sm], [T1])
            c.op("pool", lambda e: e.tensor_tensor(out=T1.t[:], in0=T1.t[:], in1=gB.t[:], op=ALU.mult), [T1, gB], [T1])
            c.op("pool", lambda e: e.tensor_tensor(out=H[i].t[:], in0=T1.t[:], in1=bB.t[:], op=ALU.add), [T1, bB], [H[i]])
            hb = R["hb"].next()
            c.op("act", lambda e: e.activation(out=hb.t[:], in_=H[i].t[:], func=AF.Copy), [H[i]], [hb])
            return hb

        def ht_from_hb(i, hb, pT_rot):
            pT = pT_rot.next()
            for k in range(8):
                c.op("pe", lambda e: e.transpose(out=pT.t[:, k, :], in_=hb.t[:, k * 128:(k + 1) * 128], identity=ident),
                     [hb, CON], [pT])
            q, j = divmod(i, 4)
            c.op("dve", lambda e: e.tensor_copy(out=HT[q].t[:, :, j * 128:(j + 1) * 128], in_=pT.t[:]), [pT], [HT[q]])

        def ln_res(lnidx):
            R = {}
            R["st"] = Rot([c.sb([128, 2, 6], F32, "st") for _ in range(2)])
            R["mv"] = Rot([c.sb([128, 2], F32, "mv") for _ in range(2)])
            R["sm"] = Rot([c.sb([128, 4], F32, "sm") for _ in range(2)])
            R["T1"] = Rot([c.sb([128, D], F32, "T1") for _ in range(2)])
            R["hb"] = Rot([c.sb([128, D], BF16, "hb") for _ in range(3)])
            R["pT"] = Rot([c.ps([128, 8, 128], BF16, "pT") for _ in range(2)])
            R["eps"] = c.sb([128, 1], F32, "eps")
            c.op("dve", lambda e: e.memset(R["eps"].t[:], LN_EPS), [], [R["eps"]])
            gB = c.sb([128, D], F32, "gB"); bB = c.sb([128, D], F32, "bB")
            c.dma("sp", gB.t[:], Dm["ln_g"][lnidx:lnidx + 1, :].partition_broadcast(128), writes=[gB], key="lng")
            c.dma("sp", bB.t[:], Dm["ln_b"][lnidx:lnidx + 1, :].partition_broadcast(128), writes=[bB], key="lnb")
            return R, gB, bB

        def out_proj_ln(KT, w_dram, lnidx):
            c.push()
            wo = [wslot([128, 8, 512], f"w{j}") for j in range(2)]
            for j in range(2):
                load_w(wo[j], wo[j].t[:], w_dram[:, j * 512:(j + 1) * 512])
            R, gB, bB = ln_res(lnidx)
            PB = Rot([c.ps([128, 512], F32, "po") for _ in range(4)])
            SX = Rot([c.sb([128, D], F32, "SX") for _ in range(3)])
            sxs = {}; hbs = {}

            def sA(i):
                Sx = SX.next(); sxs[i] = Sx
                for j in range(2):
                    pb = PB.next()
                    for k in range(8):
                        c.op("pe", lambda e: e.matmul(pb.t[:], lhsT=KT[k].t[:, i * 128:(i + 1) * 128], rhs=wo[j].t[:, k, :],
                                                      start=(k == 0), stop=(k == 7)), [KT[k], wo[j]], [pb])
                    c.op("dve", lambda e: e.scalar_tensor_tensor(out=Sx.t[:, j * 512:(j + 1) * 512], in0=H[i].t[:, j * 512:(j + 1) * 512],
                                                                 scalar=ALPHA, in1=pb.t[:], op0=ALU.mult, op1=ALU.add),
                         [H[i], pb], [Sx])

            for n in range(NT + 2):
                if n < NT: sA(n)
                if 0 <= n - 1 < NT: hbs[n - 1] = ln_tile(n - 1, sxs[n - 1], gB, bB, R)
                if 0 <= n - 2 < NT: ht_from_hb(n - 2, hbs[n - 2], R["pT"])
            c.pop()

        def proj_fm(pb, w, wap_fn, tq):
            for k in range(8):
                c.op("pe", lambda e: e.matmul(pb_ap(pb, wap_fn(k)), lhsT=wap_fn(k), rhs=HT[tq].t[:, k, :], start=(k == 0), stop=(k == 7)),
                     [w, HT[tq]], [pb])

        def pb_ap(pb, lhsT):
            m = lhsT.shape[-1]
            return pb.t[0:m, :]

        def proj_tm(pb, out_ap, w, wap_fn, i):
            q, j = divmod(i, 4)
            for k in range(8):
                c.op("pe", lambda e: e.matmul(out_ap, lhsT=HT[q].t[:, k, j * 128:(j + 1) * 128], rhs=wap_fn(k), start=(k == 0), stop=(k == 7)),
                     [w, HT[q]], [pb])

        def mixer_sb(win):
            c.push()
            YT = [c.sb([128, S], BF16, f"YT{k}") for k in range(8)]
            c.push()
            wq = [wslot([128, 8, 128], f"w{j}") for j in range(2)]
            wk = [wslot([128, 8, 128], f"w{2 + j}") for j in range(2)]
            wv = [wslot([128, 8, 128], f"w{4 + j}") for j in range(2)]
            qT = [c.sb([128, S], BF16, "qT") for _ in range(2)]
            kT = [c.sb([128, S], BF16, "kT") for _ in range(2)]
            V = [c.sb([128, NT, 128], BF16, "V") for _ in range(2)]
            Yp = c.sb([128, NT, 128], BF16, "Yp")
            Et = Rot([c.sb([128, 512], F32, "E") for _ in range(2)])
            SPt = Rot([c.sb([128, 512], F32, "SP") for _ in range(2)])
            LmP = Rot([c.sb([128, 512], BF16, "Lm") for _ in range(5)])
            Ssum = Rot([c.sb([128, 512], BF16, "Ss") for _ in range(5)])
            WT = Rot([c.sb([128, 512], BF16, "WT") for _ in range(4)])
            PZ = Rot([c.ps([128, 512], F32, "pz") for _ in range(4)])
            PY = Rot([c.ps([128, 512], F32, "py") for _ in range(2)])
            PP = Rot([c.ps([128, 512], F32, "pp") for _ in range(1)])
            PTr = Rot([c.ps([128, 8, 128], BF16, "ptr") for _ in range(1)])
            for t_ in SPt.items:
                c.op("dve", lambda e: e.memset(t_.t[:], 0.0), [], [t_])

            def load_pair(p):
                b = p % 2
                load_w(wq[b], wq[b].t[:], win[:, p * 128:(p + 1) * 128])
                load_w(wk[b], wk[b].t[:], win[:, 1024 + p * 128:1024 + (p + 1) * 128])
                load_w(wv[b], wv[b].t[:], win[:, 2048 + p * 128:2048 + (p + 1) * 128])

            def proj_items(p):
                bb = p % 2
                items = []
                for tq in range(4):
                    def fq(tq=tq):
                        pb = PP.next()
                        proj_fm(pb, wq[bb], lambda k: wq[bb].t[:, k, :], tq)
                        c.op("act", lambda e: e.activation(out=qT[bb].t[:, tq * 512:(tq + 1) * 512], in_=pb.t[:], func=AF.Copy, scale=0.125),
                             [pb], [qT[bb]])

                    def fk(tq=tq):
                        pb = PP.next()
                        proj_fm(pb, wk[bb], lambda k: wk[bb].t[:, k, :], tq)
                        c.op("dve", lambda e: e.tensor_copy(out=kT[bb].t[:, tq * 512:(tq + 1) * 512], in_=pb.t[:]), [pb], [kT[bb]])
                    items += [fq, fk]
                for i4 in range(4):
                    def fv(i4=i4):
                        pb = PP.next()
                        for jj in range(4):
                            proj_tm(pb, pb.t[:, jj * 128:(jj + 1) * 128], wv[bb], lambda k: wv[bb].t[:, k, :], i4 * 4 + jj)
                        c.op("act", lambda e: e.activation(out=V[bb].t[:, i4 * 4:(i4 + 1) * 4, :],
                                                           in_=pb.t[:].rearrange("p (a b) -> p a b", a=4), func=AF.Copy), [pb], [V[bb]])
                    items.append(fv)
                return items

            load_pair(0)
            load_pair(1)
            for it in proj_items(0):
                it()
            for p in range(8):
                b = p % 2
                if p + 2 < 8:
                    load_pair(p + 2)
                nxt = proj_items(p + 1) if p + 1 < 8 else []
                steps = []
                for hh in range(2):
                    for g in range(4):
                        grp = {"py": None, "first": True, "ss": None}
                        for idx, kb in enumerate(range(4 * g + 3, -1, -1)):
                            steps.append(dict(hh=hh, g=g, kb=kb, grp=grp))

                def stA(st):
                    hh, g, kb, grp = st["hh"], st["g"], st["kb"], st["grp"]
                    lo, hi = 64 * hh, 64 * hh + 64
                    o = kb - 4 * g
                    pz = PZ.next(); st["pz"] = pz
                    c0 = max(o, 0) * 128
                    c.op("pe", lambda e: e.matmul(pz.t[:, c0:512], lhsT=kT[b].t[lo:hi, kb * 128:(kb + 1) * 128],
                                                  rhs=qT[b].t[lo:hi, g * 512 + c0:(g + 1) * 512], start=True, stop=False, skip_group_check=True),
                         [kT[b], qT[b]], [pz])
                    E_ = Et.next(); SP_ = SPt.next(); Lm = LmP.next()
                    c.op("act", lambda e: e.activation(out=E_.t[:, c0:512], in_=pz.t[:, c0:512], func=AF.Exp), [pz], [E_])
                    c.op("act", lambda e: e.activation(out=SP_.t[:, c0:512], in_=E_.t[:, c0:512], func=AF.Ln, bias=1.0), [E_], [SP_])
                    if o >= 0:
                        c.op("dve", lambda e: e.tensor_tensor(out=Lm.t[:], in0=SP_.t[:], in1=maskLT(o), op=ALU.mult), [SP_, CON], [Lm])
                    else:
                        c.op("dve", lambda e: e.tensor_copy(out=Lm.t[:], in_=SP_.t[:]), [SP_], [Lm])
                    st["Lm"] = Lm
                    st["ss_prev"] = grp["ss"]
                    if kb > 0:
                        if grp["ss"] is None:
                            grp["ss"] = Lm
                        else:
                            ss_new = Ssum.next()
                            sp0 = grp["ss"]
                            c.op("pool", lambda e: e.tensor_tensor(out=ss_new.t[:], in0=sp0.t[:], in1=Lm.t[:], op=ALU.add), [sp0, Lm], [ss_new])
                            grp["ss"] = ss_new

                def stB(st):
                    g, kb = st["g"], st["kb"]
                    o = kb - 4 * g
                    pz, Lm, ss_prev = st["pz"], st["Lm"], st["ss_prev"]
                    c0 = max(o, 0) * 128
                    c.op("pe", lambda e: e.matmul(pz.t[:, c0:512], lhsT=uneg, rhs=Lm.t[:, c0:512], start=False, stop=(ss_prev is None), skip_group_check=True),
                         [CON, Lm], [pz])
                    if ss_prev is not None:
                        c.op("pe", lambda e: e.matmul(pz.t[:, c0:512], lhsT=ONEN.t[:], rhs=ss_prev.t[:, c0:512], start=False, stop=True, skip_group_check=True),
                             [ONEN, ss_prev], [pz])
                    W_ = WT.next(); st["W"] = W_
                    c.op("act", lambda e: e.activation(out=W_.t[:, c0:512], in_=pz.t[:, c0:512], func=AF.Exp), [pz], [W_])
                    if o >= 0:
                        c.op("dve", lambda e: e.tensor_tensor(out=W_.t[:, c0:512], in0=W_.t[:, c0:512], in1=maskLT(o)[:, c0:512], op=ALU.mult), [W_, CON], [W_])

                def stC(st):
                    hh, g, kb, grp = st["hh"], st["g"], st["kb"], st["grp"]
                    lo, hi = 64 * hh, 64 * hh + 64
                    if grp["py"] is None:
                        grp["py"] = PY.next()
                    py = grp["py"]; W_ = st["W"]
                    for jq in range(4):
                        if 4 * g + jq < kb:
                            continue
                        c.op("pe", lambda e: e.matmul(py.t[:, jq * 64:(jq + 1) * 64], lhsT=W_.t[:, jq * 128:(jq + 1) * 128],
                                                      rhs=V[b].t[:, kb, lo:hi], start=grp["first"], stop=(kb == 0 and jq == 3),
                                                      skip_group_check=True), [W_, V[b]], [py])
                        grp["first"] = False
                    if kb == 0:
                        c.op("act", lambda e: e.activation(out=Yp.t[:, 4 * g:4 * g + 4, lo:hi],
                                                           in_=py.t[:, 0:256].rearrange("p (a b) -> p a b", a=4), func=AF.Copy), [py], [Yp])

                ns = len(steps)
                for n in range(ns + 3):
                    if n < ns: stA(steps[n])
                    if 0 <= n - 2 < ns: stB(steps[n - 2])
                    if 0 <= n - 3 < ns: stC(steps[n - 3])
                    if n % 6 == 3 and nxt:
                        nxt.pop(0)()
                for it in nxt:
                    it()
                for i2 in range(2):
                    ptr = PTr.next()
                    for jj in range(8):
                        i = i2 * 8 + jj
                        c.op("pe", lambda e: e.transpose(out=ptr.t[:, jj, :], in_=Yp.t[:, i, :], identity=ident), [Yp, CON], [ptr])
                    c.op("dve", lambda e: e.tensor_copy(out=YT[p].t[:, i2 * 1024:(i2 + 1) * 1024],
                                                        in_=ptr.t[:].rearrange("p a b -> p (a b)")), [ptr], [YT[p]])
            c.pop()
            return YT


        def mixer_fox(win, bf_dram):
            c.push()
            YT = [c.sb([128, S], BF16, f"YT{k}") for k in range(8)]
            c.push()
            wq = [wslot([128, 8, 128], f"w{j}") for j in range(2)]
            wk = [wslot([128, 8, 128], f"w{2 + j}") for j in range(2)]
            wv = [wslot([128, 8, 128], f"w{4 + j}") for j in range(2)]
            wf = wslot([128, 8, 16], "w6")
            qT = [c.sb([128, S], BF16, "qT") for _ in range(2)]
            kT = [c.sb([128, S], BF16, "kT") for _ in range(2)]
            V = [c.sb([128, NT, 2, 65], BF16, "V") for _ in range(2)]
            Yp = c.sb([128, NT, 128], BF16, "Yp")
            WT = Rot([c.sb([128, 512], BF16, "WT") for _ in range(4)])
            Bh = Rot([c.sb([128, 8, 16], F32, "Bh") for _ in range(4)])
            RD = Rot([c.sb([128, 4, 1], F32, "rd") for _ in range(2)])
            lfc = c.sb([128, 256], F32, "lfc"); tmp = c.sb([128, 256], F32, "ftmp")
            T2 = c.sb([128, 256], F32, "T2"); PSc = c.sb([128, 256], F32, "PSc")
            ugt = c.sb([128, 128], F32, "ugt"); onef = c.sb([128, 128], F32, "onef")
            bfB = c.sb([128, 16], F32, "bfB")
            PZ = Rot([c.ps([128, 512], F32, "pz") for _ in range(4)])
            PY = Rot([c.ps([128, 512], F32, "py") for _ in range(2)])
            PP = Rot([c.ps([128, 512], F32, "pp") for _ in range(1)])
            PTr = Rot([c.ps([128, 8, 128], BF16, "ptr") for _ in range(1)])
            for b in range(2):
                c.op("dve", lambda e: e.memset(V[b].t[:, :, :, 64:65], 1.0), [], [V[b]])
            c.op("dve", lambda e: e.memset(onef.t[:], 1.0), [], [onef])
            c.dma("sp", ugt.t[:], Dm["con"][:, C_GT:C_GT + 128], writes=[ugt], key="cf")
            c.dma("sp", bfB.t[:], bf_dram[0:1, :].partition_broadcast(128), writes=[bfB], key="bf")
            load_w(wf, wf.t[:], win[:, 3072:3088])

            def load_pair(p):
                b = p % 2
                load_w(wq[b], wq[b].t[:], win[:, p * 128:(p + 1) * 128])
                load_w(wk[b], wk[b].t[:], win[:, 1024 + p * 128:1024 + (p + 1) * 128])
                load_w(wv[b], wv[b].t[:], win[:, 2048 + p * 128:2048 + (p + 1) * 128])

            load_pair(0)
            for i in range(NT):
                pb = PP.next()
                proj_tm(pb, pb.t[:, 0:16], wf, lambda k: wf.t[:, k, :], i)
                c.op("dve", lambda e: e.tensor_tensor(out=lfc.t[:, i * 16:(i + 1) * 16], in0=pb.t[:, 0:16], in1=bfB.t[:], op=ALU.add),
                     [pb, bfB], [lfc])
            c.op("act", lambda e: e.activation(out=tmp.t[:], in_=lfc.t[:], func=AF.Exp, scale=-1.0), [lfc], [tmp])
            c.op("act", lambda e: e.activation(out=tmp.t[:], in_=tmp.t[:], func=AF.Ln, bias=1.0), [tmp], [tmp])
            c.op("dve", lambda e: e.tensor_scalar(out=lfc.t[:], in0=tmp.t[:], scalar1=-1.0, scalar2=None, op0=ALU.mult), [tmp], [lfc])
            pb = PP.next()
            c.op("pe", lambda e: e.matmul(pb.t[:, 0:256], lhsT=onef.t[:], rhs=lfc.t[:], start=True, stop=True), [onef, lfc], [pb])
            c.op("dve", lambda e: e.tensor_copy(out=PSc.t[:, 0:16], in_=pb.t[:, 0:16]), [pb], [PSc])
            for jb in range(1, 16):
                c.op("dve", lambda e: e.tensor_tensor(out=PSc.t[:, jb * 16:(jb + 1) * 16], in0=PSc.t[:, (jb - 1) * 16:jb * 16],
                                                      in1=pb.t[:, jb * 16:(jb + 1) * 16], op=ALU.add), [pb, PSc], [PSc])
            pb = PP.next()
            c.op("pe", lambda e: e.matmul(pb.t[:, 0:256], lhsT=ugt.t[:], rhs=lfc.t[:], start=True, stop=True), [ugt, lfc], [pb])
            c.op("dve", lambda e: e.tensor_tensor(out=T2.t[:], in0=pb.t[:, 0:256], in1=PSc.t[:], op=ALU.subtract), [pb, PSc], [T2])
            T2v = T2.t[:].rearrange("p (a b) -> p a b", a=16)

            def proj_items(p):
                bb = p % 2
                items = []
                for tq in range(4):
                    def fq(tq=tq):
                        pb = PP.next()
                        proj_fm(pb, wq[bb], lambda k: wq[bb].t[:, k, :], tq)
                        c.op("act", lambda e: e.activation(out=qT[bb].t[:, tq * 512:(tq + 1) * 512], in_=pb.t[:], func=AF.Copy, scale=0.125),
                             [pb], [qT[bb]])

                    def fk(tq=tq):
                        pb = PP.next()
                        proj_fm(pb, wk[bb], lambda k: wk[bb].t[:, k, :], tq)
                        c.op("dve", lambda e: e.tensor_copy(out=kT[bb].t[:, tq * 512:(tq + 1) * 512], in_=pb.t[:]), [pb], [kT[bb]])
                    items += [fq, fk]
                for i4 in range(4):
                    def fv(i4=i4):
                        pb = PP.next()
                        for jj in range(4):
                            proj_tm(pb, pb.t[:, jj * 128:(jj + 1) * 128], wv[bb], lambda k: wv[bb].t[:, k, :], i4 * 4 + jj)
                        for hh in range(2):
                            c.op("act", lambda e: e.activation(out=V[bb].t[:, i4 * 4:(i4 + 1) * 4, hh, 0:64],
                                                               in_=pb.t[:].rearrange("p (a h d) -> p a h d", a=4, h=2)[:, :, hh, :], func=AF.Copy),
                                 [pb], [V[bb]])
                    items.append(fv)
                return items

            PSm = c.sb([128, 8, 16], F32, "PSm")
            PSv = PSc.t[:].rearrange("p (q two h) -> p q two h", two=2, h=16)
            c.op("dve", lambda e: e.tensor_tensor(out=PSm.t[:], in0=PSv[:, :, 0, :], in1=PSv[:, :, 1, :], op=ALU.add), [PSc], [PSm])
            c.op("dve", lambda e: e.tensor_scalar(out=PSm.t[:], in0=PSm.t[:], scalar1=0.5, scalar2=None, op0=ALU.mult), [PSm], [PSm])
            load_pair(1)
            for it in proj_items(0):
                it()
            for p in range(8):
                b = p % 2
                if p + 2 < 8:
                    load_pair(p + 2)
                nxt = proj_items(p + 1) if p + 1 < 8 else []
                steps = []
                for hh in range(2):
                    h = 2 * p + hh
                    B_ = Bh.next()
                    for pr in range(8):
                        c.op("dve", lambda e: e.tensor_scalar(out=B_.t[:, pr, :], in0=T2v[:, :, h], scalar1=PSm.t[:, pr, h:h + 1],
                                                              scalar2=None, op0=ALU.add), [T2, PSm], [B_])
                    for g in range(4):
                        grp = {"py": None, "first": True}
                        for kb in range(4 * g + 4):
                            steps.append(dict(hh=hh, g=g, kb=kb, grp=grp, B=B_))

                def fA(st):
                    hh, g, kb = st["hh"], st["g"], st["kb"]
                    lo, hi = 64 * hh, 64 * hh + 64
                    pz = PZ.next(); st["pz"] = pz
                    c.op("pe", lambda e: e.matmul(pz.t[:], lhsT=kT[b].t[lo:hi, kb * 128:(kb + 1) * 128],
                                                  rhs=qT[b].t[lo:hi, g * 512:(g + 1) * 512], start=True, stop=True), [kT[b], qT[b]], [pz])

                def fB(st):
                    g, kb, pz, B_ = st["g"], st["kb"], st["pz"], st["B"]
                    W_ = WT.next(); st["W"] = W_
                    for p2 in range(2):
                        if 4 * g + 2 * p2 + 1 < kb:
                            continue
                        pr = 2 * g + p2
                        c.op("act", lambda e: e.activation(out=W_.t[:, p2 * 256:(p2 + 1) * 256], in_=pz.t[:, p2 * 256:(p2 + 1) * 256],
                                                           func=AF.Exp, bias=B_.t[:, pr, kb:kb + 1]), [pz, B_], [W_])
                    for jq in range(4):
                        Q = 4 * g + jq
                        if Q == kb:
                            c.op("pool", lambda e: e.tensor_tensor(out=W_.t[:, jq * 128:(jq + 1) * 128], in0=W_.t[:, jq * 128:(jq + 1) * 128],
                                                                   in1=maskLE, op=ALU.mult), [W_, CON], [W_])

                def fC(st):
                    hh, g, kb, grp, W_ = st["hh"], st["g"], st["kb"], st["grp"], st["W"]
                    lo, hi = 64 * hh, 64 * hh + 64
                    if grp["py"] is None:
                        grp["py"] = PY.next()
                    py = grp["py"]
                    for jq in range(4):
                        Q = 4 * g + jq
                        if Q < kb:
                            continue
                        c.op("pe", lambda e: e.matmul(py.t[:, jq * 65:jq * 65 + 65], lhsT=W_.t[:, jq * 128:(jq + 1) * 128],
                                                      rhs=V[b].t[:, kb, hh, :], start=grp["first"], stop=(kb == 4 * g + 3 and jq == 3),
                                                      skip_group_check=True), [W_, V[b]], [py])
                        grp["first"] = False
                    if kb == 4 * g + 3:
                        rd = RD.next()
                        pyv = py.t[:, 0:260].rearrange("p (a b) -> p a b", a=4)
                        c.op("dve", lambda e: e.reciprocal(out=rd.t[:], in_=pyv[:, :, 64:65]), [py], [rd])
                        for jq in range(4):
                            c.op("act", lambda e: e.activation(out=Yp.t[:, 4 * g + jq, lo:hi], in_=py.t[:, jq * 65:jq * 65 + 64], func=AF.Copy,
                                                               scale=rd.t[:, jq, :]), [py, rd], [Yp])

                ns = len(steps)
                for n in range(ns + 3):
                    if n < ns: fA(steps[n])
                    if 0 <= n - 2 < ns: fB(steps[n - 2])
                    if 0 <= n - 3 < ns: fC(steps[n - 3])
                    if n % 6 == 3 and nxt:
                        nxt.pop(0)()
                for it in nxt:
                    it()
                for i2 in range(2):
                    ptr = PTr.next()
                    for jj in range(8):
                        i = i2 * 8 + jj
                        c.op("pe", lambda e: e.transpose(out=ptr.t[:, jj, :], in_=Yp.t[:, i, :], identity=ident), [Yp, CON], [ptr])
                    c.op("dve", lambda e: e.tensor_copy(out=YT[p].t[:, i2 * 1024:(i2 + 1) * 1024],
                                                        in_=ptr.t[:].rearrange("p a b -> p (a b)")), [ptr], [YT[p]])
            c.pop()
            return YT


        def mixer_mlstm(win):
            c.push()
            YT = [c.sb([128, S], BF16, f"YT{k}") for k in range(8)]
            aT = c.sb([4, S], F32, "aT"); nG = c.sb([4, S], F32, "nG")
            cols = c.sb([128, 16, 16], F32, "cols")
            c4 = c.sb([4, 644], F32, "c4")
            c.dma("sp", c4.t[:], Dm["con4"][:, :], writes=[c4], key="cf")
            I4 = c4.t[:, 512:516]; ones4 = c4.t[:, 516:644]
            c.push()
            wi = wslot([128, 8, 4], "w6"); wf = wslot([128, 8, 4], "w7")
            load_w(wi, wi.t[:], win[:, 3072:3076]); load_w(wf, wf.t[:], win[:, 3076:3080])
            bi = c.sb([4, 1], F32, "bi"); bfv = c.sb([4, 1], F32, "bfv")
            c.dma("sp", bi.t[:], Dm["ml_bi"][:, :], writes=[bi], key="bi")
            c.dma("sp", bfv.t[:], Dm["ml_bf"][:, :], writes=[bfv], key="bf")
            c.op("dve", lambda e: e.tensor_scalar(out=bfv.t[:], in0=bfv.t[:], scalar1=-1.0, scalar2=None, op0=ALU.mult), [bfv], [bfv])
            iT = c.sb([4, S], F32, "iT"); t4 = c.sb([4, S], F32, "t4"); Fp = c.sb([4, S], F32, "Fp"); G_ = c.sb([4, S], F32, "G")
            GP = c.sb([4, 16, 3], F32, "GP"); Dg = c.sb([4, 16, 12], F32, "Dg")
            PP = Rot([c.ps([128, 512], F32, "pp") for _ in range(2)])
            PCo = c.ps([128, 512], F32, "pco")
            for tq in range(4):
                pb = PP.next()
                proj_fm(pb, wi, lambda k: wi.t[:, k, :], tq)
                c.op("act", lambda e: e.activation(out=iT.t[:, tq * 512:(tq + 1) * 512], in_=pb.t[0:4, :], func=AF.Identity, bias=bi.t[:, 0:1]),
                     [pb, bi], [iT])
                pb = PP.next()
                proj_fm(pb, wf, lambda k: wf.t[:, k, :], tq)
                c.op("act", lambda e: e.activation(out=t4.t[:, tq * 512:(tq + 1) * 512], in_=pb.t[0:4, :], func=AF.Exp, bias=bfv.t[:, 0:1], scale=-1.0),
                     [pb, bfv], [t4])
            c.op("act", lambda e: e.activation(out=t4.t[:], in_=t4.t[:], func=AF.Ln, bias=1.0), [t4], [t4])
            c.op("dve", lambda e: e.tensor_tensor_scan(out=Fp.t[:], data0=t4.t[:], data1=t4.t[:], initial=0.0, op0=ALU.add, op1=ALU.max),
                 [t4], [Fp])
            c.op("dve", lambda e: e.tensor_tensor(out=aT.t[:], in0=iT.t[:], in1=Fp.t[:], op=ALU.add), [iT, Fp], [aT])
            c.op("dve", lambda e: e.tensor_tensor_scan(out=G_.t[:], data0=aT.t[:], data1=aT.t[:], initial=0.0, op0=ALU.max, op1=ALU.max),
                 [aT], [G_])
            c.op("dve", lambda e: e.tensor_scalar(out=nG.t[:], in0=G_.t[:], scalar1=-1.0, scalar2=None, op0=ALU.mult), [G_], [nG])
            c.op("dve", lambda e: e.tensor_tensor(out=iT.t[:], in0=Fp.t[:], in1=G_.t[:], op=ALU.subtract), [Fp, G_], [iT])
            nM = iT
            Gend = G_.t[:].rearrange("p (c t) -> p c t", t=128)[:, :, 127]
            c.op("dve", lambda e: e.memset(GP.t[:], 0.0), [], [GP])
            c.op("dve", lambda e: e.tensor_copy(out=GP.t[:, 1:16, 0], in_=G_.t[:].rearrange("p (c t) -> p c t", t=128)[:, 0:15, 127]), [G_], [GP])
            c.op("dve", lambda e: e.tensor_scalar(out=GP.t[:, :, 1], in0=Gend, scalar1=-1.0, scalar2=None, op0=ALU.mult), [G_], [GP])
            c.op("dve", lambda e: e.tensor_tensor(out=GP.t[:, :, 2], in0=GP.t[:, :, 0], in1=GP.t[:, :, 1], op=ALU.add), [GP], [GP])
            for cc in range(16):
                for j in range(3):
                    c.op("dve", lambda e: e.tensor_scalar(out=Dg.t[:, cc, j * 4:(j + 1) * 4], in0=I4, scalar1=GP.t[:, cc, j:j + 1], scalar2=None,
                                                          op0=ALU.mult), [c4, GP], [Dg])
            for cc in range(16):
                sl = slice(cc * 128, (cc + 1) * 128)
                o0 = cc * 16
                mmx = lambda oc, l, r, st, sp_: c.op("pe", lambda e: e.matmul(PCo.t[:, o0 + oc:o0 + oc + 4], lhsT=l, rhs=r, start=st, stop=sp_,
                                                                              skip_group_check=True), [nG, aT, nM, c4, Dg], [PCo])
                mmx(0, nG.t[:, sl], I4, True, False); mmx(0, ones4, Dg.t[:, cc, 0:4], False, True)
                mmx(4, nM.t[:, sl], I4, True, True)
                mmx(8, aT.t[:, sl], I4, True, False); mmx(8, ones4, Dg.t[:, cc, 4:8], False, True)
                mmx(12, ones4, Dg.t[:, cc, 8:12], True, True)
            c.op("act", lambda e: e.activation(out=cols.t[:].rearrange("p a b -> p (a b)"), in_=PCo.t[:, 0:256], func=AF.Exp), [PCo], [cols])
            c.pop()
            c.push()
            wq = [wslot([128, 8, 128], f"w{j}") for j in range(2)]
            wk = [wslot([128, 8, 128], f"w{2 + j}") for j in range(2)]
            wv = [wslot([128, 8, 256], f"w{4 + j}") for j in range(2)]
            wo_ = [wslot([128, 8, 256], f"w{6 + j}") for j in range(2)]
            qTc = Rot([c.sb([128, 128], BF16, "qTc") for _ in range(4)])
            kTc = Rot([c.sb([128, 128], BF16, "kTc") for _ in range(2)])
            kw = Rot([c.sb([128, 128], BF16, "kw") for _ in range(4)])
            Va = Rot([c.sb([128, 257], BF16, "Va") for _ in range(4)])
            Wt = Rot([c.sb([128, 128], F32, "Wt") for _ in range(2)])
            PTt = Rot([c.sb([128, 128], BF16, "PTt") for _ in range(4)])
            ONEF = c.sb([128, 256], F32, "ONEF")
            c.op("pool", lambda e: e.memset(ONEF.t[:], -1.0), [], [ONEF])
            tI = Rot([c.sb([128, 257], F32, "tI") for _ in range(2)])
            tot = Rot([c.sb([128, 257], F32, "tot") for _ in range(2)])
            sg = Rot([c.sb([128, 256], F32, "sg") for _ in range(4)])
            yh = Rot([c.sb([128, 256], BF16, "yh") for _ in range(3)])
            dn = Rot([c.sb([128, 2], F32, "dn") for _ in range(2)])
            Cf = c.sb([128, 257], F32, "Cf"); Cb = c.sb([128, 257], BF16, "Cb")
            PP = Rot([c.ps([128, 512], F32, "pp") for _ in range(3)])
            PW = Rot([c.ps([128, 512], F32, "pw") for _ in range(1)])
            PN = Rot([c.ps([128, 512], F32, "pn") for _ in range(2)])
            PC = c.ps([128, 512], F32, "pc")
            PTr = c.ps([128, 8, 128], BF16, "ptr")
            for v_ in Va.items:
                c.op("dve", lambda e: e.memset(v_.t[:, 256:257], 1.0), [], [v_])

            def load_head(h):
                b = h % 2
                load_w(wq[b], wq[b].t[:], win[:, h * 128:(h + 1) * 128])
                load_w(wk[b], wk[b].t[:], win[:, 512 + h * 128:512 + (h + 1) * 128])
                load_w(wv[b], wv[b].t[:], win[:, 1024 + h * 256:1024 + (h + 1) * 256])
                load_w(wo_[b], wo_[b].t[:], win[:, 2048 + h * 256:2048 + (h + 1) * 256])

            import os
            load_head(0)
            for h in range(int(os.environ.get('ML_HEADS', '4'))):
                b = h % 2
                if h + 1 < 4:
                    load_head(h + 1)
                Esel = c4.t[:, h * 128:(h + 1) * 128]
                def mS1(cc):
                    q_, j_ = divmod(cc, 4)
                    sl = slice(cc * 128, (cc + 1) * 128)
                    hsl = lambda k: HT[q_].t[:, k, j_ * 128:(j_ + 1) * 128]
                    pb = PP.next()
                    for k in range(8):
                        c.op("pe", lambda e: e.matmul(pb.t[:, 0:128], lhsT=wq[b].t[:, k, :], rhs=hsl(k), start=(k == 0), stop=(k == 7)),
                             [wq[b], HT[q_]], [pb])
                    for k in range(8):
                        c.op("pe", lambda e: e.matmul(pb.t[:, 128:256], lhsT=wk[b].t[:, k, :], rhs=hsl(k), start=(k == 0), stop=(k == 7),
                                                      skip_group_check=True), [wk[b], HT[q_]], [pb])
                    qc = qTc.next(); kc = kTc.next()
                    c.op("act", lambda e: e.activation(out=qc.t[:], in_=pb.t[:, 0:128], func=AF.Copy, scale=128.0 ** -0.5), [pb], [qc])
                    c.op("dve", lambda e: e.tensor_copy(out=kc.t[:], in_=pb.t[:, 128:256]), [pb], [kc])
                    pb = PP.next()
                    for k in range(8):
                        c.op("pe", lambda e: e.matmul(pb.t[:, 0:128], lhsT=hsl(k), rhs=wk[b].t[:, k, :], start=(k == 0), stop=(k == 7)),
                             [wk[b], HT[q_]], [pb])
                    for k in range(8):
                        c.op("pe", lambda e: e.matmul(pb.t[:, 128:384], lhsT=hsl(k), rhs=wv[b].t[:, k, :], start=(k == 0), stop=(k == 7),
                                                      skip_group_check=True), [wv[b], HT[q_]], [pb])
                    kw_ = kw.next(); va = Va.next()
                    c.op("act", lambda e: e.activation(out=kw_.t[:], in_=pb.t[:, 0:128], func=AF.Copy, scale=cols.t[:, cc, 8 + h:9 + h]),
                         [pb, cols], [kw_])
                    c.op("dve", lambda e: e.tensor_copy(out=va.t[:, 0:256], in_=pb.t[:, 128:384]), [pb], [va])
                    pw = PW.next()
                    c.op("pe", lambda e: e.matmul(pw.t[:, 0:128], lhsT=aT.t[:, sl], rhs=Esel, start=True, stop=False), [aT, c4], [pw])
                    c.op("pe", lambda e: e.matmul(pw.t[:, 0:128], lhsT=Esel, rhs=nG.t[:, sl], start=False, stop=True), [nG, c4], [pw])
                    c.op("pe", lambda e: e.matmul(pw.t[:, 128:256], lhsT=kc.t[:], rhs=qc.t[:], start=True, stop=True, skip_group_check=True),
                         [kc, qc], [pw])
                    w_ = Wt.next(); pt = PTt.next()
                    c.op("act", lambda e: e.activation(out=w_.t[:], in_=pw.t[:, 0:128], func=AF.Exp), [pw], [w_])
                    c.op("dve", lambda e: e.tensor_tensor(out=w_.t[:], in0=w_.t[:], in1=maskLE, op=ALU.mult), [w_, CON], [w_])
                    c.op("dve", lambda e: e.tensor_tensor(out=pt.t[:], in0=pw.t[:, 128:256], in1=w_.t[:], op=ALU.mult), [pw, w_], [pt])
                    pb = PP.next()
                    for k in range(8):
                        c.op("pe", lambda e: e.matmul(pb.t[:, 0:256], lhsT=hsl(k), rhs=wo_[b].t[:, k, :], start=(k == 0), stop=(k == 7)),
                             [wo_[b], HT[q_]], [pb])
                    s_ = sg.next()
                    c.op("act", lambda e: e.activation(out=s_.t[:], in_=pb.t[:, 0:256], func=AF.Exp, scale=-1.0), [pb], [s_])
                    c.op("pool", lambda e: e.tensor_scalar(out=s_.t[:], in0=s_.t[:], scalar1=1.0, scalar2=None, op0=ALU.add), [s_], [s_])
                    c.op("dve", lambda e: e.reciprocal(out=s_.t[:], in_=s_.t[:]), [s_], [s_])
                    return dict(qc=qc, kw=kw_, va=va, pt=pt, s=s_)

                def mS2(cc, d, prev_y):
                    sl = slice(cc * 128, (cc + 1) * 128)
                    qc, kw_, va, pt, s_ = d["qc"], d["kw"], d["va"], d["pt"], d["s"]
                    if cc > 0:
                        pi = PN.next()
                        c.op("pe", lambda e: e.matmul(pi.t[:, 0:257], lhsT=qc.t[:], rhs=Cb.t[:], start=True, stop=True), [qc, Cb], [pi])
                    c.op("pe", lambda e: e.matmul(PC.t[:, 0:257], lhsT=kw_.t[:], rhs=va.t[:], start=True, stop=True), [kw_, va], [PC])
                    pn = PN.next()
                    c.op("pe", lambda e: e.matmul(pn.t[:, 0:257], lhsT=pt.t[:], rhs=va.t[:], start=True, stop=True), [pt, va], [pn])
                    if prev_y is not None:
                        pcc, py_ = prev_y
                        for j2 in range(2):
                            c.op("pe", lambda e: e.transpose(out=PTr.t[:, j2, :], in_=py_.t[:, j2 * 128:(j2 + 1) * 128], identity=ident), [py_, CON], [PTr])
                        for j2 in range(2):
                            c.op("act", lambda e: e.activation(out=YT[2 * h + j2].t[:, pcc * 128:(pcc + 1) * 128], in_=PTr.t[:, j2, :], func=AF.Copy),
                                 [PTr], [YT[2 * h + j2]])
                    if cc > 0:
                        c.op("dve", lambda e: e.scalar_tensor_tensor(out=Cf.t[:], in0=Cf.t[:], scalar=cols.t[:, cc, 12 + h:13 + h], in1=PC.t[:, 0:257],
                                                                     op0=ALU.mult, op1=ALU.add), [Cf, cols, PC], [Cf])
                    else:
                        c.op("dve", lambda e: e.tensor_copy(out=Cf.t[:], in_=PC.t[:, 0:257]), [PC], [Cf])
                    to_ = tot.next()
                    if cc > 0:
                        ti = tI.next()
                        c.op("act", lambda e: e.activation(out=ti.t[:], in_=pi.t[:, 0:257], func=AF.Copy, scale=cols.t[:, cc, h:h + 1]),
                             [pi, cols], [ti])
                    c.op("act", lambda e: e.activation(out=Cb.t[:], in_=Cf.t[:], func=AF.Copy), [Cf], [Cb])
                    if cc > 0:
                        c.op("dve", lambda e: e.tensor_tensor(out=to_.t[:], in0=ti.t[:], in1=pn.t[:, 0:257], op=ALU.add), [ti, pn], [to_])
                    else:
                        c.op("dve", lambda e: e.tensor_copy(out=to_.t[:], in_=pn.t[:, 0:257]), [pn], [to_])
                    d_ = dn.next()
                    c.op("dve", lambda e: e.tensor_scalar(out=d_.t[:, 0:1], in0=to_.t[:, 256:257], scalar1=-1.0, scalar2=None, op0=ALU.mult), [to_], [d_])
                    c.op("dve", lambda e: e.tensor_tensor(out=d_.t[:, 0:1], in0=d_.t[:, 0:1], in1=to_.t[:, 256:257], op=ALU.max), [to_, d_], [d_])
                    c.op("dve", lambda e: e.tensor_tensor(out=d_.t[:, 0:1], in0=d_.t[:, 0:1], in1=cols.t[:, cc, 4 + h:5 + h], op=ALU.max), [cols, d_], [d_])
                    c.op("dve", lambda e: e.reciprocal(out=d_.t[:, 1:2], in_=d_.t[:, 0:1]), [d_], [d_])
                    y_ = yh.next()
                    c.op("dve", lambda e: e.scalar_tensor_tensor(out=y_.t[:], in0=to_.t[:, 0:256], scalar=d_.t[:, 1:2], in1=s_.t[:],
                                                                 op0=ALU.mult, op1=ALU.mult), [to_, d_, s_], [y_])
                    return (cc, y_)

                def mFlush(prev_y):
                    pcc, py_ = prev_y
                    for j2 in range(2):
                        c.op("pe", lambda e: e.transpose(out=PTr.t[:, j2, :], in_=py_.t[:, j2 * 128:(j2 + 1) * 128], identity=ident), [py_, CON], [PTr])
                    for j2 in range(2):
                        c.op("act", lambda e: e.activation(out=YT[2 * h + j2].t[:, pcc * 128:(pcc + 1) * 128], in_=PTr.t[:, j2, :], func=AF.Copy),
                             [PTr], [YT[2 * h + j2]])

                dd = {}; prev_y = None
                for n in range(16 + 2):
                    if n < 16:
                        dd[n] = mS1(n)
                    if 0 <= n - 2 < 16:
                        prev_y = mS2(n - 2, dd.pop(n - 2), prev_y)
                mFlush(prev_y)
            c.pop()
            return YT

        def mixer_gla(win):
            c.push()
            YT = [c.sb([128, S], BF16, f"YT{k}") for k in range(8)]
            c.push()
            wa = wslot([128, 8, 16], "w8")
            wa2 = wslot([16, 512], "w9")
            load_w(wa, wa.t[:], win[:, 3072:3088])
            c.dma("pool", wa2.t[:], Dm["gla_wa2"][:, :], writes=[wa2], key="w9")
            rm = wslot([128, S], "w10")
            c.dma("pool", rm.t[:], Dm["rmask"][:, :], writes=[rm], key="w10")
            nba = c.sb([128, 4], F32, "nba")
            c.dma("sp", nba.t[:], Dm["gla_ba"][:, :], writes=[nba], key="bi")
            c.op("dve", lambda e: e.tensor_scalar(out=nba.t[:], in0=nba.t[:], scalar1=-1.0, scalar2=None, op0=ALU.mult), [nba], [nba])
            gB = c.sb([128, 256], F32, "gB256")
            c.dma("sp", gB.t[:], Dm["gla_norm_g"][0:1, :].partition_broadcast(128), writes=[gB], key="bf")
            eps = c.sb([128, 1], F32, "geps")
            c.op("dve", lambda e: e.memset(eps.t[:], 1e-6), [], [eps])
            alT = c.sb([16, S], BF16, "alT")
            wq = wslot([128, 8, 128], "w0"); wk = wslot([128, 8, 128], "w1")
            wv = wslot([128, 8, 256], "w2"); wr = wslot([128, 8, 256], "w3")
            sp_ = c.sb([128, S], F32, "sp"); bp = c.sb([128, S], F32, "bp")
            qtl = c.sb([128, S], BF16, "qtl"); ktl = c.sb([128, S], BF16, "ktl")
            tE = Rot([c.sb([128, 512], F32, "tE") for _ in range(2)])
            ebl = c.sb([128, 16], F32, "ebl")
            Vc = Rot([c.sb([128, 256], BF16, "Vc") for _ in range(4)])
            AT = Rot([c.sb([128, 128], BF16, "AT") for _ in range(4)])
            khT = Rot([c.sb([128, 128], BF16, "khT") for _ in range(2)])
            kh = Rot([c.sb([128, 128], BF16, "kh") for _ in range(4)])
            NEG1 = c.sb([128, 256], F32, "NEG1")
            c.op("pool", lambda e: e.memset(NEG1.t[:], -1.0), [], [NEG1])
            junk = c.sb([128, 256], F32, "junk")
            sm = Rot([c.sb([128, 4], F32, "gsm") for _ in range(2)])
            er = Rot([c.sb([128, 256], F32, "er") for _ in range(4)])
            yh = Rot([c.sb([128, 256], BF16, "yh") for _ in range(3)])
            Sf = c.sb([128, 256], F32, "Sf"); Sb = c.sb([128, 256], BF16, "Sb")
            PP = Rot([c.ps([128, 512], F32, "pp") for _ in range(2)])
            PS_ = Rot([c.ps([128, 512], F32, "pss") for _ in range(1)])
            PO = Rot([c.ps([128, 512], F32, "pgo") for _ in range(2)])
            PC = c.ps([128, 512], F32, "pc")
            PTr = c.ps([128, 8, 128], BF16, "ptr")
            PT2 = c.ps([128, 8, 128], BF16, "pt2")
            for tq in range(4):
                pb = PP.next()
                proj_fm(pb, wa, lambda k: wa.t[:, k, :], tq)
                c.op("dve", lambda e: e.tensor_copy(out=alT.t[:, tq * 512:(tq + 1) * 512], in_=pb.t[0:16, :]), [pb], [alT])
            for h in range(4):
                load_w(wq, wq.t[:], win[:, h * 128:(h + 1) * 128])
                load_w(wk, wk.t[:], win[:, 512 + h * 128:512 + (h + 1) * 128])
                load_w(wv, wv.t[:], win[:, 1024 + h * 256:1024 + (h + 1) * 256])
                load_w(wr, wr.t[:], win[:, 2048 + h * 256:2048 + (h + 1) * 256])
                for tq in range(4):
                    ts_ = slice(tq * 512, (tq + 1) * 512)
                    pb = PP.next()
                    c.op("pe", lambda e: e.matmul(pb.t[:], lhsT=wa2.t[:, h * 128:(h + 1) * 128], rhs=alT.t[:, ts_], start=True, stop=True),
                         [wa2, alT], [pb])
                    te = tE.next()
                    c.op("act", lambda e: e.activation(out=te.t[:], in_=pb.t[:], func=AF.Exp, bias=nba.t[:, h:h + 1], scale=-1.0), [pb, nba], [te])
                    c.op("act", lambda e: e.activation(out=sp_.t[:, ts_], in_=te.t[:], func=AF.Ln, bias=1.0), [te], [sp_])
                c.op("dve", lambda e: e.tensor_tensor_scan(out=bp.t[:], data0=rm.t[:], data1=sp_.t[:], initial=0.0, op0=ALU.mult, op1=ALU.add),
                     [rm, sp_], [bp])
                c.op("act", lambda e: e.activation(out=ebl.t[:], in_=bp.t[:].rearrange("p (c t) -> p c t", t=128)[:, :, 127], func=AF.Exp,
                                                   scale=-1.0 / 16), [bp], [ebl])
                for tq in range(4):
                    ts_ = slice(tq * 512, (tq + 1) * 512)
                    pb = PP.next()
                    proj_fm(pb, wq, lambda k: wq.t[:, k, :], tq)
                    te = tE.next()
                    c.op("act", lambda e: e.activation(out=te.t[:], in_=bp.t[:, ts_], func=AF.Exp, scale=-1.0 / 16), [bp], [te])
                    c.op("dve", lambda e: e.scalar_tensor_tensor(out=qtl.t[:, ts_], in0=pb.t[:], scalar=128.0 ** -0.5, in1=te.t[:],
                                                                 op0=ALU.mult, op1=ALU.mult), [pb, te], [qtl])
                    pb = PP.next()
                    proj_fm(pb, wk, lambda k: wk.t[:, k, :], tq)
                    te = tE.next()
                    c.op("act", lambda e: e.activation(out=te.t[:], in_=bp.t[:, ts_], func=AF.Exp, scale=1.0 / 16), [bp], [te])
                    c.op("dve", lambda e: e.tensor_tensor(out=ktl.t[:, ts_], in0=pb.t[:], in1=te.t[:], op=ALU.mult), [pb, te], [ktl])
                def gS1(cc):
                    q_, j_ = divmod(cc, 4)
                    sl = slice(cc * 128, (cc + 1) * 128)
                    hsl = lambda k: HT[q_].t[:, k, j_ * 128:(j_ + 1) * 128]
                    pb = PP.next()
                    for k in range(8):
                        c.op("pe", lambda e: e.matmul(pb.t[:, 0:256], lhsT=hsl(k), rhs=wv.t[:, k, :], start=(k == 0), stop=(k == 7)),
                             [wv, HT[q_]], [pb])
                    vc = Vc.next()
                    c.op("act", lambda e: e.activation(out=vc.t[:], in_=pb.t[:, 0:256], func=AF.Copy), [pb], [vc])
                    ps = PS_.next()
                    c.op("pe", lambda e: e.matmul(ps.t[:, 0:128], lhsT=ktl.t[:, sl], rhs=qtl.t[:, sl], start=True, stop=True), [ktl, qtl], [ps])
                    at = AT.next()
                    c.op("dve", lambda e: e.tensor_tensor(out=at.t[:], in0=ps.t[:, 0:128], in1=maskLE, op=ALU.mult), [ps, CON], [at])
                    pr = PP.next()
                    for k in range(8):
                        c.op("pe", lambda e: e.matmul(pr.t[:, 0:256], lhsT=hsl(k), rhs=wr.t[:, k, :], start=(k == 0), stop=(k == 7)),
                             [wr, HT[q_]], [pr])
                    e_ = er.next()
                    c.op("act", lambda e: e.activation(out=e_.t[:], in_=pr.t[:, 0:256], func=AF.Exp, scale=-1.0), [pr], [e_])
                    c.op("pool", lambda e: e.tensor_scalar(out=e_.t[:], in0=e_.t[:], scalar1=1.0, scalar2=None, op0=ALU.add), [e_], [e_])
                    c.op("dve", lambda e: e.reciprocal(out=e_.t[:], in_=e_.t[:]), [e_], [e_])
                    c.op("dve", lambda e: e.tensor_tensor(out=e_.t[:], in0=pr.t[:, 0:256], in1=e_.t[:], op=ALU.mult), [pr, e_], [e_])
                    c.op("pool", lambda e: e.tensor_tensor(out=e_.t[:], in0=e_.t[:], in1=gB.t[:], op=ALU.mult), [e_, gB], [e_])
                    kt_ = khT.next(); k_ = kh.next()
                    c.op("dve", lambda e: e.tensor_scalar(out=kt_.t[:], in0=ktl.t[:, sl], scalar1=ebl.t[:, cc:cc + 1], scalar2=None, op0=ALU.mult),
                         [ktl, ebl], [kt_])
                    c.op("pe", lambda e: e.transpose(out=PT2.t[:, 0, :], in_=kt_.t[:], identity=ident), [kt_, CON], [PT2])
                    c.op("act", lambda e: e.activation(out=k_.t[:], in_=PT2.t[:, 0, :], func=AF.Copy), [PT2], [k_])
                    return dict(vc=vc, at=at, e=e_, k=k_)

                def gFlush(prev_y):
                    pcc, py_ = prev_y
                    for j2 in range(2):
                        c.op("pe", lambda e: e.transpose(out=PTr.t[:, j2, :], in_=py_.t[:, j2 * 128:(j2 + 1) * 128], identity=ident), [py_, CON], [PTr])
                    for j2 in range(2):
                        c.op("act", lambda e: e.activation(out=YT[2 * h + j2].t[:, pcc * 128:(pcc + 1) * 128], in_=PTr.t[:, j2, :], func=AF.Copy),
                             [PTr], [YT[2 * h + j2]])

                def gS2(cc, d, prev_y):
                    sl = slice(cc * 128, (cc + 1) * 128)
                    vc, at, e_, k_ = d["vc"], d["at"], d["e"], d["k"]
                    po = PO.next()
                    if cc > 0:
                        c.op("pe", lambda e: e.matmul(po.t[:, 0:256], lhsT=qtl.t[:, sl], rhs=Sb.t[:], start=True, stop=False), [qtl, Sb], [po])
                    c.op("pe", lambda e: e.matmul(PC.t[:, 0:256], lhsT=k_.t[:], rhs=vc.t[:], start=True, stop=True), [k_, vc], [PC])
                    c.op("pe", lambda e: e.matmul(po.t[:, 0:256], lhsT=at.t[:], rhs=vc.t[:], start=(cc == 0), stop=True), [at, vc], [po])
                    if prev_y is not None:
                        gFlush(prev_y)
                    if cc > 0:
                        c.op("dve", lambda e: e.scalar_tensor_tensor(out=Sf.t[:], in0=Sf.t[:], scalar=ebl.t[:, cc:cc + 1], in1=PC.t[:, 0:256],
                                                                     op0=ALU.mult, op1=ALU.add), [Sf, ebl, PC], [Sf])
                    else:
                        c.op("dve", lambda e: e.tensor_copy(out=Sf.t[:], in_=PC.t[:, 0:256]), [PC], [Sf])
                    m_ = sm.next()
                    c.op("act", lambda e: e.activation(out=junk.t[:], in_=po.t[:, 0:256], func=AF.Square, accum_out=m_.t[:, 0:1]), [po], [junk, m_])
                    c.op("act", lambda e: e.activation(out=Sb.t[:], in_=Sf.t[:], func=AF.Copy), [Sf], [Sb])
                    c.op("act", lambda e: e.activation(out=m_.t[:, 1:2], in_=m_.t[:, 0:1], func=AF.Ln, bias=eps.t[:, 0:1], scale=1.0 / 256),
                         [m_, eps], [m_])
                    c.op("act", lambda e: e.activation(out=m_.t[:, 2:3], in_=m_.t[:, 1:2], func=AF.Exp, scale=-0.5), [m_], [m_])
                    y_ = yh.next()
                    c.op("dve", lambda e: e.scalar_tensor_tensor(out=y_.t[:], in0=po.t[:, 0:256], scalar=m_.t[:, 2:3], in1=e_.t[:],
                                                                 op0=ALU.mult, op1=ALU.mult), [po, m_, e_], [y_])
                    return (cc, y_)

                dd = {}; prev_y = None
                for n in range(16 + 2):
                    if n < 16:
                        dd[n] = gS1(n)
                    if 0 <= n - 2 < 16:
                        prev_y = gS2(n - 2, dd.pop(n - 2), prev_y)
                gFlush(prev_y)
            c.pop()
            return YT

        def xattn(layer):
            c.push()
            OT = [c.sb([128, S], BF16, f"OT{k}") for k in range(8)]
            c.push()
            wq = [wslot([128, 8, 256], f"w{j}") for j in range(2)]
            wk = [wslot([128, 8, 256], f"w{2 + j}") for j in range(2)]
            wv = [wslot([128, 8, 256], f"w{4 + j}") for j in range(2)]
            qTs = [c.sb([128, 2, S], BF16, "xqT") for _ in range(2)]
            kTs = [c.sb([128, 2, 256], BF16, "xkT") for _ in range(2)]
            Vas = [c.sb([128, 2, 257], BF16, "xV") for _ in range(2)]
            PT = [Rot([c.sb([128, 512], BF16, "xPT") for _ in range(2)]) for _ in range(2)]
            Ot = Rot([c.sb([128, 256], BF16, "xO") for _ in range(3)])
            rd = Rot([c.sb([128, 1], F32, "xrd") for _ in range(3)])
            PP = Rot([c.ps([128, 512], F32, "pp") for _ in range(2)])
            PS_ = Rot([c.ps([128, 512], F32, "psc") for _ in range(2)])
            PO = Rot([c.ps([128, 512], F32, "pxo") for _ in range(2)])
            PTr = Rot([c.ps([128, 8, 128], BF16, "ptr") for _ in range(2)])
            for Va_ in Vas:
                c.op("dve", lambda e: e.memset(Va_.t[:, :, 256:257], 1.0), [], [Va_])
            wkv = Dm["xa_wkv"][layer]

            def load_head(h):
                b = h % 2
                load_w(wq[b], wq[b].t[:], Dm["xa_wq"][layer][:, h * 256:(h + 1) * 256])
                load_w(wk[b], wk[b].t[:], wkv[:, h * 256:(h + 1) * 256])
                load_w(wv[b], wv[b].t[:], wkv[:, 1024 + h * 256:1024 + (h + 1) * 256])

            def xproj_items(h):
                bb = h % 2
                items = []
                for dc in range(2):
                    def fk(dc=dc):
                        pb = PP.next()
                        for k in range(8):
                            c.op("pe", lambda e: e.matmul(pb.t[:, 0:256], lhsT=wk[bb].t[:, k, dc * 128:(dc + 1) * 128], rhs=memT.t[:, k, :],
                                                          start=(k == 0), stop=(k == 7)), [wk[bb], memT], [pb])
                        c.op("dve", lambda e: e.tensor_copy(out=kTs[bb].t[:, dc, :], in_=pb.t[:, 0:256]), [pb], [kTs[bb]])
                    items.append(fk)
                for mt in range(2):
                    def fv(mt=mt):
                        pb = PP.next()
                        for k in range(8):
                            c.op("pe", lambda e: e.matmul(pb.t[:, 0:256], lhsT=memT.t[:, k, mt * 128:(mt + 1) * 128], rhs=wv[bb].t[:, k, :],
                                                          start=(k == 0), stop=(k == 7)), [wv[bb], memT], [pb])
                        c.op("dve", lambda e: e.tensor_copy(out=Vas[bb].t[:, mt, 0:256], in_=pb.t[:, 0:256]), [pb], [Vas[bb]])
                    items.append(fv)
                for dc in range(2):
                    for tq in range(4):
                        def fq(dc=dc, tq=tq):
                            pb = PP.next()
                            proj_fm(pb, wq[bb], lambda k: wq[bb].t[:, k, dc * 128:(dc + 1) * 128], tq)
                            c.op("act", lambda e: e.activation(out=qTs[bb].t[:, dc, tq * 512:(tq + 1) * 512], in_=pb.t[:], func=AF.Copy, scale=1.0 / 16),
                                 [pb], [qTs[bb]])
                        items.append(fq)
                return items

            load_head(0)
            load_head(1)
            for it in xproj_items(0):
                it()
            for h in range(4):
                b = h % 2
                if 1 <= h and h + 1 < 4:
                    load_head(h + 1)
                nxt = xproj_items(h + 1) if h + 1 < 4 else []
                qT, kT, Va = qTs[b], kTs[b], Vas[b]
                def xP(tq):
                    pts = []
                    for mt in range(2):
                        psc = PS_.next()
                        for dc in range(2):
                            c.op("pe", lambda e: e.matmul(psc.t[:], lhsT=kT.t[:, dc, mt * 128:(mt + 1) * 128],
                                                          rhs=qT.t[:, dc, tq * 512:(tq + 1) * 512], start=(dc == 0), stop=(dc == 1)),
                                 [kT, qT], [psc])
                        pt = PT[mt].next()
                        c.op("act", lambda e: e.activation(out=pt.t[:], in_=psc.t[:], func=AF.Exp), [psc], [pt])
                        pts.append(pt)
                    return pts

                def xV(i, pts):
                    jt = i % 4
                    po = PO.next()
                    for mt in range(2):
                        c.op("pe", lambda e: e.matmul(po.t[:, 0:257], lhsT=pts[mt].t[:, jt * 128:(jt + 1) * 128], rhs=Va.t[:, mt, :],
                                                      start=(mt == 0), stop=(mt == 1)), [pts[mt], Va], [po])
                    r_ = rd.next(); o_ = Ot.next()
                    c.op("dve", lambda e: e.reciprocal(out=r_.t[:], in_=po.t[:, 256:257]), [po], [r_])
                    c.op("act", lambda e: e.activation(out=o_.t[:], in_=po.t[:, 0:256], func=AF.Copy, scale=r_.t[:, 0:1]), [po, r_], [o_])
                    return o_

                def xT(i, o_):
                    ptr = PTr.next()
                    for j2 in range(2):
                        c.op("pe", lambda e: e.transpose(out=ptr.t[:, j2, :], in_=o_.t[:, j2 * 128:(j2 + 1) * 128], identity=ident),
                             [o_, CON], [ptr])
                    for j2 in range(2):
                        c.op("dve", lambda e: e.tensor_copy(out=OT[2 * h + j2].t[:, i * 128:(i + 1) * 128], in_=ptr.t[:, j2, :]),
                             [ptr], [OT[2 * h + j2]])

                ptsl = {0: xP(0)}
                prev = None
                for tq in range(4):
                    if tq + 1 < 4:
                        ptsl[tq + 1] = xP(tq + 1)
                    for jt in range(4):
                        i = tq * 4 + jt
                        o_ = xV(i, ptsl[tq])
                        if prev is not None:
                            xT(*prev)
                        prev = (i, o_)
                        if i >= 2 and nxt:
                            nxt.pop(0)()
                xT(*prev)
                for it in nxt:
                    it()
            c.pop()
            out_proj_ln(OT, Dm["xa_wo"][layer], layer * 3 + 1)
            c.pop()

        def ffn(layer):
            c.push()
            groups = [list(range(g, min(g + 4, NCH))) for g in range(0, NCH, 4)]
            wup = Dm["ffn_up"][layer]
            wdn = Dm["ffn_down"][layer]
            cw = c.sb([128, 44 * 3], F32, "cw"); cb = c.sb([128, 44], F32, "cb")
            c.dma("sp", cw.t[:], Dm["ffn_conv"][layer], writes=[cw], key="cw")
            c.dma("sp", cb.t[:], Dm["ffn_conv_b"][layer], writes=[cb], key="cb")
            act = [c.sb([128, S], BF16, f"act{j}") for j in range(4)]
            wd = [wslot([128, 4, D], f"w{j}") for j in range(2)]
            wg = [wslot([128, 8, 128], f"w{2 + j}") for j in range(2)]
            wv = [wslot([128, 8, 128], f"w{4 + j}") for j in range(2)]
            ug = c.sb([128, S + 2], F32, "ug"); uv = c.sb([128, S + 2], F32, "uv")
            cg = c.sb([128, S], F32, "cg"); cv = c.sb([128, S], F32, "cv")
            gg = c.sb([128, S], F32, "gg")
            PU = Rot([c.ps([128, 512], F32, "pu") for _ in range(4)])
            PD = Rot([c.ps([128, 512], F32, "pd") for _ in range(4)])
            c.op("dve", lambda e: e.memset(ug.t[:, 0:2], 0.0), [], [ug])
            c.op("dve", lambda e: e.memset(uv.t[:, 0:2], 0.0), [], [uv])

            def load_up(j):
                b = j % 2
                load_w(wg[b], wg[b].t[:], wup[:, j * 128:(j + 1) * 128])
                load_w(wv[b], wv[b].t[:], wup[:, DFF + j * 128:DFF + (j + 1) * 128])

            def conv_part(u, w, ch, dst):
                c.op("dve", lambda e: e.scalar_tensor_tensor(out=dst.t[:], in0=u.t[:, 1:S + 1], scalar=cw.t[:, ch * 3 + 1:ch * 3 + 2],
                                                             in1=dst.t[:], op0=ALU.mult, op1=ALU.add), [u, cw, dst], [dst])
                c.op("dve", lambda e: e.scalar_tensor_tensor(out=dst.t[:], in0=u.t[:, 0:S], scalar=cw.t[:, ch * 3:ch * 3 + 1],
                                                             in1=dst.t[:], op0=ALU.mult, op1=ALU.add), [u, cw, dst], [dst])

            load_up(0)
            for gi, grp in enumerate(groups):
                wdb = wd[gi % 2]
                load_w(wdb, wdb.t[:, 0:len(grp), :], wdn[grp[0] * 128:(grp[-1] + 1) * 128, :])
                for jj, j in enumerate(grp):
                    b = j % 2
                    if j + 1 < NCH:
                        load_up(j + 1)
                    for (w_, u_, cdst, ch) in ((wg[b], ug, cg, j), (wv[b], uv, cv, NCH + j)):
                        for tq in range(4):
                            pb = PU.next()
                            proj_fm(pb, w_, lambda k: w_.t[:, k, :], tq)
                            c.op("act", lambda e: e.activation(out=u_.t[:, 2 + tq * 512:2 + (tq + 1) * 512], in_=pb.t[:], func=AF.Copy),
                                 [pb], [u_])
                            c.op("act", lambda e: e.activation(out=cdst.t[:, tq * 512:(tq + 1) * 512], in_=pb.t[:], func=AF.Identity,
                                                               bias=cb.t[:, ch:ch + 1], scale=cw.t[:, ch * 3 + 2:ch * 3 + 3]),
                                 [pb, cb, cw], [cdst])
                        conv_part(u_, w_, ch, cdst)
                    c.op("act", lambda e: e.activation(out=gg.t[:], in_=cg.t[:], func=AF.Gelu_apprx_tanh), [cg], [gg])
                    c.op("pool", lambda e: e.tensor_tensor(out=act[jj].t[:], in0=gg.t[:], in1=cv.t[:], op=ALU.mult), [gg, cv], [act[jj]])
                for i in range(NT):
                    for half in range(2):
                        pd = PD.next()
                        for jj in range(len(grp)):
                            c.op("pe", lambda e: e.matmul(pd.t[:], lhsT=act[jj].t[:, i * 128:(i + 1) * 128],
                                                          rhs=wdb.t[:, jj, half * 512:(half + 1) * 512],
                                                          start=(jj == 0), stop=(jj == len(grp) - 1)), [act[jj], wdb], [pd])
                        hs = H[i].t[:, half * 512:(half + 1) * 512]
                        if gi == 0:
                            c.op("dve", lambda e: e.scalar_tensor_tensor(out=hs, in0=hs, scalar=ALPHA, in1=pd.t[:], op0=ALU.mult, op1=ALU.add),
                                 [H[i], pd], [H[i]])
                        else:
                            c.op("dve", lambda e: e.tensor_tensor(out=hs, in0=hs, in1=pd.t[:], op=ALU.add), [H[i], pd], [H[i]])
            c.pop()
            c.push()
            R, gB, bB = ln_res(layer * 3 + 2)
            hbs = {}
            for n in range(NT + 1):
                if n < NT: hbs[n] = ln_tile(n, H[n], gB, bB, R)
                if 0 <= n - 1 < NT: ht_from_hb(n - 1, hbs[n - 1], R["pT"])
            c.pop()

        c.push()
        hbR = Rot([c.sb([128, D], BF16, "hb") for _ in range(2)])
        pTR = Rot([c.ps([128, 8, 128], BF16, "pT") for _ in range(2)])
        for i in range(NT):
            to_ht(i, hbR, pTR)
        mf = c.sb([128, D], F32, "mf")
        for mt in range(2):
            c.dma("sp", mf.t[:], Dm["mem"][mt * 128:(mt + 1) * 128, :], writes=[mf], key="mem")
            hb = hbR.next()
            c.op("act", lambda e: e.activation(out=hb.t[:], in_=mf.t[:], func=AF.Copy), [mf], [hb])
            pT = pTR.next()
            for k in range(8):
                c.op("pe", lambda e: e.transpose(out=pT.t[:, k, :], in_=hb.t[:, k * 128:(k + 1) * 128], identity=ident), [hb, CON], [pT])
            c.op("dve", lambda e: e.tensor_copy(out=memT.t[:, :, mt * 128:(mt + 1) * 128], in_=pT.t[:]), [pT], [memT])
        c.pop()

        def finish():
            for i in range(NT):
                c.dma("sp", out[i * 128:(i + 1) * 128, :], H[i].t[:], reads=[H[i]], key="out")
            c.barrier()

        done = False
        for layer in range(first, nlayers):
            kind = layer % 4
            if kind == 0:
                YT = mixer_sb(Dm["sb_win"])
            elif kind == 1:
                YT = mixer_fox(Dm["fox_win"], Dm["fox_bf"])
            elif kind == 2:
                YT = mixer_mlstm(Dm["ml_win"])
            else:
                YT = mixer_gla(Dm["gla_win"])
            out_proj_ln(YT, Dm["mix_wo"][layer], layer * 3 + 0)
            c.pop()
            if stop == (layer, "a"):
                break
            xattn(layer)
            if stop == (layer, "b"):
                break
            ffn(layer)
        finish()
    return nc


_NC_CACHE = {}


def prep_inputs(inputs):
    con, con4, rmask = make_consts()
    shared = {
        "ln_g": np.ascontiguousarray(inputs["ln_g"].reshape(12, D)),
        "ln_b": np.ascontiguousarray(inputs["ln_b"].reshape(12, D)),
        "mix_wo": inputs["mix_wo"], "sb_win": inputs["sb_win"][0], "fox_win": inputs["fox_win"][0],
        "fox_bf": inputs["fox_bf"].reshape(1, 16),
        "ml_win": inputs["ml_win"][0], "ml_bi": inputs["ml_bi"].reshape(4, 1), "ml_bf": inputs["ml_bf"].reshape(4, 1),
        "gla_win": inputs["gla_win"][0], "gla_wa2": inputs["gla_wa2"][0],
        "gla_ba": np.ascontiguousarray(inputs["gla_ba"].reshape(4, 128).T),
        "gla_norm_g": inputs["gla_norm_g"].reshape(1, 256),
        "xa_wq": inputs["xa_wq"], "xa_wkv": inputs["xa_wkv"], "xa_wo": inputs["xa_wo"],
        "ffn_up": inputs["ffn_up"],
        "ffn_conv": np.ascontiguousarray(inputs["ffn_conv"].reshape(4, 3, 44, 128).transpose(0, 3, 2, 1).reshape(4, 128, 132)),
        "ffn_conv_b": np.ascontiguousarray(inputs["ffn_conv_b"].reshape(4, 44, 128).transpose(0, 2, 1)),
        "ffn_down": inputs["ffn_down"],
        "con": con, "con4": con4, "rmask": rmask,
    }
    shared = {k: np.ascontiguousarray(v, dtype=np.float32) for k, v in shared.items()}
    return shared


def kernel(**inputs):
    inputs = {k: np.asarray(v) for k, v in inputs.items()}
    shared = prep_inputs(inputs)
    if "nc" not in _NC_CACHE:
        _NC_CACHE["nc"] = build()
    nc = _NC_CACHE["nc"]
    in_maps = []
    for b in range(8):
        m = dict(shared)
        m["x"] = np.ascontiguousarray(inputs["x"][b], dtype=np.float32)
        m["mem"] = np.ascontiguousarray(inputs["mem"][b], dtype=np.float32)
        in_maps.append(m)
    res = run_bass_kernel_spmd(nc, in_maps, core_ids=list(range(8)))
    return np.stack([np.asarray(r["out"], dtype=np.float32) for r in res.results], axis=0)
```

```python
import numpy as np
import concourse.bass as bass
import concourse.mybir as mybir
from concourse.bass_utils import run_bass_kernel_spmd
from contextlib import ExitStack

F32 = mybir.dt.float32
BF16 = mybir.dt.bfloat16
AF = mybir.ActivationFunctionType
ALU = mybir.AluOpType
AX = mybir.AxisListType

S = 2048
D = 1024
NT = 16
ALPHA = 8.0 ** 0.25
LN_EPS = 1e-5
DFF = 2816
NCH = 22


class Eng:
    def __init__(s, name, h, sem):
        s.name, s.h, s.sem, s.cnt, s.seen = name, h, sem, 0, {}


class DSem:
    def __init__(s, key, sem):
        s.key, s.sem, s.cnt = key, sem, 0


class TT:
    def __init__(s, t, name):
        s.t, s.name, s.w, s.r = t, name, None, {}


class Ctx:
    def __init__(s, nc, es):
        s.nc = nc
        s.stacks = [es]
        s.E = {}
        for name, h in (("pe", nc.tensor), ("act", nc.scalar), ("dve", nc.vector),
                        ("pool", nc.gpsimd), ("sp", nc.sync)):
            s.E[name] = Eng(name, h, es.enter_context(nc.semaphore("s_" + name)))
        s.dsems = {}
        s.nalloc = 0
        s.same_eng_sync = True
        for key in ["con", "x", "cf", "bf", "bi", "lng", "lnb", "mem", "cw", "cb", "out"] + [f"w{j}" for j in range(11)]:
            s.dsem(key)
        for E in s.E.values():
            nc.gpsimd.sem_clear(E.sem)
        for ds in s.dsems.values():
            nc.gpsimd.sem_clear(ds.sem)
        nc.all_engine_barrier()

    def push(s):
        st = ExitStack()
        st.__enter__()
        s.stacks.append(st)

    def pop(s):
        s.barrier()
        st = s.stacks.pop()
        st.__exit__(None, None, None)

    def sb(s, shape, dt, name="t"):
        s.nalloc += 1
        name = f"{name}_{s.nalloc}"
        return TT(s.stacks[-1].enter_context(s.nc.sbuf_tensor(name, list(shape), dt)), name)

    def ps(s, shape, dt, name="p"):
        s.nalloc += 1
        name = f"{name}_{s.nalloc}"
        t = TT(s.stacks[-1].enter_context(s.nc.psum_tensor(name, list(shape), dt)), name)
        t.psum = True
        return t

    def dsem(s, key):
        if key not in s.dsems:
            s.dsems[key] = DSem("dma_" + key, s.stacks[0].enter_context(s.nc.semaphore("d_" + key)))
        return s.dsems[key]

    def _wait(s, E, key, sem, val):
        if E.seen.get(key, 0) >= val:
            return
        E.h.wait_ge(sem, val)
        E.seen[key] = val

    def _waits(s, E, reads, writes):
        deps = []
        for t in reads:
            if t.w: deps.append(t.w)
            if getattr(t, "psum", False):
                deps.extend(t.r.values())
        for t in writes:
            if t.w: deps.append(t.w)
            deps.extend(t.r.values())
        for (key, sem, val, ds) in deps:
            if key == E.name and (E.name == "pe" or not s.same_eng_sync):
                continue
            if ds is not None:
                val = max(val, ds.cnt)
            s._wait(E, key, sem, val)

    def op(s, eng, fn, reads=(), writes=()):
        E = s.E[eng]
        s._waits(E, reads, writes)
        ins = fn(E.h)
        E.cnt += 1
        ins.then_inc(E.sem, 1)
        tok = (E.name, E.sem, E.cnt, None)
        for t in reads: t.r[E.name] = tok
        for t in writes:
            t.w = tok; t.r = {}
        return ins

    def dma(s, q, out, in_, reads=(), writes=(), key="g"):
        E = s.E[q]
        s._waits(E, reads, writes)
        ds = s.dsem(key)
        ins = E.h.dma_start(out=out, in_=in_)
        ds.cnt += 16
        ins.then_inc(ds.sem, 16)
        tok = (ds.key, ds.sem, ds.cnt, ds)
        for t in reads: t.r[ds.key] = tok
        for t in writes:
            t.w = tok; t.r = {}
        return ins

    def barrier(s):
        engs = list(s.E.values())
        for E in engs:
            for F in engs:
                if F.cnt > 0:
                    s._wait(E, F.name, F.sem, F.cnt)
            for ds in s.dsems.values():
                if ds.cnt > 0:
                    s._wait(E, ds.key, ds.sem, ds.cnt)


class Rot:
    def __init__(s, items):
        s.items, s.i = list(items), 0

    def next(s):
        x = s.items[s.i % len(s.items)]
        s.i += 1
        return x


C_ID, C_LE, C_LT, C_UN, C_GT, NCON = 0, 128, 256, 2304, 2432, 2560


def make_consts():
    con = np.zeros((128, NCON), np.float32)
    p = np.arange(128)[:, None]
    f = np.arange(128)[None, :]
    con[:, C_ID:C_ID + 128] = (p == f)
    con[:, C_LE:C_LE + 128] = (p <= f)
    t = np.arange(512)[None, :]
    for o in range(4):
        con[:, C_LT + 512 * o:C_LT + 512 * (o + 1)] = ((o * 128 + p) < t)
    con[:, C_UN:C_UN + 128] = -1.0 * (p >= f)
    con[:, C_GT:C_GT + 128] = (p > f)
    con4 = np.zeros((4, 4 * 128 + 4 + 128), np.float32)
    for h in range(4):
        con4[h, h * 128:(h + 1) * 128] = 1.0
        con4[h, 512 + h] = 1.0
    con4[:, 516:644] = 1.0
    rmask = np.ones((128, S), np.float32)
    rmask[:, ::128] = 0.0
    return con, con4, rmask


def build(nlayers=4, stop=None, first=0):
    nc = bass.Bass("TRN2", target_bir_lowering=False)
    Dm = {}

    def din(name, shape):
        Dm[name] = nc.dram_tensor(name, list(shape), F32, kind="ExternalInput").ap()

    din("x", [S, D]); din("mem", [256, D]); din("ln_g", [12, D]); din("ln_b", [12, D])
    din("mix_wo", [4, D, D]); din("sb_win", [D, 3072]); din("fox_win", [D, 3088]); din("fox_bf", [1, 16])
    din("ml_win", [D, 3080]); din("ml_bi", [4, 1]); din("ml_bf", [4, 1])
    din("gla_win", [D, 3088]); din("gla_wa2", [16, 512]); din("gla_ba", [128, 4]); din("gla_norm_g", [1, 256])
    din("xa_wq", [4, D, D]); din("xa_wkv", [4, D, 2 * D]); din("xa_wo", [4, D, D])
    din("ffn_up", [4, D, 2 * DFF]); din("ffn_conv", [4, 128, 44 * 3]); din("ffn_conv_b", [4, 128, 44])
    din("ffn_down", [4, DFF, D])
    din("con", [128, NCON]); din("con4", [4, 644]); din("rmask", [128, S])
    out = nc.dram_tensor("out", [S, D], F32, kind="ExternalOutput").ap()

    with ExitStack() as es:
        c = Ctx(nc, es)
        H = [c.sb([128, D], F32, f"H{i}") for i in range(NT)]
        HT = [c.sb([128, 8, 512], BF16, f"HT{q}") for q in range(4)]
        memT = c.sb([128, 8, 256], BF16, "memT")
        CON = c.sb([128, NCON], BF16, "CON")
        ONEN = c.sb([128, 128], BF16, "ONEN")
        ident = CON.t[:, C_ID:C_ID + 128]
        maskLE = CON.t[:, C_LE:C_LE + 128]
        uneg = CON.t[:, C_UN:C_UN + 128]

        def maskLT(o):
            return CON.t[:, C_LT + 512 * o:C_LT + 512 * (o + 1)]

        c.dma("pool", CON.t[:], Dm["con"][:, :], writes=[CON], key="con")
        c.op("dve", lambda e: e.memset(ONEN.t[:], -1.0), [], [ONEN])
        for i in range(NT):
            c.dma("sp", H[i].t[:], Dm["x"][i * 128:(i + 1) * 128, :], writes=[H[i]], key="x")

        wcount = [0]

        def load_w(slot, ap_dst, src):
            wcount[0] += 1
            c.dma("pool", ap_dst, src.rearrange("(kc p) n -> p kc n", p=128), writes=[slot], key=slot.key)

        def wslot(shape, key, name="w"):
            t = c.sb(shape, BF16, name)
            t.key = key
            return t

        def to_ht(i, hb_rot, pT_rot):
            hb = hb_rot.next()
            c.op("act", lambda e: e.activation(out=hb.t[:], in_=H[i].t[:], func=AF.Copy), [H[i]], [hb])
            pT = pT_rot.next()
            for k in range(8):
                c.op("pe", lambda e: e.transpose(out=pT.t[:, k, :], in_=hb.t[:, k * 128:(k + 1) * 128], identity=ident),
                     [hb, CON], [pT])
            q, j = divmod(i, 4)
            c.op("dve", lambda e: e.tensor_copy(out=HT[q].t[:, :, j * 128:(j + 1) * 128], in_=pT.t[:]), [pT], [HT[q]])

        def ln_tile(i, Sx, gB, bB, R):
            st = R["st"].next(); mv = R["mv"].next(); sm = R["sm"].next(); T1 = R["T1"].next()
            for hh in range(2):
                c.op("dve", lambda e: e.bn_stats(out=st.t[:, hh, :], in_=Sx.t[:, hh * 512:(hh + 1) * 512]), [Sx], [st])
            c.op("dve", lambda e: e.bn_aggr(out=mv.t[:], in_=st.t[:]), [st], [mv])
            c.op("act", lambda e: e.activation(out=sm.t[:, 0:1], in_=mv.t[:, 1:2], func=AF.Ln, bias=R["eps"].t[:, 0:1]), [mv, R["eps"]], [sm])
            c.op("act", lambda e: e.activation(out=sm.t[:, 1:2], in_=sm.t[:, 0:1], func=AF.Exp, scale=-0.5), [sm], [sm])
            c.op("dve", lambda e: e.tensor_scalar(out=sm.t[:, 2:3], in0=mv.t[:, 0:1], scalar1=sm.t[:, 1:2], scalar2=-1.0,
                                                  op0=ALU.mult, op1=ALU.mult), [mv, sm], [sm])
            c.op("act", lambda e: e.activation(out=T1.t[:], in_=Sx.t[:], func=AF.Identity, bias=sm.t[:, 2:3], scale=sm.t[:, 1:2]),
                 [Sx, sm], [T1])
            c.op("pool", lambda e: e.tensor_tensor(out=T1.t[:], in0=T1.t[:], in1=gB.t[:], op=ALU.mult), [T1, gB], [T1])
            c.op("pool", lambda e: e.tensor_tensor(out=H[i].t[:], in0=T1.t[:], in1=bB.t[:], op=ALU.add), [T1, bB], [H[i]])
            hb = R["hb"].next()
            c.op("act", lambda e: e.activation(out=hb.t[:], in_=H[i].t[:], func=AF.Copy), [H[i]], [hb])
            return hb

        def ht_from_hb(i, hb, pT_rot):
            pT = pT_rot.next()
            for k in range(8):
                c.op("pe", lambda e: e.transpose(out=pT.t[:, k, :], in_=hb.t[:, k * 128:(k + 1) * 128], identity=ident),
                     [hb, CON], [pT])
            q, j = divmod(i, 4)
            c.op("dve", lambda e: e.tensor_copy(out=HT[q].t[:, :, j * 128:(j + 1) * 128], in_=pT.t[:]), [pT], [HT[q]])

        def ln_res(lnidx):
            R = {}
            R["st"] = Rot([c.sb([128, 2, 6], F32, "st") for _ in range(2)])
            R["mv"] = Rot([c.sb([128, 2], F32, "mv") for _ in range(2)])
            R["sm"] = Rot([c.sb([128, 4], F32, "sm") for _ in range(2)])
            R["T1"] = Rot([c.sb([128, D], F32, "T1") for _ in range(2)])
            R["hb"] = Rot([c.sb([128, D], BF16, "hb") for _ in range(3)])
            R["pT"] = Rot([c.ps([128, 8, 128], BF16, "pT") for _ in range(2)])
            R["eps"] = c.sb([128, 1], F32, "eps")
            c.op("dve", lambda e: e.memset(R["eps"].t[:], LN_EPS), [], [R["eps"]])
            gB = c.sb([128, D], F32, "gB"); bB = c.sb([128, D], F32, "bB")
            c.dma("sp", gB.t[:], Dm["ln_g"][lnidx:lnidx + 1, :].partition_broadcast(128), writes=[gB], key="lng")
            c.dma("sp", bB.t[:], Dm["ln_b"][lnidx:lnidx + 1, :].partition_broadcast(128), writes=[bB], key="lnb")
            return R, gB, bB

        def out_proj_ln(KT, w_dram, lnidx):
            c.push()
            wo = [wslot([128, 8, 512], f"w{j}") for j in range(2)]
            for j in range(2):
                load_w(wo[j], wo[j].t[:], w_dram[:, j * 512:(j + 1) * 512])
            R, gB, bB = ln_res(lnidx)
            PB = Rot([c.ps([128, 512], F32, "po") for _ in range(4)])
            SX = Rot([c.sb([128, D], F32, "SX") for _ in range(3)])
            sxs = {}; hbs = {}

            def sA(i):
                Sx = SX.next(); sxs[i] = Sx
                for j in range(2):
                    pb = PB.next()
                    for k in range(8):
                        c.op("pe", lambda e: e.matmul(pb.t[:], lhsT=KT[k].t[:, i * 128:(i + 1) * 128], rhs=wo[j].t[:, k, :],
                                                      start=(k == 0), stop=(k == 7)), [KT[k], wo[j]], [pb])
                    c.op("dve", lambda e: e.scalar_tensor_tensor(out=Sx.t[:, j * 512:(j + 1) * 512], in0=H[i].t[:, j * 512:(j + 1) * 512],
                                                                 scalar=ALPHA, in1=pb.t[:], op0=ALU.mult, op1=ALU.add),
                         [H[i], pb], [Sx])

            for n in range(NT + 2):
                if n < NT: sA(n)
                if 0 <= n - 1 < NT: hbs[n - 1] = ln_tile(n - 1, sxs[n - 1], gB, bB, R)
                if 0 <= n - 2 < NT: ht_from_hb(n - 2, hbs[n - 2], R["pT"])
            c.pop()

        def proj_fm(pb, w, wap_fn, tq):
            for k in range(8):
                c.op("pe", lambda e: e.matmul(pb_ap(pb, wap_fn(k)), lhsT=wap_fn(k), rhs=HT[tq].t[:, k, :], start=(k == 0), stop=(k == 7)),
                     [w, HT[tq]], [pb])

        def pb_ap(pb, lhsT):
            m = lhsT.shape[-1]
            return pb.t[0:m, :]

        def proj_tm(pb, out_ap, w, wap_fn, i):
            q, j = divmod(i, 4)
            for k in range(8):
                c.op("pe", lambda e: e.matmul(out_ap, lhsT=HT[q].t[:, k, j * 128:(j + 1) * 128], rhs=wap_fn(k), start=(k == 0), stop=(k == 7)),
                     [w, HT[q]], [pb])

        def mixer_sb(win):
            c.push()
            YT = [c.sb([128, S], BF16, f"YT{k}") for k in range(8)]
            c.push()
            wq = [wslot([128, 8, 128], f"w{j}") for j in range(2)]
            wk = [wslot([128, 8, 128], f"w{2 + j}") for j in range(2)]
            wv = [wslot([128, 8, 128], f"w{4 + j}") for j in range(2)]
            qT = [c.sb([128, S], BF16, "qT") for _ in range(2)]
            kT = [c.sb([128, S], BF16, "kT") for _ in range(2)]
            V = [c.sb([128, NT, 128], BF16, "V") for _ in range(2)]
            Yp = c.sb([128, NT, 128], BF16, "Yp")
            Et = Rot([c.sb([128, 512], F32, "E") for _ in range(2)])
            SPt = Rot([c.sb([128, 512], F32, "SP") for _ in range(2)])
            LmP = Rot([c.sb([128, 512], BF16, "Lm") for _ in range(5)])
            Ssum = Rot([c.sb([128, 512], BF16, "Ss") for _ in range(5)])
            WT = Rot([c.sb([128, 512], BF16, "WT") for _ in range(4)])
            PZ = Rot([c.ps([128, 512], F32, "pz") for _ in range(4)])
            PY = Rot([c.ps([128, 512], F32, "py") for _ in range(2)])
            PP = Rot([c.ps([128, 512], F32, "pp") for _ in range(1)])
            PTr = Rot([c.ps([128, 8, 128], BF16, "ptr") for _ in range(1)])
            for t_ in SPt.items:
                c.op("dve", lambda e: e.memset(t_.t[:], 0.0), [], [t_])

            def load_pair(p):
                b = p % 2
                load_w(wq[b], wq[b].t[:], win[:, p * 128:(p + 1) * 128])
                load_w(wk[b], wk[b].t[:], win[:, 1024 + p * 128:1024 + (p + 1) * 128])
                load_w(wv[b], wv[b].t[:], win[:, 2048 + p * 128:2048 + (p + 1) * 128])

            def proj_items(p):
                bb = p % 2
                items = []
                for tq in range(4):
                    def fq(tq=tq):
                        pb = PP.next()
                        proj_fm(pb, wq[bb], lambda k: wq[bb].t[:, k, :], tq)
                        c.op("act", lambda e: e.activation(out=qT[bb].t[:, tq * 512:(tq + 1) * 512], in_=pb.t[:], func=AF.Copy, scale=0.125),
                             [pb], [qT[bb]])

                    def fk(tq=tq):
                        pb = PP.next()
                        proj_fm(pb, wk[bb], lambda k: wk[bb].t[:, k, :], tq)
                        c.op("dve", lambda e: e.tensor_copy(out=kT[bb].t[:, tq * 512:(tq + 1) * 512], in_=pb.t[:]), [pb], [kT[bb]])
                    items += [fq, fk]
                for i4 in range(4):
                    def fv(i4=i4):
                        pb = PP.next()
                        for jj in range(4):
                            proj_tm(pb, pb.t[:, jj * 128:(jj + 1) * 128], wv[bb], lambda k: wv[bb].t[:, k, :], i4 * 4 + jj)
                        c.op("act", lambda e: e.activation(out=V[bb].t[:, i4 * 4:(i4 + 1) * 4, :],
                                                           in_=pb.t[:].rearrange("p (a b) -> p a b", a=4), func=AF.Copy), [pb], [V[bb]])
                    items.append(fv)
                return items

            load_pair(0)
            load_pair(1)
            for it in proj_items(0):
                it()
            for p in range(8):
                b = p % 2
                if p + 2 < 8:
                    load_pair(p + 2)
                nxt = proj_items(p + 1) if p + 1 < 8 else []
                steps = []
                for hh in range(2):
                    for g in range(4):
                        grp = {"py": None, "first": True, "ss": None}
                        for idx, kb in enumerate(range(4 * g + 3, -1, -1)):
                            steps.append(dict(hh=hh, g=g, kb=kb, grp=grp))

                def stA(st):
                    hh, g, kb, grp = st["hh"], st["g"], st["kb"], st["grp"]
                    lo, hi = 64 * hh, 64 * hh + 64
                    o = kb - 4 * g
                    pz = PZ.next(); st["pz"] = pz
                    c0 = max(o, 0) * 128
                    c.op("pe", lambda e: e.matmul(pz.t[:, c0:512], lhsT=kT[b].t[lo:hi, kb * 128:(kb + 1) * 128],
                                                  rhs=qT[b].t[lo:hi, g * 512 + c0:(g + 1) * 512], start=True, stop=False, skip_group_check=True),
                         [kT[b], qT[b]], [pz])
                    E_ = Et.next(); SP_ = SPt.next(); Lm = LmP.next()
                    c.op("act", lambda e: e.activation(out=E_.t[:, c0:512], in_=pz.t[:, c0:512], func=AF.Exp), [pz], [E_])
                    c.op("act", lambda e: e.activation(out=SP_.t[:, c0:512], in_=E_.t[:, c0:512], func=AF.Ln, bias=1.0), [E_], [SP_])
                    if o >= 0:
                        c.op("dve", lambda e: e.tensor_tensor(out=Lm.t[:], in0=SP_.t[:], in1=maskLT(o), op=ALU.mult), [SP_, CON], [Lm])
                    else:
                        c.op("dve", lambda e: e.tensor_copy(out=Lm.t[:], in_=SP_.t[:]), [SP_], [Lm])
                    st["Lm"] = Lm
                    st["ss_prev"] = grp["ss"]
                    if kb > 0:
                        if grp["ss"] is None:
                            grp["ss"] = Lm
                        else:
                            ss_new = Ssum.next()
                            sp0 = grp["ss"]
                            c.op("pool", lambda e: e.tensor_tensor(out=ss_new.t[:], in0=sp0.t[:], in1=Lm.t[:], op=ALU.add), [sp0, Lm], [ss_new])
                            grp["ss"] = ss_new

                def stB(st):
                    g, kb = st["g"], st["kb"]
                    o = kb - 4 * g
                    pz, Lm, ss_prev = st["pz"], st["Lm"], st["ss_prev"]
                    c0 = max(o, 0) * 128
                    c.op("pe", lambda e: e.matmul(pz.t[:, c0:512], lhsT=uneg, rhs=Lm.t[:, c0:512], start=False, stop=(ss_prev is None), skip_group_check=True),
                         [CON, Lm], [pz])
                    if ss_prev is not None:
                        c.op("pe", lambda e: e.matmul(pz.t[:, c0:512], lhsT=ONEN.t[:], rhs=ss_prev.t[:, c0:512], start=False, stop=True, skip_group_check=True),
                             [ONEN, ss_prev], [pz])
                    W_ = WT.next(); st["W"] = W_
                    c.op("act", lambda e: e.activation(out=W_.t[:, c0:512], in_=pz.t[:, c0:512], func=AF.Exp), [pz], [W_])
                    if o >= 0:
                        c.op("dve", lambda e: e.tensor_tensor(out=W_.t[:, c0:512], in0=W_.t[:, c0:512], in1=maskLT(o)[:, c0:512], op=ALU.mult), [W_, CON], [W_])

                def stC(st):
                    hh, g, kb, grp = st["hh"], st["g"], st["kb"], st["grp"]
                    lo, hi = 64 * hh, 64 * hh + 64
                    if grp["py"] is None:
                        grp["py"] = PY.next()
                    py = grp["py"]; W_ = st["W"]
                    for jq in range(4):
                        if 4 * g + jq < kb:
                            continue
                        c.op("pe", lambda e: e.matmul(py.t[:, jq * 64:(jq + 1) * 64], lhsT=W_.t[:, jq * 128:(jq + 1) * 128],
                                                      rhs=V[b].t[:, kb, lo:hi], start=grp["first"], stop=(kb == 0 and jq == 3),
                                                      skip_group_check=True), [W_, V[b]], [py])
                        grp["first"] = False
                    if kb == 0:
                        c.op("act", lambda e: e.activation(out=Yp.t[:, 4 * g:4 * g + 4, lo:hi],
                                                           in_=py.t[:, 0:256].rearrange("p (a b) -> p a b", a=4), func=AF.Copy), [py], [Yp])

                ns = len(steps)
                for n in range(ns + 3):
                    if n < ns: stA(steps[n])
                    if 0 <= n - 2 < ns: stB(steps[n - 2])
                    if 0 <= n - 3 < ns: stC(steps[n - 3])
                    if n % 6 == 3 and nxt:
                        nxt.pop(0)()
                for it in nxt:
                    it()
                for i2 in range(2):
                    ptr = PTr.next()
                    for jj in range(8):
                        i = i2 * 8 + jj
                        c.op("pe", lambda e: e.transpose(out=ptr.t[:, jj, :], in_=Yp.t[:, i, :], identity=ident), [Yp, CON], [ptr])
                    c.op("dve", lambda e: e.tensor_copy(out=YT[p].t[:, i2 * 1024:(i2 + 1) * 1024],
                                                        in_=ptr.t[:].rearrange("p a b -> p (a b)")), [ptr], [YT[p]])
            c.pop()
            return YT


        def mixer_fox(win, bf_dram):
            c.push()
            YT = [c.sb([128, S], BF16, f"YT{k}") for k in range(8)]
            c.push()
            wq = [wslot([128, 8, 128], f"w{j}") for j in range(2)]
            wk = [wslot([128, 8, 128], f"w{2 + j}") for j in range(2)]
            wv = [wslot([128, 8, 128], f"w{4 + j}") for j in range(2)]
            wf = wslot([128, 8, 16], "w6")
            qT = [c.sb([128, S], BF16, "qT") for _ in range(2)]
            kT = [c.sb([128, S], BF16, "kT") for _ in range(2)]
            V = [c.sb([128, NT, 2, 65], BF16, "V") for _ in range(2)]
            Yp = c.sb([128, NT, 128], BF16, "Yp")
            WT = Rot([c.sb([128, 512], BF16, "WT") for _ in range(4)])
            Bh = Rot([c.sb([128, 8, 16], F32, "Bh") for _ in range(4)])
            RD = Rot([c.sb([128, 4, 1], F32, "rd") for _ in range(2)])
            lfc = c.sb([128, 256], F32, "lfc"); tmp = c.sb([128, 256], F32, "ftmp")
            T2 = c.sb([128, 256], F32, "T2"); PSc = c.sb([128, 256], F32, "PSc")
            ugt = c.sb([128, 128], F32, "ugt"); onef = c.sb([128, 128], F32, "onef")
            bfB = c.sb([128, 16], F32, "bfB")
            PZ = Rot([c.ps([128, 512], F32, "pz") for _ in range(4)])
            PY = Rot([c.ps([128, 512], F32, "py") for _ in range(2)])
            PP = Rot([c.ps([128, 512], F32, "pp") for _ in range(1)])
            PTr = Rot([c.ps([128, 8, 128], BF16, "ptr") for _ in range(1)])
            for b in range(2):
                c.op("dve", lambda e: e.memset(V[b].t[:, :, :, 64:65], 1.0), [], [V[b]])
            c.op("dve", lambda e: e.memset(onef.t[:], 1.0), [], [onef])
            c.dma("sp", ugt.t[:], Dm["con"][:, C_GT:C_GT + 128], writes=[ugt], key="cf")
            c.dma("sp", bfB.t[:], bf_dram[0:1, :].partition_broadcast(128), writes=[bfB], key="bf")
            load_w(wf, wf.t[:], win[:, 3072:3088])

            def load_pair(p):
                b = p % 2
                load_w(wq[b], wq[b].t[:], win[:, p * 128:(p + 1) * 128])
                load_w(wk[b], wk[b].t[:], win[:, 1024 + p * 128:1024 + (p + 1) * 128])
                load_w(wv[b], wv[b].t[:], win[:, 2048 + p * 128:2048 + (p + 1) * 128])

            load_pair(0)
            for i in range(NT):
                pb = PP.next()
                proj_tm(pb, pb.t[:, 0:16], wf, lambda k: wf.t[:, k, :], i)
                c.op("dve", lambda e: e.tensor_tensor(out=lfc.t[:, i * 16:(i + 1) * 16], in0=pb.t[:, 0:16], in1=bfB.t[:], op=ALU.add),
                     [pb, bfB], [lfc])
            c.op("act", lambda e: e.activation(out=tmp.t[:], in_=lfc.t[:], func=AF.Exp, scale=-1.0), [lfc], [tmp])
            c.op("act", lambda e: e.activation(out=tmp.t[:], in_=tmp.t[:], func=AF.Ln, bias=1.0), [tmp], [tmp])
            c.op("dve", lambda e: e.tensor_scalar(out=lfc.t[:], in0=tmp.t[:], scalar1=-1.0, scalar2=None, op0=ALU.mult), [tmp], [lfc])
            pb = PP.next()
            c.op("pe", lambda e: e.matmul(pb.t[:, 0:256], lhsT=onef.t[:], rhs=lfc.t[:], start=True, stop=True), [onef, lfc], [pb])
            c.op("dve", lambda e: e.tensor_copy(out=PSc.t[:, 0:16], in_=pb.t[:, 0:16]), [pb], [PSc])
            for jb in range(1, 16):
                c.op("dve", lambda e: e.tensor_tensor(out=PSc.t[:, jb * 16:(jb + 1) * 16], in0=PSc.t[:, (jb - 1) * 16:jb * 16],
                                                      in1=pb.t[:, jb * 16:(jb + 1) * 16], op=ALU.add), [pb, PSc], [PSc])
            pb = PP.next()
            c.op("pe", lambda e: e.matmul(pb.t[:, 0:256], lhsT=ugt.t[:], rhs=lfc.t[:], start=True, stop=True), [ugt, lfc], [pb])
            c.op("dve", lambda e: e.tensor_tensor(out=T2.t[:], in0=pb.t[:, 0:256], in1=PSc.t[:], op=ALU.subtract), [pb, PSc], [T2])
            T2v = T2.t[:].rearrange("p (a b) -> p a b", a=16)

            def proj_items(p):
                bb = p % 2
                items = []
                for tq in range(4):
                    def fq(tq=tq):
                        pb = PP.next()
                        proj_fm(pb, wq[bb], lambda k: wq[bb].t[:, k, :], tq)
                        c.op("act", lambda e: e.activation(out=qT[bb].t[:, tq * 512:(tq + 1) * 512], in_=pb.t[:], func=AF.Copy, scale=0.125),
                             [pb], [qT[bb]])

                    def fk(tq=tq):
                        pb = PP.next()
                        proj_fm(pb, wk[bb], lambda k: wk[bb].t[:, k, :], tq)
                        c.op("dve", lambda e: e.tensor_copy(out=kT[bb].t[:, tq * 512:(tq + 1) * 512], in_=pb.t[:]), [pb], [kT[bb]])
                    items += [fq, fk]
                for i4 in range(4):
                    def fv(i4=i4):
                        pb = PP.next()
                        for jj in range(4):
                            proj_tm(pb, pb.t[:, jj * 128:(jj + 1) * 128], wv[bb], lambda k: wv[bb].t[:, k, :], i4 * 4 + jj)
                        for hh in range(2):
                            c.op("act", lambda e: e.activation(out=V[bb].t[:, i4 * 4:(i4 + 1) * 4, hh, 0:64],
                                                               in_=pb.t[:].rearrange("p (a h d) -> p a h d", a=4, h=2)[:, :, hh, :], func=AF.Copy),
                                 [pb], [V[bb]])
                    items.append(fv)
                return items

            PSm = c.sb([128, 8, 16], F32, "PSm")
            PSv = PSc.t[:].rearrange("p (q two h) -> p q two h", two=2, h=16)
            c.op("dve", lambda e: e.tensor_tensor(out=PSm.t[:], in0=PSv[:, :, 0, :], in1=PSv[:, :, 1, :], op=ALU.add), [PSc], [PSm])
            c.op("dve", lambda e: e.tensor_scalar(out=PSm.t[:], in0=PSm.t[:], scalar1=0.5, scalar2=None, op0=ALU.mult), [PSm], [PSm])
            load_pair(1)
            for it in proj_items(0):
                it()
            for p in range(8):
                b = p % 2
                if p + 2 < 8:
                    load_pair(p + 2)
                nxt = proj_items(p + 1) if p + 1 < 8 else []
                steps = []
                for hh in range(2):
                    h = 2 * p + hh
                    B_ = Bh.next()
                    for pr in range(8):
                        c.op("dve", lambda e: e.tensor_scalar(out=B_.t[:, pr, :], in0=T2v[:, :, h], scalar1=PSm.t[:, pr, h:h + 1],
                                                              scalar2=None, op0=ALU.add), [T2, PSm], [B_])
                    for g in range(4):
                        grp = {"py": None, "first": True}
                        for kb in range(4 * g + 4):
                            steps.append(dict(hh=hh, g=g, kb=kb, grp=grp, B=B_))

                def fA(st):
                    hh, g, kb = st["hh"], st["g"], st["kb"]
                    lo, hi = 64 * hh, 64 * hh + 64
                    pz = PZ.next(); st["pz"] = pz
                    c.op("pe", lambda e: e.matmul(pz.t[:], lhsT=kT[b].t[lo:hi, kb * 128:(kb + 1) * 128],
                                                  rhs=qT[b].t[lo:hi, g * 512:(g + 1) * 512], start=True, stop=True), [kT[b], qT[b]], [pz])

                def fB(st):
                    g, kb, pz, B_ = st["g"], st["kb"], st["pz"], st["B"]
                    W_ = WT.next(); st["W"] = W_
                    for p2 in range(2):
                        if 4 * g + 2 * p2 + 1 < kb:
                            continue
                        pr = 2 * g + p2
                        c.op("act", lambda e: e.activation(out=W_.t[:, p2 * 256:(p2 + 1) * 256], in_=pz.t[:, p2 * 256:(p2 + 1) * 256],
                                                           func=AF.Exp, bias=B_.t[:, pr, kb:kb + 1]), [pz, B_], [W_])
                    for jq in range(4):
                        Q = 4 * g + jq
                        if Q == kb:
                            c.op("pool", lambda e: e.tensor_tensor(out=W_.t[:, jq * 128:(jq + 1) * 128], in0=W_.t[:, jq * 128:(jq + 1) * 128],
                                                                   in1=maskLE, op=ALU.mult), [W_, CON], [W_])

                def fC(st):
                    hh, g, kb, grp, W_ = st["hh"], st["g"], st["kb"], st["grp"], st["W"]
                    lo, hi = 64 * hh, 64 * hh + 64
                    if grp["py"] is None:
                        grp["py"] = PY.next()
                    py = grp["py"]
                    for jq in range(4):
                        Q = 4 * g + jq
                        if Q < kb:
                            continue
                        c.op("pe", lambda e: e.matmul(py.t[:, jq * 65:jq * 65 + 65], lhsT=W_.t[:, jq * 128:(jq + 1) * 128],
                                                      rhs=V[b].t[:, kb, hh, :], start=grp["first"], stop=(kb == 4 * g + 3 and jq == 3),
                                                      skip_group_check=True), [W_, V[b]], [py])
                        grp["first"] = False
                    if kb == 4 * g + 3:
                        rd = RD.next()
                        pyv = py.t[:, 0:260].rearrange("p (a b) -> p a b", a=4)
                        c.op("dve", lambda e: e.reciprocal(out=rd.t[:], in_=pyv[:, :, 64:65]), [py], [rd])
                        for jq in range(4):
                            c.op("act", lambda e: e.activation(out=Yp.t[:, 4 * g + jq, lo:hi], in_=py.t[:, jq * 65:jq * 65 + 64], func=AF.Copy,
                                                               scale=rd.t[:, jq, :]), [py, rd], [Yp])

                ns = len(steps)
                for n in range(ns + 3):
                    if n < ns: fA(steps[n])
                    if 0 <= n - 2 < ns: fB(steps[n - 2])
                    if 0 <= n - 3 < ns: fC(steps[n - 3])
                    if n % 6 == 3 and nxt:
                        nxt.pop(0)()
                for it in nxt:
                    it()
                for i2 in range(2):
                    ptr = PTr.next()
                    for jj in range(8):
                        i = i2 * 8 + jj
                        c.op("pe", lambda e: e.transpose(out=ptr.t[:, jj, :], in_=Yp.t[:, i, :], identity=ident), [Yp, CON], [ptr])
                    c.op("dve", lambda e: e.tensor_copy(out=YT[p].t[:, i2 * 1024:(i2 + 1) * 1024],
                                                        in_=ptr.t[:].rearrange("p a b -> p (a b)")), [ptr], [YT[p]])
            c.pop()
            return YT


        def mixer_mlstm(win):
            c.push()
            YT = [c.sb([128, S], BF16, f"YT{k}") for k in range(8)]
            aT = c.sb([4, S], F32, "aT"); nG = c.sb([4, S], F32, "nG")
            cols = c.sb([128, 16, 16], F32, "cols")
            c4 = c.sb([4, 644], F32, "c4")
            c.dma("sp", c4.t[:], Dm["con4"][:, :], writes=[c4], key="cf")
            I4 = c4.t[:, 512:516]; ones4 = c4.t[:, 516:644]
            c.push()
            wi = wslot([128, 8, 4], "w6"); wf = wslot([128, 8, 4], "w7")
            load_w(wi, wi.t[:], win[:, 3072:3076]); load_w(wf, wf.t[:], win[:, 3076:3080])
            bi = c.sb([4, 1], F32, "bi"); bfv = c.sb([4, 1], F32, "bfv")
            c.dma("sp", bi.t[:], Dm["ml_bi"][:, :], writes=[bi], key="bi")
            c.dma("sp", bfv.t[:], Dm["ml_bf"][:, :], writes=[bfv], key="bf")
            c.op("dve", lambda e: e.tensor_scalar(out=bfv.t[:], in0=bfv.t[:], scalar1=-1.0, scalar2=None, op0=ALU.mult), [bfv], [bfv])
            iT = c.sb([4, S], F32, "iT"); t4 = c.sb([4, S], F32, "t4"); Fp = c.sb([4, S], F32, "Fp"); G_ = c.sb([4, S], F32, "G")
            GP = c.sb([4, 16, 3], F32, "GP"); Dg = c.sb([4, 16, 12], F32, "Dg")
            PP = Rot([c.ps([128, 512], F32, "pp") for _ in range(2)])
            PCo = c.ps([128, 512], F32, "pco")
            for tq in range(4):
                pb = PP.next()
                proj_fm(pb, wi, lambda k: wi.t[:, k, :], tq)
                c.op("act", lambda e: e.activation(out=iT.t[:, tq * 512:(tq + 1) * 512], in_=pb.t[0:4, :], func=AF.Identity, bias=bi.t[:, 0:1]),
                     [pb, bi], [iT])
                pb = PP.next()
                proj_fm(pb, wf, lambda k: wf.t[:, k, :], tq)
                c.op("act", lambda e: e.activation(out=t4.t[:, tq * 512:(tq + 1) * 512], in_=pb.t[0:4, :], func=AF.Exp, bias=bfv.t[:, 0:1], scale=-1.0),
                     [pb, bfv], [t4])
            c.op("act", lambda e: e.activation(out=t4.t[:], in_=t4.t[:], func=AF.Ln, bias=1.0), [t4], [t4])
            c.op("dve", lambda e: e.tensor_tensor_scan(out=Fp.t[:], data0=t4.t[:], data1=t4.t[:], initial=0.0, op0=ALU.add, op1=ALU.max),
                 [t4], [Fp])
            c.op("dve", lambda e: e.tensor_tensor(out=aT.t[:], in0=iT.t[:], in1=Fp.t[:], op=ALU.add), [iT, Fp], [aT])
            c.op("dve", lambda e: e.tensor_tensor_scan(out=G_.t[:], data0=aT.t[:], data1=aT.t[:], initial=0.0, op0=ALU.max, op1=ALU.max),
                 [aT], [G_])
            c.op("dve", lambda e: e.tensor_scalar(out=nG.t[:], in0=G_.t[:], scalar1=-1.0, scalar2=None, op0=ALU.mult), [G_], [nG])
            c.op("dve", lambda e: e.tensor_tensor(out=iT.t[:], in0=Fp.t[:], in1=G_.t[:], op=ALU.subtract), [Fp, G_], [iT])
            nM = iT
            Gend = G_.t[:].rearrange("p (c t) -> p c t", t=128)[:, :, 127]
            c.op("dve", lambda e: e.memset(GP.t[:], 0.0), [], [GP])
            c.op("dve", lambda e: e.tensor_copy(out=GP.t[:, 1:16, 0], in_=G_.t[:].rearrange("p (c t) -> p c t", t=128)[:, 0:15, 127]), [G_], [GP])
            c.op("dve", lambda e: e.tensor_scalar(out=GP.t[:, :, 1], in0=Gend, scalar1=-1.0, scalar2=None, op0=ALU.mult), [G_], [GP])
            c.op("dve", lambda e: e.tensor_tensor(out=GP.t[:, :, 2], in0=GP.t[:, :, 0], in1=GP.t[:, :, 1], op=ALU.add), [GP], [GP])
            for cc in range(16):
                for j in range(3):
                    c.op("dve", lambda e: e.tensor_scalar(out=Dg.t[:, cc, j * 4:(j + 1) * 4], in0=I4, scalar1=GP.t[:, cc, j:j + 1], scalar2=None,
                                                          op0=ALU.mult), [c4, GP], [Dg])
            for cc in range(16):
                sl = slice(cc * 128, (cc + 1) * 128)
                o0 = cc * 16
                mmx = lambda oc, l, r, st, sp_: c.op("pe", lambda e: e.matmul(PCo.t[:, o0 + oc:o0 + oc + 4], lhsT=l, rhs=r, start=st, stop=sp_,
                                                                              skip_group_check=True), [nG, aT, nM, c4, Dg], [PCo])
                mmx(0, nG.t[:, sl], I4, True, False); mmx(0, ones4, Dg.t[:, cc, 0:4], False, True)
                mmx(4, nM.t[:, sl], I4, True, True)
                mmx(8, aT.t[:, sl], I4, True, False); mmx(8, ones4, Dg.t[:, cc, 4:8], False, True)
                mmx(12, ones4, Dg.t[:, cc, 8:12], True, True)
            c.op("act", lambda e: e.activation(out=cols.t[:].rearrange("p a b -> p (a b)"), in_=PCo.t[:, 0:256], func=AF.Exp), [PCo], [cols])
            c.pop()
            c.push()
            wq = [wslot([128, 8, 128], f"w{j}") for j in range(2)]
            wk = [wslot([128, 8, 128], f"w{2 + j}") for j in range(2)]
            wv = [wslot([128, 8, 256], f"w{4 + j}") for j in range(2)]
            wo_ = [wslot([128, 8, 256], f"w{6 + j}") for j in range(2)]
            qTc = Rot([c.sb([128, 128], BF16, "qTc") for _ in range(4)])
            kTc = Rot([c.sb([128, 128], BF16, "kTc") for _ in range(2)])
            kw = Rot([c.sb([128, 128], BF16, "kw") for _ in range(4)])
            Va = Rot([c.sb([128, 257], BF16, "Va") for _ in range(4)])
            Wt = Rot([c.sb([128, 128], F32, "Wt") for _ in range(3)])
            PTt = Rot([c.sb([128, 128], BF16, "PTt") for _ in range(4)])
            ONEF = c.sb([128, 256], F32, "ONEF")
            c.op("pool", lambda e: e.memset(ONEF.t[:], -1.0), [], [ONEF])
            tI = Rot([c.sb([128, 257], F32, "tI") for _ in range(2)])
            tot = Rot([c.sb([128, 257], F32, "tot") for _ in range(2)])
            sg = Rot([c.sb([128, 256], F32, "sg") for _ in range(4)])
            yh = Rot([c.sb([128, 256], BF16, "yh") for _ in range(3)])
            dn = Rot([c.sb([128, 2], F32, "dn") for _ in range(2)])
            Cf = c.sb([128, 257], F32, "Cf"); Cb = c.sb([128, 257], BF16, "Cb")
            PP = Rot([c.ps([128, 512], F32, "pp") for _ in range(2)])
            PW = Rot([c.ps([128, 512], F32, "pw") for _ in range(1)])
            PS2 = Rot([c.ps([128, 512], F32, "ps2") for _ in range(1)])
            PN = Rot([c.ps([128, 512], F32, "pn") for _ in range(2)])
            PC = c.ps([128, 512], F32, "pc")
            PTr = c.ps([128, 8, 128], BF16, "ptr")
            for v_ in Va.items:
                c.op("dve", lambda e: e.memset(v_.t[:, 256:257], 1.0), [], [v_])

            def load_head(h):
                b = h % 2
                load_w(wq[b], wq[b].t[:], win[:, h * 128:(h + 1) * 128])
                load_w(wk[b], wk[b].t[:], win[:, 512 + h * 128:512 + (h + 1) * 128])
                load_w(wv[b], wv[b].t[:], win[:, 1024 + h * 256:1024 + (h + 1) * 256])
                load_w(wo_[b], wo_[b].t[:], win[:, 2048 + h * 256:2048 + (h + 1) * 256])

            import os
            load_head(0)
            for h in range(int(os.environ.get('ML_HEADS', '4'))):
                b = h % 2
                if h + 1 < 4:
                    load_head(h + 1)
                Esel = c4.t[:, h * 128:(h + 1) * 128]
                def mS1(cc):
                    q_, j_ = divmod(cc, 4)
                    sl = slice(cc * 128, (cc + 1) * 128)
                    hsl = lambda k: HT[q_].t[:, k, j_ * 128:(j_ + 1) * 128]
                    pw = PW.next()
                    c.op("pe", lambda e: e.matmul(pw.t[:, 0:128], lhsT=aT.t[:, sl], rhs=Esel, start=True, stop=False), [aT, c4], [pw])
                    c.op("pe", lambda e: e.matmul(pw.t[:, 0:128], lhsT=Esel, rhs=nG.t[:, sl], start=False, stop=True), [nG, c4], [pw])
                    w_ = Wt.next()
                    c.op("act", lambda e: e.activation(out=w_.t[:], in_=pw.t[:, 0:128], func=AF.Exp), [pw], [w_])
                    c.op("dve", lambda e: e.tensor_tensor(out=w_.t[:], in0=w_.t[:], in1=maskLE, op=ALU.mult), [w_, CON], [w_])
                    pb = PP.next()
                    for k in range(8):
                        c.op("pe", lambda e: e.matmul(pb.t[:, 0:128], lhsT=wq[b].t[:, k, :], rhs=hsl(k), start=(k == 0), stop=(k == 7)),
                             [wq[b], HT[q_]], [pb])
                    for k in range(8):
                        c.op("pe", lambda e: e.matmul(pb.t[:, 128:256], lhsT=wk[b].t[:, k, :], rhs=hsl(k), start=(k == 0), stop=(k == 7),
                                                      skip_group_check=True), [wk[b], HT[q_]], [pb])
                    qc = qTc.next(); kc = kTc.next()
                    c.op("act", lambda e: e.activation(out=qc.t[:], in_=pb.t[:, 0:128], func=AF.Copy, scale=128.0 ** -0.5), [pb], [qc])
                    c.op("dve", lambda e: e.tensor_copy(out=kc.t[:], in_=pb.t[:, 128:256]), [pb], [kc])
                    pb = PP.next()
                    for k in range(8):
                        c.op("pe", lambda e: e.matmul(pb.t[:, 0:128], lhsT=hsl(k), rhs=wk[b].t[:, k, :], start=(k == 0), stop=(k == 7)),
                             [wk[b], HT[q_]], [pb])
                    for k in range(8):
                        c.op("pe", lambda e: e.matmul(pb.t[:, 128:384], lhsT=hsl(k), rhs=wv[b].t[:, k, :], start=(k == 0), stop=(k == 7),
                                                      skip_group_check=True), [wv[b], HT[q_]], [pb])
                    kw_ = kw.next(); va = Va.next()
                    c.op("act", lambda e: e.activation(out=kw_.t[:], in_=pb.t[:, 0:128], func=AF.Copy, scale=cols.t[:, cc, 8 + h:9 + h]),
                         [pb, cols], [kw_])
                    c.op("dve", lambda e: e.tensor_copy(out=va.t[:, 0:256], in_=pb.t[:, 128:384]), [pb], [va])
                    ps_ = PS2.next()
                    c.op("pe", lambda e: e.matmul(ps_.t[:, 0:128], lhsT=kc.t[:], rhs=qc.t[:], start=True, stop=True), [kc, qc], [ps_])
                    pt = PTt.next()
                    c.op("dve", lambda e: e.tensor_tensor(out=pt.t[:], in0=ps_.t[:, 0:128], in1=w_.t[:], op=ALU.mult), [ps_, w_], [pt])
                    pb = PP.next()
                    for k in range(8):
                        c.op("pe", lambda e: e.matmul(pb.t[:, 0:256], lhsT=hsl(k), rhs=wo_[b].t[:, k, :], start=(k == 0), stop=(k == 7)),
                             [wo_[b], HT[q_]], [pb])
                    s_ = sg.next()
                    c.op("act", lambda e: e.activation(out=s_.t[:], in_=pb.t[:, 0:256], func=AF.Tanh, scale=0.5), [pb], [s_])
                    c.op("dve", lambda e: e.tensor_scalar(out=s_.t[:], in0=s_.t[:], scalar1=0.5, scalar2=0.5, op0=ALU.mult, op1=ALU.add), [s_], [s_])
                    return dict(qc=qc, kw=kw_, va=va, pt=pt, s=s_)

                def mS2(cc, d, prev_y):
                    sl = slice(cc * 128, (cc + 1) * 128)
                    qc, kw_, va, pt, s_ = d["qc"], d["kw"], d["va"], d["pt"], d["s"]
                    if cc > 0:
                        pi = PN.next()
                        c.op("pe", lambda e: e.matmul(pi.t[:, 0:257], lhsT=qc.t[:], rhs=Cb.t[:], start=True, stop=True), [qc, Cb], [pi])
                    c.op("pe", lambda e: e.matmul(PC.t[:, 0:257], lhsT=kw_.t[:], rhs=va.t[:], start=True, stop=True), [kw_, va], [PC])
                    pn = PN.next()
                    c.op("pe", lambda e: e.matmul(pn.t[:, 0:257], lhsT=pt.t[:], rhs=va.t[:], start=True, stop=True), [pt, va], [pn])
                    if prev_y is not None:
                        pcc, py_ = prev_y
                        for j2 in range(2):
                            c.op("pe", lambda e: e.transpose(out=PTr.t[:, j2, :], in_=py_.t[:, j2 * 128:(j2 + 1) * 128], identity=ident), [py_, CON], [PTr])
                        for j2 in range(2):
                            c.op("act", lambda e: e.activation(out=YT[2 * h + j2].t[:, pcc * 128:(pcc + 1) * 128], in_=PTr.t[:, j2, :], func=AF.Copy),
                                 [PTr], [YT[2 * h + j2]])
                    if cc > 0:
                        c.op("dve", lambda e: e.scalar_tensor_tensor(out=Cf.t[:], in0=Cf.t[:], scalar=cols.t[:, cc, 12 + h:13 + h], in1=PC.t[:, 0:257],
                                                                     op0=ALU.mult, op1=ALU.add), [Cf, cols, PC], [Cf])
                    else:
                        c.op("dve", lambda e: e.tensor_copy(out=Cf.t[:], in_=PC.t[:, 0:257]), [PC], [Cf])
                    to_ = tot.next()
                    if cc > 0:
                        ti = tI.next()
                        c.op("act", lambda e: e.activation(out=ti.t[:], in_=pi.t[:, 0:257], func=AF.Copy, scale=cols.t[:, cc, h:h + 1]),
                             [pi, cols], [ti])
                    c.op("act", lambda e: e.activation(out=Cb.t[:], in_=Cf.t[:], func=AF.Copy), [Cf], [Cb])
                    if cc > 0:
                        c.op("dve", lambda e: e.tensor_tensor(out=to_.t[:], in0=ti.t[:], in1=pn.t[:, 0:257], op=ALU.add), [ti, pn], [to_])
                    else:
                        c.op("dve", lambda e: e.tensor_copy(out=to_.t[:], in_=pn.t[:, 0:257]), [pn], [to_])
                    d_ = dn.next()
                    c.op("dve", lambda e: e.tensor_scalar(out=d_.t[:, 0:1], in0=to_.t[:, 256:257], scalar1=-1.0, scalar2=None, op0=ALU.mult), [to_], [d_])
                    c.op("dve", lambda e: e.tensor_tensor(out=d_.t[:, 0:1], in0=d_.t[:, 0:1], in1=to_.t[:, 256:257], op=ALU.max), [to_, d_], [d_])
                    c.op("dve", lambda e: e.tensor_tensor(out=d_.t[:, 0:1], in0=d_.t[:, 0:1], in1=cols.t[:, cc, 4 + h:5 + h], op=ALU.max), [cols, d_], [d_])
                    c.op("dve", lambda e: e.reciprocal(out=d_.t[:, 1:2], in_=d_.t[:, 0:1]), [d_], [d_])
                    y_ = yh.next()
                    c.op("dve", lambda e: e.scalar_tensor_tensor(out=y_.t[:], in0=to_.t[:, 0:256], scalar=d_.t[:, 1:2], in1=s_.t[:],
                                                                 op0=ALU.mult, op1=ALU.mult), [to_, d_, s_], [y_])
                    return (cc, y_)

                def mFlush(prev_y):
                    pcc, py_ = prev_y
                    for j2 in range(2):
                        c.op("pe", lambda e: e.transpose(out=PTr.t[:, j2, :], in_=py_.t[:, j2 * 128:(j2 + 1) * 128], identity=ident), [py_, CON], [PTr])
                    for j2 in range(2):
                        c.op("act", lambda e: e.activation(out=YT[2 * h + j2].t[:, pcc * 128:(pcc + 1) * 128], in_=PTr.t[:, j2, :], func=AF.Copy),
                             [PTr], [YT[2 * h + j2]])

                dd = {}; prev_y = None
                for n in range(16 + 2):
                    if n < 16:
                        dd[n] = mS1(n)
                    if 0 <= n - 2 < 16:
                        prev_y = mS2(n - 2, dd.pop(n - 2), prev_y)
                mFlush(prev_y)
            c.pop()
            return YT

        def mixer_gla(win):
            c.push()
            YT = [c.sb([128, S], BF16, f"YT{k}") for k in range(8)]
            c.push()
            wa = wslot([128, 8, 16], "w8")
            wa2 = wslot([16, 512], "w9")
            load_w(wa, wa.t[:], win[:, 3072:3088])
            c.dma("pool", wa2.t[:], Dm["gla_wa2"][:, :], writes=[wa2], key="w9")
            rm = wslot([128, S], "w10")
            c.dma("pool", rm.t[:], Dm["rmask"][:, :], writes=[rm], key="w10")
            nba = c.sb([128, 4], F32, "nba")
            c.dma("sp", nba.t[:], Dm["gla_ba"][:, :], writes=[nba], key="bi")
            c.op("dve", lambda e: e.tensor_scalar(out=nba.t[:], in0=nba.t[:], scalar1=-1.0, scalar2=None, op0=ALU.mult), [nba], [nba])
            gB = c.sb([128, 256], F32, "gB256")
            c.dma("sp", gB.t[:], Dm["gla_norm_g"][0:1, :].partition_broadcast(128), writes=[gB], key="bf")
            c.op("dve", lambda e: e.tensor_scalar(out=gB.t[:], in0=gB.t[:], scalar1=0.5, scalar2=None, op0=ALU.mult), [gB], [gB])
            eps = c.sb([128, 1], F32, "geps")
            c.op("dve", lambda e: e.memset(eps.t[:], 1e-6), [], [eps])
            alT = c.sb([16, S], BF16, "alT")
            wq = wslot([128, 8, 128], "w0"); wk = wslot([128, 8, 128], "w1")
            wv = wslot([128, 8, 256], "w2"); wr = wslot([128, 8, 256], "w3")
            sp_ = c.sb([128, S], F32, "sp"); bp = c.sb([128, S], F32, "bp")
            qtl = c.sb([128, S], BF16, "qtl"); ktl = c.sb([128, S], BF16, "ktl")
            tE = Rot([c.sb([128, 512], F32, "tE") for _ in range(2)])
            ebl = c.sb([128, 16], F32, "ebl")
            Vc = Rot([c.sb([128, 256], BF16, "Vc") for _ in range(4)])
            AT = Rot([c.sb([128, 128], BF16, "AT") for _ in range(4)])
            khT = Rot([c.sb([128, 128], BF16, "khT") for _ in range(3)])
            kh = Rot([c.sb([128, 128], BF16, "kh") for _ in range(4)])
            NEG1 = c.sb([128, 256], F32, "NEG1")
            c.op("pool", lambda e: e.memset(NEG1.t[:], -1.0), [], [NEG1])
            junk = c.sb([128, 256], F32, "junk")
            sm = Rot([c.sb([128, 4], F32, "gsm") for _ in range(2)])
            er = Rot([c.sb([128, 256], F32, "er") for _ in range(4)])
            yh = Rot([c.sb([128, 256], BF16, "yh") for _ in range(3)])
            Sf = c.sb([128, 256], F32, "Sf"); Sb = c.sb([128, 256], BF16, "Sb")
            PP = Rot([c.ps([128, 512], F32, "pp") for _ in range(2)])
            PS_ = Rot([c.ps([128, 512], F32, "pss") for _ in range(1)])
            PO = Rot([c.ps([128, 512], F32, "pgo") for _ in range(2)])
            PC = c.ps([128, 512], F32, "pc")
            PTr = c.ps([128, 8, 128], BF16, "ptr")
            PT2 = c.ps([128, 8, 128], BF16, "pt2")
            for tq in range(4):
                pb = PP.next()
                proj_fm(pb, wa, lambda k: wa.t[:, k, :], tq)
                c.op("dve", lambda e: e.tensor_copy(out=alT.t[:, tq * 512:(tq + 1) * 512], in_=pb.t[0:16, :]), [pb], [alT])
            for h in range(4):
                load_w(wq, wq.t[:], win[:, h * 128:(h + 1) * 128])
                load_w(wk, wk.t[:], win[:, 512 + h * 128:512 + (h + 1) * 128])
                load_w(wv, wv.t[:], win[:, 1024 + h * 256:1024 + (h + 1) * 256])
                load_w(wr, wr.t[:], win[:, 2048 + h * 256:2048 + (h + 1) * 256])
                for tq in range(4):
                    ts_ = slice(tq * 512, (tq + 1) * 512)
                    pb = PP.next()
                    c.op("pe", lambda e: e.matmul(pb.t[:], lhsT=wa2.t[:, h * 128:(h + 1) * 128], rhs=alT.t[:, ts_], start=True, stop=True),
                         [wa2, alT], [pb])
                    te = tE.next()
                    c.op("act", lambda e: e.activation(out=te.t[:], in_=pb.t[:], func=AF.Exp, bias=nba.t[:, h:h + 1], scale=-1.0), [pb, nba], [te])
                    c.op("act", lambda e: e.activation(out=sp_.t[:, ts_], in_=te.t[:], func=AF.Ln, bias=1.0), [te], [sp_])
                c.op("dve", lambda e: e.tensor_tensor_scan(out=bp.t[:], data0=rm.t[:], data1=sp_.t[:], initial=0.0, op0=ALU.mult, op1=ALU.add),
                     [rm, sp_], [bp])
                c.op("act", lambda e: e.activation(out=ebl.t[:], in_=bp.t[:].rearrange("p (c t) -> p c t", t=128)[:, :, 127], func=AF.Exp,
                                                   scale=-1.0 / 16), [bp], [ebl])
                for tq in range(4):
                    ts_ = slice(tq * 512, (tq + 1) * 512)
                    pb = PP.next()
                    proj_fm(pb, wq, lambda k: wq.t[:, k, :], tq)
                    te = tE.next()
                    c.op("act", lambda e: e.activation(out=te.t[:], in_=bp.t[:, ts_], func=AF.Exp, scale=-1.0 / 16), [bp], [te])
                    c.op("dve", lambda e: e.scalar_tensor_tensor(out=qtl.t[:, ts_], in0=pb.t[:], scalar=128.0 ** -0.5, in1=te.t[:],
                                                                 op0=ALU.mult, op1=ALU.mult), [pb, te], [qtl])
                    pb = PP.next()
                    proj_fm(pb, wk, lambda k: wk.t[:, k, :], tq)
                    te = tE.next()
                    c.op("act", lambda e: e.activation(out=te.t[:], in_=bp.t[:, ts_], func=AF.Exp, scale=1.0 / 16), [bp], [te])
                    c.op("dve", lambda e: e.tensor_tensor(out=ktl.t[:, ts_], in0=pb.t[:], in1=te.t[:], op=ALU.mult), [pb, te], [ktl])
                def gS1(cc):
                    q_, j_ = divmod(cc, 4)
                    sl = slice(cc * 128, (cc + 1) * 128)
                    hsl = lambda k: HT[q_].t[:, k, j_ * 128:(j_ + 1) * 128]
                    kt_ = khT.next(); k_ = kh.next()
                    c.op("dve", lambda e: e.tensor_scalar(out=kt_.t[:], in0=ktl.t[:, sl], scalar1=ebl.t[:, cc:cc + 1], scalar2=None, op0=ALU.mult),
                         [ktl, ebl], [kt_])
                    ps = PS_.next()
                    c.op("pe", lambda e: e.matmul(ps.t[:, 0:128], lhsT=ktl.t[:, sl], rhs=qtl.t[:, sl], start=True, stop=True), [ktl, qtl], [ps])
                    at = AT.next()
                    c.op("dve", lambda e: e.tensor_tensor(out=at.t[:], in0=ps.t[:, 0:128], in1=maskLE, op=ALU.mult), [ps, CON], [at])
                    pb = PP.next()
                    for k in range(8):
                        c.op("pe", lambda e: e.matmul(pb.t[:, 0:256], lhsT=hsl(k), rhs=wv.t[:, k, :], start=(k == 0), stop=(k == 7)),
                             [wv, HT[q_]], [pb])
                    vc = Vc.next()
                    c.op("act", lambda e: e.activation(out=vc.t[:], in_=pb.t[:, 0:256], func=AF.Copy), [pb], [vc])
                    c.op("pe", lambda e: e.transpose(out=PT2.t[:, 0, :], in_=kt_.t[:], identity=ident), [kt_, CON], [PT2])
                    c.op("act", lambda e: e.activation(out=k_.t[:], in_=PT2.t[:, 0, :], func=AF.Copy), [PT2], [k_])
                    pr = PP.next()
                    for k in range(8):
                        c.op("pe", lambda e: e.matmul(pr.t[:, 0:256], lhsT=hsl(k), rhs=wr.t[:, k, :], start=(k == 0), stop=(k == 7)),
                             [wr, HT[q_]], [pr])
                    e_ = er.next()
                    c.op("act", lambda e: e.activation(out=e_.t[:], in_=pr.t[:, 0:256], func=AF.Tanh, scale=0.5), [pr], [e_])
                    c.op("dve", lambda e: e.scalar_tensor_tensor(out=e_.t[:], in0=e_.t[:], scalar=1.0, in1=pr.t[:, 0:256], op0=ALU.add, op1=ALU.mult),
                         [e_, pr], [e_])
                    c.op("pool", lambda e: e.tensor_tensor(out=e_.t[:], in0=e_.t[:], in1=gB.t[:], op=ALU.mult), [e_, gB], [e_])
                    return dict(vc=vc, at=at, e=e_, k=k_)

                def gFlush(prev_y):
                    pcc, py_ = prev_y
                    for j2 in range(2):
                        c.op("pe", lambda e: e.transpose(out=PTr.t[:, j2, :], in_=py_.t[:, j2 * 128:(j2 + 1) * 128], identity=ident), [py_, CON], [PTr])
                    for j2 in range(2):
                        c.op("act", lambda e: e.activation(out=YT[2 * h + j2].t[:, pcc * 128:(pcc + 1) * 128], in_=PTr.t[:, j2, :], func=AF.Copy),
                             [PTr], [YT[2 * h + j2]])

                def gS2(cc, d, prev_y):
                    sl = slice(cc * 128, (cc + 1) * 128)
                    vc, at, e_, k_ = d["vc"], d["at"], d["e"], d["k"]
                    po = PO.next()
                    if cc > 0:
                        c.op("pe", lambda e: e.matmul(po.t[:, 0:256], lhsT=qtl.t[:, sl], rhs=Sb.t[:], start=True, stop=False), [qtl, Sb], [po])
                    c.op("pe", lambda e: e.matmul(PC.t[:, 0:256], lhsT=k_.t[:], rhs=vc.t[:], start=True, stop=True), [k_, vc], [PC])
                    c.op("pe", lambda e: e.matmul(po.t[:, 0:256], lhsT=at.t[:], rhs=vc.t[:], start=(cc == 0), stop=True), [at, vc], [po])
                    if prev_y is not None:
                        gFlush(prev_y)
                    if cc > 0:
                        c.op("dve", lambda e: e.scalar_tensor_tensor(out=Sf.t[:], in0=Sf.t[:], scalar=ebl.t[:, cc:cc + 1], in1=PC.t[:, 0:256],
                                                                     op0=ALU.mult, op1=ALU.add), [Sf, ebl, PC], [Sf])
                    else:
                        c.op("dve", lambda e: e.tensor_copy(out=Sf.t[:], in_=PC.t[:, 0:256]), [PC], [Sf])
                    m_ = sm.next()
                    c.op("act", lambda e: e.activation(out=junk.t[:], in_=po.t[:, 0:256], func=AF.Square, accum_out=m_.t[:, 0:1]), [po], [junk, m_])
                    c.op("act", lambda e: e.activation(out=Sb.t[:], in_=Sf.t[:], func=AF.Copy), [Sf], [Sb])
                    c.op("act", lambda e: e.activation(out=m_.t[:, 1:2], in_=m_.t[:, 0:1], func=AF.Ln, bias=eps.t[:, 0:1], scale=1.0 / 256),
                         [m_, eps], [m_])
                    c.op("act", lambda e: e.activation(out=m_.t[:, 2:3], in_=m_.t[:, 1:2], func=AF.Exp, scale=-0.5), [m_], [m_])
                    y_ = yh.next()
                    c.op("dve", lambda e: e.scalar_tensor_tensor(out=y_.t[:], in0=po.t[:, 0:256], scalar=m_.t[:, 2:3], in1=e_.t[:],
                                                                 op0=ALU.mult, op1=ALU.mult), [po, m_, e_], [y_])
                    return (cc, y_)

                dd = {}; prev_y = None
                for n in range(16 + 2):
                    if n < 16:
                        dd[n] = gS1(n)
                    if 0 <= n - 2 < 16:
                        prev_y = gS2(n - 2, dd.pop(n - 2), prev_y)
                gFlush(prev_y)
            c.pop()
            return YT

        def xattn(layer):
            c.push()
            OT = [c.sb([128, S], BF16, f"OT{k}") for k in range(8)]
            c.push()
            wq = [wslot([128, 8, 256], f"w{j}") for j in range(2)]
            wk = [wslot([128, 8, 256], f"w{2 + j}") for j in range(2)]
            wv = [wslot([128, 8, 256], f"w{4 + j}") for j in range(2)]
            qTs = [c.sb([128, 2, S], BF16, "xqT") for _ in range(2)]
            kTs = [c.sb([128, 2, 256], BF16, "xkT") for _ in range(2)]
            Vas = [c.sb([128, 2, 257], BF16, "xV") for _ in range(2)]
            PT = [Rot([c.sb([128, 512], BF16, "xPT") for _ in range(2)]) for _ in range(2)]
            Ot = Rot([c.sb([128, 256], BF16, "xO") for _ in range(3)])
            rd = Rot([c.sb([128, 1], F32, "xrd") for _ in range(3)])
            PP = Rot([c.ps([128, 512], F32, "pp") for _ in range(2)])
            PS_ = Rot([c.ps([128, 512], F32, "psc") for _ in range(2)])
            PO = Rot([c.ps([128, 512], F32, "pxo") for _ in range(2)])
            PTr = Rot([c.ps([128, 8, 128], BF16, "ptr") for _ in range(2)])
            for Va_ in Vas:
                c.op("dve", lambda e: e.memset(Va_.t[:, :, 256:257], 1.0), [], [Va_])
            wkv = Dm["xa_wkv"][layer]

            def load_head(h):
                b = h % 2
                load_w(wq[b], wq[b].t[:], Dm["xa_wq"][layer][:, h * 256:(h + 1) * 256])
                load_w(wk[b], wk[b].t[:], wkv[:, h * 256:(h + 1) * 256])
                load_w(wv[b], wv[b].t[:], wkv[:, 1024 + h * 256:1024 + (h + 1) * 256])

            def xproj_items(h):
                bb = h % 2
                items = []
                for dc in range(2):
                    def fk(dc=dc):
                        pb = PP.next()
                        for k in range(8):
                            c.op("pe", lambda e: e.matmul(pb.t[:, 0:256], lhsT=wk[bb].t[:, k, dc * 128:(dc + 1) * 128], rhs=memT.t[:, k, :],
                                                          start=(k == 0), stop=(k == 7)), [wk[bb], memT], [pb])
                        c.op("dve", lambda e: e.tensor_copy(out=kTs[bb].t[:, dc, :], in_=pb.t[:, 0:256]), [pb], [kTs[bb]])
                    items.append(fk)
                for mt in range(2):
                    def fv(mt=mt):
                        pb = PP.next()
                        for k in range(8):
                            c.op("pe", lambda e: e.matmul(pb.t[:, 0:256], lhsT=memT.t[:, k, mt * 128:(mt + 1) * 128], rhs=wv[bb].t[:, k, :],
                                                          start=(k == 0), stop=(k == 7)), [wv[bb], memT], [pb])
                        c.op("dve", lambda e: e.tensor_copy(out=Vas[bb].t[:, mt, 0:256], in_=pb.t[:, 0:256]), [pb], [Vas[bb]])
                    items.append(fv)
                for dc in range(2):
                    for tq in range(4):
                        def fq(dc=dc, tq=tq):
                            pb = PP.next()
                            proj_fm(pb, wq[bb], lambda k: wq[bb].t[:, k, dc * 128:(dc + 1) * 128], tq)
                            c.op("act", lambda e: e.activation(out=qTs[bb].t[:, dc, tq * 512:(tq + 1) * 512], in_=pb.t[:], func=AF.Copy, scale=1.0 / 16),
                                 [pb], [qTs[bb]])
                        items.append(fq)
                return items

            load_head(0)
            load_head(1)
            for it in xproj_items(0):
                it()
            for h in range(4):
                b = h % 2
                if 1 <= h and h + 1 < 4:
                    load_head(h + 1)
                nxt = xproj_items(h + 1) if h + 1 < 4 else []
                qT, kT, Va = qTs[b], kTs[b], Vas[b]
                def xP(tq):
                    pts = []
                    for mt in range(2):
                        psc = PS_.next()
                        for dc in range(2):
                            c.op("pe", lambda e: e.matmul(psc.t[:], lhsT=kT.t[:, dc, mt * 128:(mt + 1) * 128],
                                                          rhs=qT.t[:, dc, tq * 512:(tq + 1) * 512], start=(dc == 0), stop=(dc == 1)),
                                 [kT, qT], [psc])
                        pt = PT[mt].next()
                        c.op("act", lambda e: e.activation(out=pt.t[:], in_=psc.t[:], func=AF.Exp), [psc], [pt])
                        pts.append(pt)
                    return pts

                def xV(i, pts):
                    jt = i % 4
                    po = PO.next()
                    for mt in range(2):
                        c.op("pe", lambda e: e.matmul(po.t[:, 0:257], lhsT=pts[mt].t[:, jt * 128:(jt + 1) * 128], rhs=Va.t[:, mt, :],
                                                      start=(mt == 0), stop=(mt == 1)), [pts[mt], Va], [po])
                    r_ = rd.next(); o_ = Ot.next()
                    c.op("dve", lambda e: e.reciprocal(out=r_.t[:], in_=po.t[:, 256:257]), [po], [r_])
                    c.op("act", lambda e: e.activation(out=o_.t[:], in_=po.t[:, 0:256], func=AF.Copy, scale=r_.t[:, 0:1]), [po, r_], [o_])
                    return o_

                def xT(i, o_):
                    ptr = PTr.next()
                    for j2 in range(2):
                        c.op("pe", lambda e: e.transpose(out=ptr.t[:, j2, :], in_=o_.t[:, j2 * 128:(j2 + 1) * 128], identity=ident),
                             [o_, CON], [ptr])
                    for j2 in range(2):
                        c.op("dve", lambda e: e.tensor_copy(out=OT[2 * h + j2].t[:, i * 128:(i + 1) * 128], in_=ptr.t[:, j2, :]),
                             [ptr], [OT[2 * h + j2]])

                ptsl = {0: xP(0)}
                prev = None
                for tq in range(4):
                    if tq + 1 < 4:
                        ptsl[tq + 1] = xP(tq + 1)
                    for jt in range(4):
                        i = tq * 4 + jt
                        o_ = xV(i, ptsl[tq])
                        if prev is not None:
                            xT(*prev)
                        prev = (i, o_)
                        if i >= 2 and nxt:
                            nxt.pop(0)()
                xT(*prev)
                for it in nxt:
                    it()
            c.pop()
            out_proj_ln(OT, Dm["xa_wo"][layer], layer * 3 + 1)
            c.pop()

        def ffn(layer):
            c.push()
            groups = [list(range(g, min(g + 4, NCH))) for g in range(0, NCH, 4)]
            wup = Dm["ffn_up"][layer]
            wdn = Dm["ffn_down"][layer]
            cw = c.sb([128, 44 * 3], F32, "cw"); cb = c.sb([128, 44], F32, "cb")
            c.dma("sp", cw.t[:], Dm["ffn_conv"][layer], writes=[cw], key="cw")
            c.dma("sp", cb.t[:], Dm["ffn_conv_b"][layer], writes=[cb], key="cb")
            act = [c.sb([128, S], BF16, f"act{j}") for j in range(4)]
            wd = [wslot([128, 4, D], f"w{j}") for j in range(2)]
            wg = [wslot([128, 8, 128], f"w{2 + j}") for j in range(2)]
            wv = [wslot([128, 8, 128], f"w{4 + j}") for j in range(2)]
            ug = c.sb([128, S + 2], F32, "ug"); uv = c.sb([128, S + 2], F32, "uv")
            cg = c.sb([128, S], F32, "cg"); cv = c.sb([128, S], F32, "cv")
            gg = c.sb([128, S], F32, "gg")
            PU = Rot([c.ps([128, 512], F32, "pu") for _ in range(4)])
            PD = Rot([c.ps([128, 512], F32, "pd") for _ in range(4)])
            c.op("dve", lambda e: e.memset(ug.t[:, 0:2], 0.0), [], [ug])
            c.op("dve", lambda e: e.memset(uv.t[:, 0:2], 0.0), [], [uv])

            def load_up(j):
                b = j % 2
                load_w(wg[b], wg[b].t[:], wup[:, j * 128:(j + 1) * 128])
                load_w(wv[b], wv[b].t[:], wup[:, DFF + j * 128:DFF + (j + 1) * 128])

            def conv_part(u, w, ch, dst):
                c.op("dve", lambda e: e.scalar_tensor_tensor(out=dst.t[:], in0=u.t[:, 1:S + 1], scalar=cw.t[:, ch * 3 + 1:ch * 3 + 2],
                                                             in1=dst.t[:], op0=ALU.mult, op1=ALU.add), [u, cw, dst], [dst])
                c.op("dve", lambda e: e.scalar_tensor_tensor(out=dst.t[:], in0=u.t[:, 0:S], scalar=cw.t[:, ch * 3:ch * 3 + 1],
                                                             in1=dst.t[:], op0=ALU.mult, op1=ALU.add), [u, cw, dst], [dst])

            load_up(0)
            for gi, grp in enumerate(groups):
                wdb = wd[gi % 2]
                load_w(wdb, wdb.t[:, 0:len(grp), :], wdn[grp[0] * 128:(grp[-1] + 1) * 128, :])
                for jj, j in enumerate(grp):
                    b = j % 2
                    if j + 1 < NCH:
                        load_up(j + 1)
                    for (w_, u_, cdst, ch) in ((wg[b], ug, cg, j), (wv[b], uv, cv, NCH + j)):
                        for tq in range(4):
                            pb = PU.next()
                            proj_fm(pb, w_, lambda k: w_.t[:, k, :], tq)
                            c.op("act", lambda e: e.activation(out=u_.t[:, 2 + tq * 512:2 + (tq + 1) * 512], in_=pb.t[:], func=AF.Copy),
                                 [pb], [u_])
                            c.op("act", lambda e: e.activation(out=cdst.t[:, tq * 512:(tq + 1) * 512], in_=pb.t[:], func=AF.Identity,
                                                               bias=cb.t[:, ch:ch + 1], scale=cw.t[:, ch * 3 + 2:ch * 3 + 3]),
                                 [pb, cb, cw], [cdst])
                        conv_part(u_, w_, ch, cdst)
                    c.op("act", lambda e: e.activation(out=gg.t[:], in_=cg.t[:], func=AF.Gelu_apprx_tanh), [cg], [gg])
                    c.op("pool", lambda e: e.tensor_tensor(out=act[jj].t[:], in0=gg.t[:], in1=cv.t[:], op=ALU.mult), [gg, cv], [act[jj]])
                for i in range(NT):
                    for half in range(2):
                        pd = PD.next()
                        for jj in range(len(grp)):
                            c.op("pe", lambda e: e.matmul(pd.t[:], lhsT=act[jj].t[:, i * 128:(i + 1) * 128],
                                                          rhs=wdb.t[:, jj, half * 512:(half + 1) * 512],
                                                          start=(jj == 0), stop=(jj == len(grp) - 1)), [act[jj], wdb], [pd])
                        hs = H[i].t[:, half * 512:(half + 1) * 512]
                        if gi == 0:
                            c.op("dve", lambda e: e.scalar_tensor_tensor(out=hs, in0=hs, scalar=ALPHA, in1=pd.t[:], op0=ALU.mult, op1=ALU.add),
                                 [H[i], pd], [H[i]])
                        else:
                            c.op("dve", lambda e: e.tensor_tensor(out=hs, in0=hs, in1=pd.t[:], op=ALU.add), [H[i], pd], [H[i]])
            c.pop()
            c.push()
            R, gB, bB = ln_res(layer * 3 + 2)
            hbs = {}
            for n in range(NT + 1):
                if n < NT: hbs[n] = ln_tile(n, H[n], gB, bB, R)
                if 0 <= n - 1 < NT: ht_from_hb(n - 1, hbs[n - 1], R["pT"])
            c.pop()

        c.push()
        hbR = Rot([c.sb([128, D], BF16, "hb") for _ in range(2)])
        pTR = Rot([c.ps([128, 8, 128], BF16, "pT") for _ in range(2)])
        for i in range(NT):
            to_ht(i, hbR, pTR)
        mf = c.sb([128, D], F32, "mf")
        for mt in range(2):
            c.dma("sp", mf.t[:], Dm["mem"][mt * 128:(mt + 1) * 128, :], writes=[mf], key="mem")
            hb = hbR.next()
            c.op("act", lambda e: e.activation(out=hb.t[:], in_=mf.t[:], func=AF.Copy), [mf], [hb])
            pT = pTR.next()
            for k in range(8):
                c.op("pe", lambda e: e.transpose(out=pT.t[:, k, :], in_=hb.t[:, k * 128:(k + 1) * 128], identity=ident), [hb, CON], [pT])
            c.op("dve", lambda e: e.tensor_copy(out=memT.t[:, :, mt * 128:(mt + 1) * 128], in_=pT.t[:]), [pT], [memT])
        c.pop()

        def finish():
            for i in range(NT):
                c.dma("sp", out[i * 128:(i + 1) * 128, :], H[i].t[:], reads=[H[i]], key="out")
            c.barrier()

        done = False
        for layer in range(first, nlayers):
            kind = layer % 4
            if kind == 0:
                YT = mixer_sb(Dm["sb_win"])
            elif kind == 1:
                YT = mixer_fox(Dm["fox_win"], Dm["fox_bf"])
            elif kind == 2:
                YT = mixer_mlstm(Dm["ml_win"])
            else:
                YT = mixer_gla(Dm["gla_win"])
            out_proj_ln(YT, Dm["mix_wo"][layer], layer * 3 + 0)
            c.pop()
            if stop == (layer, "a"):
                break
            xattn(layer)
            if stop == (layer, "b"):
                break
            ffn(layer)
        finish()
    return nc


_NC_CACHE = {}


def prep_inputs(inputs):
    con, con4, rmask = make_consts()
    shared = {
        "ln_g": np.ascontiguousarray(inputs["ln_g"].reshape(12, D)),
        "ln_b": np.ascontiguousarray(inputs["ln_b"].reshape(12, D)),
        "mix_wo": inputs["mix_wo"], "sb_win": inputs["sb_win"][0], "fox_win": inputs["fox_win"][0],
        "fox_bf": inputs["fox_bf"].reshape(1, 16),
        "ml_win": inputs["ml_win"][0], "ml_bi": inputs["ml_bi"].reshape(4, 1), "ml_bf": inputs["ml_bf"].reshape(4, 1),
        "gla_win": inputs["gla_win"][0], "gla_wa2": inputs["gla_wa2"][0],
        "gla_ba": np.ascontiguousarray(inputs["gla_ba"].reshape(4, 128).T),
        "gla_norm_g": inputs["gla_norm_g"].reshape(1, 256),
        "xa_wq": inputs["xa_wq"], "xa_wkv": inputs["xa_wkv"], "xa_wo": inputs["xa_wo"],
        "ffn_up": inputs["ffn_up"],
        "ffn_conv": np.ascontiguousarray(inputs["ffn_conv"].reshape(4, 3, 44, 128).transpose(0, 3, 2, 1).reshape(4, 128, 132)),
        "ffn_conv_b": np.ascontiguousarray(inputs["ffn_conv_b"].reshape(4, 44, 128).transpose(0, 2, 1)),
        "ffn_down": inputs["ffn_down"],
        "con": con, "con4": con4, "rmask": rmask,
    }
    shared = {k: np.ascontiguousarray(v, dtype=np.float32) for k, v in shared.items()}
    return shared


def kernel(**inputs):
    inputs = {k: np.asarray(v) for k, v in inputs.items()}
    shared = prep_inputs(inputs)
    if "nc" not in _NC_CACHE:
        _NC_CACHE["nc"] = build()
    nc = _NC_CACHE["nc"]
    in_maps = []
    for b in range(8):
        m = dict(shared)
        m["x"] = np.ascontiguousarray(inputs["x"][b], dtype=np.float32)
        m["mem"] = np.ascontiguousarray(inputs["mem"][b], dtype=np.float32)
        in_maps.append(m)
    res = run_bass_kernel_spmd(nc, in_maps, core_ids=list(range(8)))
    return np.stack([np.asarray(r["out"], dtype=np.float32) for r in res.results], axis=0)
```

```python
import numpy as np
import concourse.bass as bass
import concourse.mybir as mybir
from concourse.bass_utils import run_bass_kernel_spmd
from contextlib import ExitStack

F32 = mybir.dt.float32
BF16 = mybir.dt.bfloat16
AF = mybir.ActivationFunctionType
ALU = mybir.AluOpType
AX = mybir.AxisListType

S = 2048
D = 1024
NT = 16
ALPHA = 8.0 ** 0.25
LN_EPS = 1e-5
DFF = 2816
NCH = 22


class Eng:
    def __init__(s, name, h, sem):
        s.name, s.h, s.sem, s.cnt, s.seen = name, h, sem, 0, {}


class DSem:
    def __init__(s, key, sem):
        s.key, s.sem, s.cnt = key, sem, 0


class TT:
    def __init__(s, t, name):
        s.t, s.name, s.w, s.r = t, name, None, {}


class Ctx:
    def __init__(s, nc, es):
        s.nc = nc
        s.stacks = [es]
        s.E = {}
        for name, h in (("pe", nc.tensor), ("act", nc.scalar), ("dve", nc.vector),
                        ("pool", nc.gpsimd), ("sp", nc.sync)):
            s.E[name] = Eng(name, h, es.enter_context(nc.semaphore("s_" + name)))
        s.dsems = {}
        s.nalloc = 0
        s.same_eng_sync = True
        for key in ["con", "x", "cf", "bf", "bi", "lng", "lnb", "mem", "cw", "cb", "out"] + [f"w{j}" for j in range(11)]:
            s.dsem(key)
        for E in s.E.values():
            nc.gpsimd.sem_clear(E.sem)
        for ds in s.dsems.values():
            nc.gpsimd.sem_clear(ds.sem)
        nc.all_engine_barrier()

    def push(s):
        st = ExitStack()
        st.__enter__()
        s.stacks.append(st)

    def pop(s):
        s.barrier()
        st = s.stacks.pop()
        st.__exit__(None, None, None)

    def sb(s, shape, dt, name="t"):
        s.nalloc += 1
        name = f"{name}_{s.nalloc}"
        return TT(s.stacks[-1].enter_context(s.nc.sbuf_tensor(name, list(shape), dt)), name)

    def ps(s, shape, dt, name="p"):
        s.nalloc += 1
        name = f"{name}_{s.nalloc}"
        t = TT(s.stacks[-1].enter_context(s.nc.psum_tensor(name, list(shape), dt)), name)
        t.psum = True
        return t

    def dsem(s, key):
        if key not in s.dsems:
            s.dsems[key] = DSem("dma_" + key, s.stacks[0].enter_context(s.nc.semaphore("d_" + key)))
        return s.dsems[key]

    def _wait(s, E, key, sem, val):
        if E.seen.get(key, 0) >= val:
            return
        E.h.wait_ge(sem, val)
        E.seen[key] = val

    def _waits(s, E, reads, writes):
        deps = []
        for t in reads:
            if t.w: deps.append(t.w)
            if getattr(t, "psum", False):
                deps.extend(t.r.values())
        for t in writes:
            if t.w: deps.append(t.w)
            deps.extend(t.r.values())
        for (key, sem, val, ds) in deps:
            if key == E.name and (E.name == "pe" or not s.same_eng_sync):
                continue
            if ds is not None:
                val = max(val, ds.cnt)
            s._wait(E, key, sem, val)

    def op(s, eng, fn, reads=(), writes=()):
        E = s.E[eng]
        s._waits(E, reads, writes)
        ins = fn(E.h)
        E.cnt += 1
        ins.then_inc(E.sem, 1)
        tok = (E.name, E.sem, E.cnt, None)
        for t in reads: t.r[E.name] = tok
        for t in writes:
            t.w = tok; t.r = {}
        return ins

    def dma(s, q, out, in_, reads=(), writes=(), key="g"):
        E = s.E[q]
        s._waits(E, reads, writes)
        ds = s.dsem(key)
        ins = E.h.dma_start(out=out, in_=in_)
        ds.cnt += 16
        ins.then_inc(ds.sem, 16)
        tok = (ds.key, ds.sem, ds.cnt, ds)
        for t in reads: t.r[ds.key] = tok
        for t in writes:
            t.w = tok; t.r = {}
        return ins

    def barrier(s):
        engs = list(s.E.values())
        for E in engs:
            for F in engs:
                if F.cnt > 0:
                    s._wait(E, F.name, F.sem, F.cnt)
            for ds in s.dsems.values():
                if ds.cnt > 0:
                    s._wait(E, ds.key, ds.sem, ds.cnt)


class Rot:
    def __init__(s, items):
        s.items, s.i = list(items), 0

    def next(s):
        x = s.items[s.i % len(s.items)]
        s.i += 1
        return x


C_ID, C_LE, C_LT, C_UN, C_GT, NCON = 0, 128, 256, 2304, 2432, 2560


def make_consts():
    con = np.zeros((128, NCON), np.float32)
    p = np.arange(128)[:, None]
    f = np.arange(128)[None, :]
    con[:, C_ID:C_ID + 128] = (p == f)
    con[:, C_LE:C_LE + 128] = (p <= f)
    t = np.arange(512)[None, :]
    for o in range(4):
        con[:, C_LT + 512 * o:C_LT + 512 * (o + 1)] = ((o * 128 + p) < t)
    con[:, C_UN:C_UN + 128] = -1.0 * (p >= f)
    con[:, C_GT:C_GT + 128] = (p > f)
    con4 = np.zeros((4, 4 * 128 + 4 + 128), np.float32)
    for h in range(4):
        con4[h, h * 128:(h + 1) * 128] = 1.0
        con4[h, 512 + h] = 1.0
    con4[:, 516:644] = 1.0
    rmask = np.ones((128, S), np.float32)
    rmask[:, ::128] = 0.0
    return con, con4, rmask


def build(nlayers=4, stop=None, first=0):
    nc = bass.Bass("TRN2", target_bir_lowering=False)
    Dm = {}

    def din(name, shape):
        Dm[name] = nc.dram_tensor(name, list(shape), F32, kind="ExternalInput").ap()

    din("x", [S, D]); din("mem", [256, D]); din("ln_g", [12, D]); din("ln_b", [12, D])
    din("mix_wo", [4, D, D]); din("sb_win", [D, 3072]); din("fox_win", [D, 3088]); din("fox_bf", [1, 16])
    din("ml_win", [D, 3080]); din("ml_bi", [4, 1]); din("ml_bf", [4, 1])
    din("gla_win", [D, 3088]); din("gla_wa2", [16, 512]); din("gla_ba", [128, 4]); din("gla_norm_g", [1, 256])
    din("xa_wq", [4, D, D]); din("xa_wkv", [4, D, 2 * D]); din("xa_wo", [4, D, D])
    din("ffn_up", [4, D, 2 * DFF]); din("ffn_conv", [4, 128, 44 * 3]); din("ffn_conv_b", [4, 128, 44])
    din("ffn_down", [4, DFF, D])
    din("con", [128, NCON]); din("con4", [4, 644]); din("rmask", [128, S])
    out = nc.dram_tensor("out", [S, D], F32, kind="ExternalOutput").ap()

    with ExitStack() as es:
        c = Ctx(nc, es)
        H = [c.sb([128, D], F32, f"H{i}") for i in range(NT)]
        HT = [c.sb([128, 8, 512], BF16, f"HT{q}") for q in range(4)]
        memT = c.sb([128, 8, 256], BF16, "memT")
        CON = c.sb([128, NCON], BF16, "CON")
        ONEN = c.sb([128, 128], BF16, "ONEN")
        ident = CON.t[:, C_ID:C_ID + 128]
        maskLE = CON.t[:, C_LE:C_LE + 128]
        uneg = CON.t[:, C_UN:C_UN + 128]

        def maskLT(o):
            return CON.t[:, C_LT + 512 * o:C_LT + 512 * (o + 1)]

        c.dma("pool", CON.t[:], Dm["con"][:, :], writes=[CON], key="con")
        c.op("dve", lambda e: e.memset(ONEN.t[:], -1.0), [], [ONEN])
        for i in range(NT):
            c.dma("sp", H[i].t[:], Dm["x"][i * 128:(i + 1) * 128, :], writes=[H[i]], key="x")

        wcount = [0]

        def load_w(slot, ap_dst, src):
            wcount[0] += 1
            c.dma("pool", ap_dst, src.rearrange("(kc p) n -> p kc n", p=128), writes=[slot], key=slot.key)

        def wslot(shape, key, name="w"):
            t = c.sb(shape, BF16, name)
            t.key = key
            return t

        def to_ht(i, hb_rot, pT_rot):
            hb = hb_rot.next()
            c.op("act", lambda e: e.activation(out=hb.t[:], in_=H[i].t[:], func=AF.Copy), [H[i]], [hb])
            pT = pT_rot.next()
            for k in range(8):
                c.op("pe", lambda e: e.transpose(out=pT.t[:, k, :], in_=hb.t[:, k * 128:(k + 1) * 128], identity=ident),
                     [hb, CON], [pT])
            q, j = divmod(i, 4)
            c.op("dve", lambda e: e.tensor_copy(out=HT[q].t[:, :, j * 128:(j + 1) * 128], in_=pT.t[:]), [pT], [HT[q]])

        def ln_tile(i, Sx, gB, bB, R):
            st = R["st"].next(); mv = R["mv"].next(); sm = R["sm"].next(); T1 = R["T1"].next()
            for hh in range(2):
                c.op("dve", lambda e: e.bn_stats(out=st.t[:, hh, :], in_=Sx.t[:, hh * 512:(hh + 1) * 512]), [Sx], [st])
            c.op("dve", lambda e: e.bn_aggr(out=mv.t[:], in_=st.t[:]), [st], [mv])
            c.op("act", lambda e: e.activation(out=sm.t[:, 0:1], in_=mv.t[:, 1:2], func=AF.Ln, bias=R["eps"].t[:, 0:1]), [mv, R["eps"]], [sm])
            c.op("act", lambda e: e.activation(out=sm.t[:, 1:2], in_=sm.t[:, 0:1], func=AF.Exp, scale=-0.5), [sm], [sm])
            c.op("dve", lambda e: e.tensor_scalar(out=sm.t[:, 2:3], in0=mv.t[:, 0:1], scalar1=sm.t[:, 1:2], scalar2=-1.0,
                                                  op0=ALU.mult, op1=ALU.mult), [mv, sm], [sm])
            c.op("act", lambda e: e.activation(out=T1.t[:], in_=Sx.t[:], func=AF.Identity, bias=sm.t[:, 2:3], scale=sm.t[:, 1:2]),
                 [Sx, sm], [T1])
            c.op("pool", lambda e: e.tensor_tensor(out=T1.t[:], in0=T1.t[:], in1=gB.t[:], op=ALU.mult), [T1, gB], [T1])
            c.op("pool", lambda e: e.tensor_tensor(out=H[i].t[:], in0=T1.t[:], in1=bB.t[:], op=ALU.add), [T1, bB], [H[i]])
            hb = R["hb"].next()
            c.op("act", lambda e: e.activation(out=hb.t[:], in_=H[i].t[:], func=AF.Copy), [H[i]], [hb])
            return hb

        def ht_from_hb(i, hb, pT_rot):
            pT = pT_rot.next()
            for k in range(8):
                c.op("pe", lambda e: e.transpose(out=pT.t[:, k, :], in_=hb.t[:, k * 128:(k + 1) * 128], identity=ident),
                     [hb, CON], [pT])
            q, j = divmod(i, 4)
            c.op("dve", lambda e: e.tensor_copy(out=HT[q].t[:, :, j * 128:(j + 1) * 128], in_=pT.t[:]), [pT], [HT[q]])

        def ln_res(lnidx):
            R = {}
            R["st"] = Rot([c.sb([128, 2, 6], F32, "st") for _ in range(2)])
            R["mv"] = Rot([c.sb([128, 2], F32, "mv") for _ in range(2)])
            R["sm"] = Rot([c.sb([128, 4], F32, "sm") for _ in range(2)])
            R["T1"] = Rot([c.sb([128, D], F32, "T1") for _ in range(2)])
            R["hb"] = Rot([c.sb([128, D], BF16, "hb") for _ in range(3)])
            R["pT"] = Rot([c.ps([128, 8, 128], BF16, "pT") for _ in range(2)])
            R["eps"] = c.sb([128, 1], F32, "eps")
            c.op("dve", lambda e: e.memset(R["eps"].t[:], LN_EPS), [], [R["eps"]])
            gB = c.sb([128, D], F32, "gB"); bB = c.sb([128, D], F32, "bB")
            c.dma("sp", gB.t[:], Dm["ln_g"][lnidx:lnidx + 1, :].partition_broadcast(128), writes=[gB], key="lng")
            c.dma("sp", bB.t[:], Dm["ln_b"][lnidx:lnidx + 1, :].partition_broadcast(128), writes=[bB], key="lnb")
            return R, gB, bB

        def out_proj_ln(KT, w_dram, lnidx):
            c.push()
            wo = [wslot([128, 8, 512], f"w{j}") for j in range(2)]
            for j in range(2):
                load_w(wo[j], wo[j].t[:], w_dram[:, j * 512:(j + 1) * 512])
            R, gB, bB = ln_res(lnidx)
            PB = Rot([c.ps([128, 512], F32, "po") for _ in range(4)])
            SX = Rot([c.sb([128, D], F32, "SX") for _ in range(3)])
            sxs = {}; hbs = {}

            def sA(i):
                Sx = SX.next(); sxs[i] = Sx
                for j in range(2):
                    pb = PB.next()
                    for k in range(8):
                        c.op("pe", lambda e: e.matmul(pb.t[:], lhsT=KT[k].t[:, i * 128:(i + 1) * 128], rhs=wo[j].t[:, k, :],
                                                      start=(k == 0), stop=(k == 7)), [KT[k], wo[j]], [pb])
                    c.op("dve", lambda e: e.scalar_tensor_tensor(out=Sx.t[:, j * 512:(j + 1) * 512], in0=H[i].t[:, j * 512:(j + 1) * 512],
                                                                 scalar=ALPHA, in1=pb.t[:], op0=ALU.mult, op1=ALU.add),
                         [H[i], pb], [Sx])

            for n in range(NT + 2):
                if n < NT: sA(n)
                if 0 <= n - 1 < NT: hbs[n - 1] = ln_tile(n - 1, sxs[n - 1], gB, bB, R)
                if 0 <= n - 2 < NT: ht_from_hb(n - 2, hbs[n - 2], R["pT"])
            c.pop()

        def proj_fm(pb, w, wap_fn, tq):
            for k in range(8):
                c.op("pe", lambda e: e.matmul(pb_ap(pb, wap_fn(k)), lhsT=wap_fn(k), rhs=HT[tq].t[:, k, :], start=(k == 0), stop=(k == 7)),
                     [w, HT[tq]], [pb])

        def pb_ap(pb, lhsT):
            m = lhsT.shape[-1]
            return pb.t[0:m, :]

        def proj_tm(pb, out_ap, w, wap_fn, i):
            q, j = divmod(i, 4)
            for k in range(8):
                c.op("pe", lambda e: e.matmul(out_ap, lhsT=HT[q].t[:, k, j * 128:(j + 1) * 128], rhs=wap_fn(k), start=(k == 0), stop=(k == 7)),
                     [w, HT[q]], [pb])

        def mixer_sb(win):
            c.push()
            YT = [c.sb([128, S], BF16, f"YT{k}") for k in range(8)]
            c.push()
            wq = [wslot([128, 8, 128], f"w{j}") for j in range(2)]
            wk = [wslot([128, 8, 128], f"w{2 + j}") for j in range(2)]
            wv = [wslot([128, 8, 128], f"w{4 + j}") for j in range(2)]
            qT = [c.sb([128, 2, S], BF16, "qTz") for _ in range(2)]
            for q__ in qT:
                c.op("pool", lambda e: e.memset(q__.t[:], 0.0), [], [q__])
            kT = [c.sb([128, S], BF16, "kT") for _ in range(2)]
            V = [c.sb([128, NT, 128], BF16, "V") for _ in range(2)]
            Yp = c.sb([128, NT, 128], BF16, "Yp")
            SPt = Rot([c.sb([128, 512], F32, "SP") for _ in range(3)])
            LmP = Rot([c.sb([128, 512], BF16, "Lm") for _ in range(5)])
            Ssum = Rot([c.sb([128, 512], BF16, "Ss") for _ in range(5)])
            WT = Rot([c.sb([128, 512], BF16, "WT") for _ in range(4)])
            PZ = Rot([c.ps([128, 512], F32, "pz") for _ in range(4)])
            PY = Rot([c.ps([128, 512], F32, "py") for _ in range(2)])
            PP = Rot([c.ps([128, 512], F32, "pp") for _ in range(1)])
            PTr = Rot([c.ps([128, 8, 128], BF16, "ptr") for _ in range(1)])
            for t_ in SPt.items:
                c.op("dve", lambda e: e.memset(t_.t[:], 0.0), [], [t_])

            def load_pair(p):
                b = p % 2
                load_w(wq[b], wq[b].t[:], win[:, p * 128:(p + 1) * 128])
                load_w(wk[b], wk[b].t[:], win[:, 1024 + p * 128:1024 + (p + 1) * 128])
                load_w(wv[b], wv[b].t[:], win[:, 2048 + p * 128:2048 + (p + 1) * 128])

            def proj_items(p):
                bb = p % 2
                items = []
                for tq in range(4):
                    def fq(tq=tq):
                        pb = PP.next()
                        proj_fm(pb, wq[bb], lambda k: wq[bb].t[:, k, :], tq)
                        for hq in range(2):
                            c.op("act", lambda e: e.activation(out=qT[bb].t[64 * hq:64 * hq + 64, hq, tq * 512:(tq + 1) * 512],
                                                               in_=pb.t[64 * hq:64 * hq + 64, :], func=AF.Copy, scale=0.125), [pb], [qT[bb]])

                    def fk(tq=tq):
                        pb = PP.next()
                        proj_fm(pb, wk[bb], lambda k: wk[bb].t[:, k, :], tq)
                        c.op("dve", lambda e: e.tensor_copy(out=kT[bb].t[:, tq * 512:(tq + 1) * 512], in_=pb.t[:]), [pb], [kT[bb]])
                    items += [fq, fk]
                for i4 in range(4):
                    def fv(i4=i4):
                        pb = PP.next()
                        for jj in range(4):
                            proj_tm(pb, pb.t[:, jj * 128:(jj + 1) * 128], wv[bb], lambda k: wv[bb].t[:, k, :], i4 * 4 + jj)
                        c.op("act", lambda e: e.activation(out=V[bb].t[:, i4 * 4:(i4 + 1) * 4, :],
                                                           in_=pb.t[:].rearrange("p (a b) -> p a b", a=4), func=AF.Copy), [pb], [V[bb]])
                    items.append(fv)
                return items

            load_pair(0)
            load_pair(1)
            for it in proj_items(0):
                it()
            for p in range(8):
                b = p % 2
                if p + 2 < 8:
                    load_pair(p + 2)
                nxt = proj_items(p + 1) if p + 1 < 8 else []
                steps = []
                for hh in range(2):
                    for g in range(4):
                        grp = {"py": None, "first": True, "ss": None}
                        for idx, kb in enumerate(range(4 * g + 3, -1, -1)):
                            steps.append(dict(hh=hh, g=g, kb=kb, grp=grp))

                def stA(st):
                    hh, g, kb, grp = st["hh"], st["g"], st["kb"], st["grp"]
                    lo, hi = 64 * hh, 64 * hh + 64
                    o = kb - 4 * g
                    pz = PZ.next(); st["pz"] = pz
                    c0 = max(o, 0) * 128
                    c.op("pe", lambda e: e.matmul(pz.t[:, c0:512], lhsT=kT[b].t[:, kb * 128:(kb + 1) * 128],
                                                  rhs=qT[b].t[:, hh, g * 512 + c0:(g + 1) * 512], start=True, stop=False, skip_group_check=True),
                         [kT[b], qT[b]], [pz])
                    SP_ = SPt.next(); Lm = LmP.next()
                    c.op("act", lambda e: e.activation(out=SP_.t[:, c0:512], in_=pz.t[:, c0:512], func=AF.Exp), [pz], [SP_])
                    c.op("act", lambda e: e.activation(out=SP_.t[:, c0:512], in_=SP_.t[:, c0:512], func=AF.Ln, bias=1.0), [SP_], [SP_])
                    if o >= 0:
                        c.op("dve", lambda e: e.tensor_tensor(out=Lm.t[:], in0=SP_.t[:], in1=maskLT(o), op=ALU.mult), [SP_, CON], [Lm])
                    else:
                        c.op("dve", lambda e: e.tensor_copy(out=Lm.t[:], in_=SP_.t[:]), [SP_], [Lm])
                    st["Lm"] = Lm
                    st["ss_prev"] = grp["ss"]
                    if kb > 0:
                        if grp["ss"] is None:
                            grp["ss"] = Lm
                        else:
                            ss_new = Ssum.next()
                            sp0 = grp["ss"]
                            c.op("pool", lambda e: e.tensor_tensor(out=ss_new.t[:], in0=sp0.t[:], in1=Lm.t[:], op=ALU.add), [sp0, Lm], [ss_new])
                            grp["ss"] = ss_new

                def stB(st):
                    g, kb = st["g"], st["kb"]
                    o = kb - 4 * g
                    pz, Lm, ss_prev = st["pz"], st["Lm"], st["ss_prev"]
                    c0 = max(o, 0) * 128
                    c.op("pe", lambda e: e.matmul(pz.t[:, c0:512], lhsT=uneg, rhs=Lm.t[:, c0:512], start=False, stop=(ss_prev is None), skip_group_check=True),
                         [CON, Lm], [pz])
                    if ss_prev is not None:
                        c.op("pe", lambda e: e.matmul(pz.t[:, c0:512], lhsT=ONEN.t[:], rhs=ss_prev.t[:, c0:512], start=False, stop=True, skip_group_check=True),
                             [ONEN, ss_prev], [pz])
                    W_ = WT.next(); st["W"] = W_
                    c.op("act", lambda e: e.activation(out=W_.t[:, c0:512], in_=pz.t[:, c0:512], func=AF.Exp), [pz], [W_])
                    if o >= 0:
                        c.op("dve", lambda e: e.tensor_tensor(out=W_.t[:, c0:512], in0=W_.t[:, c0:512], in1=maskLT(o)[:, c0:512], op=ALU.mult), [W_, CON], [W_])

                def stC(st):
                    hh, g, kb, grp = st["hh"], st["g"], st["kb"], st["grp"]
                    lo, hi = 64 * hh, 64 * hh + 64
                    if grp["py"] is None:
                        grp["py"] = PY.next()
                    py = grp["py"]; W_ = st["W"]
                    for jq in range(4):
                        if 4 * g + jq < kb:
                            continue
                        c.op("pe", lambda e: e.matmul(py.t[:, jq * 64:(jq + 1) * 64], lhsT=W_.t[:, jq * 128:(jq + 1) * 128],
                                                      rhs=V[b].t[:, kb, lo:hi], start=grp["first"], stop=(kb == 0 and jq == 3),
                                                      skip_group_check=True), [W_, V[b]], [py])
                        grp["first"] = False
                    if kb == 0:
                        c.op("act", lambda e: e.activation(out=Yp.t[:, 4 * g:4 * g + 4, lo:hi],
                                                           in_=py.t[:, 0:256].rearrange("p (a b) -> p a b", a=4), func=AF.Copy), [py], [Yp])

                ns = len(steps)
                for n in range(ns + 3):
                    if n < ns: stA(steps[n])
                    if 0 <= n - 2 < ns: stB(steps[n - 2])
                    if 0 <= n - 3 < ns: stC(steps[n - 3])
                    if n % 6 == 3 and nxt:
                        nxt.pop(0)()
                for it in nxt:
                    it()
                for i2 in range(2):
                    ptr = PTr.next()
                    for jj in range(8):
                        i = i2 * 8 + jj
                        c.op("pe", lambda e: e.transpose(out=ptr.t[:, jj, :], in_=Yp.t[:, i, :], identity=ident), [Yp, CON], [ptr])
                    c.op("dve", lambda e: e.tensor_copy(out=YT[p].t[:, i2 * 1024:(i2 + 1) * 1024],
                                                        in_=ptr.t[:].rearrange("p a b -> p (a b)")), [ptr], [YT[p]])
            c.pop()
            return YT


        def mixer_fox(win, bf_dram):
            c.push()
            YT = [c.sb([128, S], BF16, f"YT{k}") for k in range(8)]
            c.push()
            wq = [wslot([128, 8, 128], f"w{j}") for j in range(2)]
            wk = [wslot([128, 8, 128], f"w{2 + j}") for j in range(2)]
            wv = [wslot([128, 8, 128], f"w{4 + j}") for j in range(2)]
            wf = wslot([128, 8, 16], "w6")
            qT = [c.sb([128, 2, S], BF16, "qTz") for _ in range(2)]
            for q__ in qT:
                c.op("pool", lambda e: e.memset(q__.t[:], 0.0), [], [q__])
            kT = [c.sb([128, S], BF16, "kT") for _ in range(2)]
            V = [c.sb([128, NT, 2, 65], BF16, "V") for _ in range(2)]
            Yp = c.sb([128, NT, 128], BF16, "Yp")
            WT = Rot([c.sb([128, 512], BF16, "WT") for _ in range(4)])
            Bh = Rot([c.sb([128, 8, 16], F32, "Bh") for _ in range(4)])
            RD = Rot([c.sb([128, 4, 1], F32, "rd") for _ in range(2)])
            lfc = c.sb([128, 256], F32, "lfc"); tmp = c.sb([128, 256], F32, "ftmp")
            T2 = c.sb([128, 256], F32, "T2"); PSc = c.sb([128, 256], F32, "PSc")
            ugt = c.sb([128, 128], F32, "ugt"); onef = c.sb([128, 128], F32, "onef")
            bfB = c.sb([128, 16], F32, "bfB")
            PZ = Rot([c.ps([128, 512], F32, "pz") for _ in range(4)])
            PY = Rot([c.ps([128, 512], F32, "py") for _ in range(2)])
            PP = Rot([c.ps([128, 512], F32, "pp") for _ in range(1)])
            PTr = Rot([c.ps([128, 8, 128], BF16, "ptr") for _ in range(1)])
            for b in range(2):
                c.op("dve", lambda e: e.memset(V[b].t[:, :, :, 64:65], 1.0), [], [V[b]])
            c.op("dve", lambda e: e.memset(onef.t[:], 1.0), [], [onef])
            c.dma("sp", ugt.t[:], Dm["con"][:, C_GT:C_GT + 128], writes=[ugt], key="cf")
            c.dma("sp", bfB.t[:], bf_dram[0:1, :].partition_broadcast(128), writes=[bfB], key="bf")
            load_w(wf, wf.t[:], win[:, 3072:3088])

            def load_pair(p):
                b = p % 2
                load_w(wq[b], wq[b].t[:], win[:, p * 128:(p + 1) * 128])
                load_w(wk[b], wk[b].t[:], win[:, 1024 + p * 128:1024 + (p + 1) * 128])
                load_w(wv[b], wv[b].t[:], win[:, 2048 + p * 128:2048 + (p + 1) * 128])

            load_pair(0)
            for i in range(NT):
                pb = PP.next()
                proj_tm(pb, pb.t[:, 0:16], wf, lambda k: wf.t[:, k, :], i)
                c.op("dve", lambda e: e.tensor_tensor(out=lfc.t[:, i * 16:(i + 1) * 16], in0=pb.t[:, 0:16], in1=bfB.t[:], op=ALU.add),
                     [pb, bfB], [lfc])
            c.op("act", lambda e: e.activation(out=tmp.t[:], in_=lfc.t[:], func=AF.Exp, scale=-1.0), [lfc], [tmp])
            c.op("act", lambda e: e.activation(out=tmp.t[:], in_=tmp.t[:], func=AF.Ln, bias=1.0), [tmp], [tmp])
            c.op("dve", lambda e: e.tensor_scalar(out=lfc.t[:], in0=tmp.t[:], scalar1=-1.0, scalar2=None, op0=ALU.mult), [tmp], [lfc])
            pb = PP.next()
            c.op("pe", lambda e: e.matmul(pb.t[:, 0:256], lhsT=onef.t[:], rhs=lfc.t[:], start=True, stop=True), [onef, lfc], [pb])
            c.op("dve", lambda e: e.tensor_copy(out=PSc.t[:, 0:16], in_=pb.t[:, 0:16]), [pb], [PSc])
            for jb in range(1, 16):
                c.op("dve", lambda e: e.tensor_tensor(out=PSc.t[:, jb * 16:(jb + 1) * 16], in0=PSc.t[:, (jb - 1) * 16:jb * 16],
                                                      in1=pb.t[:, jb * 16:(jb + 1) * 16], op=ALU.add), [pb, PSc], [PSc])
            pb = PP.next()
            c.op("pe", lambda e: e.matmul(pb.t[:, 0:256], lhsT=ugt.t[:], rhs=lfc.t[:], start=True, stop=True), [ugt, lfc], [pb])
            c.op("dve", lambda e: e.tensor_tensor(out=T2.t[:], in0=pb.t[:, 0:256], in1=PSc.t[:], op=ALU.subtract), [pb, PSc], [T2])
            T2v = T2.t[:].rearrange("p (a b) -> p a b", a=16)

            def proj_items(p):
                bb = p % 2
                items = []
                for tq in range(4):
                    def fq(tq=tq):
                        pb = PP.next()
                        proj_fm(pb, wq[bb], lambda k: wq[bb].t[:, k, :], tq)
                        for hq in range(2):
                            c.op("act", lambda e: e.activation(out=qT[bb].t[64 * hq:64 * hq + 64, hq, tq * 512:(tq + 1) * 512],
                                                               in_=pb.t[64 * hq:64 * hq + 64, :], func=AF.Copy, scale=0.125), [pb], [qT[bb]])

                    def fk(tq=tq):
                        pb = PP.next()
                        proj_fm(pb, wk[bb], lambda k: wk[bb].t[:, k, :], tq)
                        c.op("dve", lambda e: e.tensor_copy(out=kT[bb].t[:, tq * 512:(tq + 1) * 512], in_=pb.t[:]), [pb], [kT[bb]])
                    items += [fq, fk]
                for i4 in range(4):
                    def fv(i4=i4):
                        pb = PP.next()
                        for jj in range(4):
                            proj_tm(pb, pb.t[:, jj * 128:(jj + 1) * 128], wv[bb], lambda k: wv[bb].t[:, k, :], i4 * 4 + jj)
                        for hh in range(2):
                            c.op("act", lambda e: e.activation(out=V[bb].t[:, i4 * 4:(i4 + 1) * 4, hh, 0:64],
                                                               in_=pb.t[:].rearrange("p (a h d) -> p a h d", a=4, h=2)[:, :, hh, :], func=AF.Copy),
                                 [pb], [V[bb]])
                    items.append(fv)
                return items

            PSm = c.sb([128, 8, 16], F32, "PSm")
            PSv = PSc.t[:].rearrange("p (q two h) -> p q two h", two=2, h=16)
            c.op("dve", lambda e: e.tensor_tensor(out=PSm.t[:], in0=PSv[:, :, 0, :], in1=PSv[:, :, 1, :], op=ALU.add), [PSc], [PSm])
            c.op("dve", lambda e: e.tensor_scalar(out=PSm.t[:], in0=PSm.t[:], scalar1=0.5, scalar2=None, op0=ALU.mult), [PSm], [PSm])
            load_pair(1)
            for it in proj_items(0):
                it()
            for p in range(8):
                b = p % 2
                if p + 2 < 8:
                    load_pair(p + 2)
                nxt = proj_items(p + 1) if p + 1 < 8 else []
                steps = []
                for hh in range(2):
                    h = 2 * p + hh
                    B_ = Bh.next()
                    for pr in range(8):
                        c.op("dve", lambda e: e.tensor_scalar(out=B_.t[:, pr, :], in0=T2v[:, :, h], scalar1=PSm.t[:, pr, h:h + 1],
                                                              scalar2=None, op0=ALU.add), [T2, PSm], [B_])
                    for g in range(4):
                        grp = {"py": None, "first": True}
                        for kb in range(4 * g + 4):
                            steps.append(dict(hh=hh, g=g, kb=kb, grp=grp, B=B_))

                def fA(st):
                    hh, g, kb = st["hh"], st["g"], st["kb"]
                    lo, hi = 64 * hh, 64 * hh + 64
                    pz = PZ.next(); st["pz"] = pz
                    c.op("pe", lambda e: e.matmul(pz.t[:], lhsT=kT[b].t[:, kb * 128:(kb + 1) * 128],
                                                  rhs=qT[b].t[:, hh, g * 512:(g + 1) * 512], start=True, stop=True), [kT[b], qT[b]], [pz])

                def fB(st):
                    g, kb, pz, B_ = st["g"], st["kb"], st["pz"], st["B"]
                    W_ = WT.next(); st["W"] = W_
                    for p2 in range(2):
                        if 4 * g + 2 * p2 + 1 < kb:
                            continue
                        pr = 2 * g + p2
                        c.op("act", lambda e: e.activation(out=W_.t[:, p2 * 256:(p2 + 1) * 256], in_=pz.t[:, p2 * 256:(p2 + 1) * 256],
                                                           func=AF.Exp, bias=B_.t[:, pr, kb:kb + 1]), [pz, B_], [W_])
                    for jq in range(4):
                        Q = 4 * g + jq
                        if Q == kb:
                            c.op("pool", lambda e: e.tensor_tensor(out=W_.t[:, jq * 128:(jq + 1) * 128], in0=W_.t[:, jq * 128:(jq + 1) * 128],
                                                                   in1=maskLE, op=ALU.mult), [W_, CON], [W_])

                def fC(st):
                    hh, g, kb, grp, W_ = st["hh"], st["g"], st["kb"], st["grp"], st["W"]
                    lo, hi = 64 * hh, 64 * hh + 64
                    if grp["py"] is None:
                        grp["py"] = PY.next()
                    py = grp["py"]
                    for jq in range(4):
                        Q = 4 * g + jq
                        if Q < kb:
                            continue
                        c.op("pe", lambda e: e.matmul(py.t[:, jq * 65:jq * 65 + 65], lhsT=W_.t[:, jq * 128:(jq + 1) * 128],
                                                      rhs=V[b].t[:, kb, hh, :], start=grp["first"], stop=(kb == 4 * g + 3 and jq == 3),
                                                      skip_group_check=True), [W_, V[b]], [py])
                        grp["first"] = False
                    if kb == 4 * g + 3:
                        rd = RD.next()
                        pyv = py.t[:, 0:260].rearrange("p (a b) -> p a b", a=4)
                        c.op("dve", lambda e: e.reciprocal(out=rd.t[:], in_=pyv[:, :, 64:65]), [py], [rd])
                        for jq in range(4):
                            c.op("act", lambda e: e.activation(out=Yp.t[:, 4 * g + jq, lo:hi], in_=py.t[:, jq * 65:jq * 65 + 64], func=AF.Copy,
                                                               scale=rd.t[:, jq, :]), [py, rd], [Yp])

                ns = len(steps)
                for n in range(ns + 3):
                    if n < ns: fA(steps[n])
                    if 0 <= n - 2 < ns: fB(steps[n - 2])
                    if 0 <= n - 3 < ns: fC(steps[n - 3])
                    if n % 6 == 3 and nxt:
                        nxt.pop(0)()
                for it in nxt:
                    it()
                for i2 in range(2):
                    ptr = PTr.next()
                    for jj in range(8):
                        i = i2 * 8 + jj
                        c.op("pe", lambda e: e.transpose(out=ptr.t[:, jj, :], in_=Yp.t[:, i, :], identity=ident), [Yp, CON], [ptr])
                    c.op("dve", lambda e: e.tensor_copy(out=YT[p].t[:, i2 * 1024:(i2 + 1) * 1024],
                                                        in_=ptr.t[:].rearrange("p a b -> p (a b)")), [ptr], [YT[p]])
            c.pop()
            return YT


        def mixer_mlstm(win):
            c.push()
            YT = [c.sb([128, S], BF16, f"YT{k}") for k in range(8)]
            aT = c.sb([4, S], F32, "aT"); nG = c.sb([4, S], F32, "nG")
            cols = c.sb([128, 16, 16], F32, "cols")
            c4 = c.sb([4, 644], F32, "c4")
            c.dma("sp", c4.t[:], Dm["con4"][:, :], writes=[c4], key="cf")
            I4 = c4.t[:, 512:516]; ones4 = c4.t[:, 516:644]
            c.push()
            wi = wslot([128, 8, 4], "w6"); wf = wslot([128, 8, 4], "w7")
            load_w(wi, wi.t[:], win[:, 3072:3076]); load_w(wf, wf.t[:], win[:, 3076:3080])
            bi = c.sb([4, 1], F32, "bi"); bfv = c.sb([4, 1], F32, "bfv")
            c.dma("sp", bi.t[:], Dm["ml_bi"][:, :], writes=[bi], key="bi")
            c.dma("sp", bfv.t[:], Dm["ml_bf"][:, :], writes=[bfv], key="bf")
            c.op("dve", lambda e: e.tensor_scalar(out=bfv.t[:], in0=bfv.t[:], scalar1=-1.0, scalar2=None, op0=ALU.mult), [bfv], [bfv])
            iT = c.sb([4, S], F32, "iT"); t4 = c.sb([4, S], F32, "t4"); Fp = c.sb([4, S], F32, "Fp"); G_ = c.sb([4, S], F32, "G")
            GP = c.sb([4, 16, 3], F32, "GP"); Dg = c.sb([4, 16, 12], F32, "Dg")
            PP = Rot([c.ps([128, 512], F32, "pp") for _ in range(2)])
            PCo = c.ps([128, 512], F32, "pco")
            for tq in range(4):
                pb = PP.next()
                proj_fm(pb, wi, lambda k: wi.t[:, k, :], tq)
                c.op("act", lambda e: e.activation(out=iT.t[:, tq * 512:(tq + 1) * 512], in_=pb.t[0:4, :], func=AF.Identity, bias=bi.t[:, 0:1]),
                     [pb, bi], [iT])
                pb = PP.next()
                proj_fm(pb, wf, lambda k: wf.t[:, k, :], tq)
                c.op("act", lambda e: e.activation(out=t4.t[:, tq * 512:(tq + 1) * 512], in_=pb.t[0:4, :], func=AF.Exp, bias=bfv.t[:, 0:1], scale=-1.0),
                     [pb, bfv], [t4])
            c.op("act", lambda e: e.activation(out=t4.t[:], in_=t4.t[:], func=AF.Ln, bias=1.0), [t4], [t4])
            c.op("dve", lambda e: e.tensor_tensor_scan(out=Fp.t[:], data0=t4.t[:], data1=t4.t[:], initial=0.0, op0=ALU.add, op1=ALU.max),
                 [t4], [Fp])
            c.op("dve", lambda e: e.tensor_tensor(out=aT.t[:], in0=iT.t[:], in1=Fp.t[:], op=ALU.add), [iT, Fp], [aT])
            c.op("dve", lambda e: e.tensor_tensor_scan(out=G_.t[:], data0=aT.t[:], data1=aT.t[:], initial=0.0, op0=ALU.max, op1=ALU.max),
                 [aT], [G_])
            c.op("dve", lambda e: e.tensor_scalar(out=nG.t[:], in0=G_.t[:], scalar1=-1.0, scalar2=None, op0=ALU.mult), [G_], [nG])
            c.op("dve", lambda e: e.tensor_tensor(out=iT.t[:], in0=Fp.t[:], in1=G_.t[:], op=ALU.subtract), [Fp, G_], [iT])
            nM = iT
            Gend = G_.t[:].rearrange("p (c t) -> p c t", t=128)[:, :, 127]
            c.op("dve", lambda e: e.memset(GP.t[:], 0.0), [], [GP])
            c.op("dve", lambda e: e.tensor_copy(out=GP.t[:, 1:16, 0], in_=G_.t[:].rearrange("p (c t) -> p c t", t=128)[:, 0:15, 127]), [G_], [GP])
            c.op("dve", lambda e: e.tensor_scalar(out=GP.t[:, :, 1], in0=Gend, scalar1=-1.0, scalar2=None, op0=ALU.mult), [G_], [GP])
            c.op("dve", lambda e: e.tensor_tensor(out=GP.t[:, :, 2], in0=GP.t[:, :, 0], in1=GP.t[:, :, 1], op=ALU.add), [GP], [GP])
            for cc in range(16):
                for j in range(3):
                    c.op("dve", lambda e: e.tensor_scalar(out=Dg.t[:, cc, j * 4:(j + 1) * 4], in0=I4, scalar1=GP.t[:, cc, j:j + 1], scalar2=None,
                                                          op0=ALU.mult), [c4, GP], [Dg])
            for cc in range(16):
                sl = slice(cc * 128, (cc + 1) * 128)
                o0 = cc * 16
                mmx = lambda oc, l, r, st, sp_: c.op("pe", lambda e: e.matmul(PCo.t[:, o0 + oc:o0 + oc + 4], lhsT=l, rhs=r, start=st, stop=sp_,
                                                                              skip_group_check=True), [nG, aT, nM, c4, Dg], [PCo])
                mmx(0, nG.t[:, sl], I4, True, False); mmx(0, ones4, Dg.t[:, cc, 0:4], False, True)
                mmx(4, nM.t[:, sl], I4, True, True)
                mmx(8, aT.t[:, sl], I4, True, False); mmx(8, ones4, Dg.t[:, cc, 4:8], False, True)
                mmx(12, ones4, Dg.t[:, cc, 8:12], True, True)
            c.op("act", lambda e: e.activation(out=cols.t[:].rearrange("p a b -> p (a b)"), in_=PCo.t[:, 0:256], func=AF.Exp), [PCo], [cols])
            c.pop()
            c.push()
            wq = [wslot([128, 8, 128], f"w{j}") for j in range(2)]
            wk = [wslot([128, 8, 128], f"w{2 + j}") for j in range(2)]
            wv = [wslot([128, 8, 256], f"w{4 + j}") for j in range(2)]
            wo_ = [wslot([128, 8, 256], f"w{6 + j}") for j in range(2)]
            qTc = Rot([c.sb([128, 128], BF16, "qTc") for _ in range(4)])
            kTc = Rot([c.sb([128, 128], BF16, "kTc") for _ in range(2)])
            kw = Rot([c.sb([128, 128], BF16, "kw") for _ in range(4)])
            Va = Rot([c.sb([128, 257], BF16, "Va") for _ in range(4)])
            Wt = Rot([c.sb([128, 128], F32, "Wt") for _ in range(3)])
            PTt = Rot([c.sb([128, 128], BF16, "PTt") for _ in range(4)])
            ONEF = c.sb([128, 256], F32, "ONEF")
            c.op("pool", lambda e: e.memset(ONEF.t[:], -1.0), [], [ONEF])
            tI = Rot([c.sb([128, 257], F32, "tI") for _ in range(2)])
            tot = Rot([c.sb([128, 257], F32, "tot") for _ in range(2)])
            sg = Rot([c.sb([128, 256], F32, "sg") for _ in range(4)])
            yh = Rot([c.sb([128, 256], BF16, "yh") for _ in range(3)])
            dn = Rot([c.sb([128, 2], F32, "dn") for _ in range(2)])
            Cf = c.sb([128, 257], F32, "Cf"); Cb = c.sb([128, 257], BF16, "Cb")
            PP = Rot([c.ps([128, 512], F32, "pp") for _ in range(2)])
            PW = Rot([c.ps([128, 512], F32, "pw") for _ in range(1)])
            PS2 = Rot([c.ps([128, 512], F32, "ps2") for _ in range(1)])
            PN = Rot([c.ps([128, 512], F32, "pn") for _ in range(2)])
            PC = c.ps([128, 512], F32, "pc")
            PTr = c.ps([128, 8, 128], BF16, "ptr")
            for v_ in Va.items:
                c.op("dve", lambda e: e.memset(v_.t[:, 256:257], 1.0), [], [v_])

            def load_head(h):
                b = h % 2
                load_w(wq[b], wq[b].t[:], win[:, h * 128:(h + 1) * 128])
                load_w(wk[b], wk[b].t[:], win[:, 512 + h * 128:512 + (h + 1) * 128])
                load_w(wv[b], wv[b].t[:], win[:, 1024 + h * 256:1024 + (h + 1) * 256])
                load_w(wo_[b], wo_[b].t[:], win[:, 2048 + h * 256:2048 + (h + 1) * 256])

            import os
            load_head(0)
            for h in range(int(os.environ.get('ML_HEADS', '4'))):
                b = h % 2
                if h + 1 < 4:
                    load_head(h + 1)
                Esel = c4.t[:, h * 128:(h + 1) * 128]
                def mS1(cc):
                    q_, j_ = divmod(cc, 4)
                    sl = slice(cc * 128, (cc + 1) * 128)
                    hsl = lambda k: HT[q_].t[:, k, j_ * 128:(j_ + 1) * 128]
                    pw = PW.next()
                    c.op("pe", lambda e: e.matmul(pw.t[:, 0:128], lhsT=aT.t[:, sl], rhs=Esel, start=True, stop=False), [aT, c4], [pw])
                    c.op("pe", lambda e: e.matmul(pw.t[:, 0:128], lhsT=Esel, rhs=nG.t[:, sl], start=False, stop=True), [nG, c4], [pw])
                    w_ = Wt.next()
                    c.op("act", lambda e: e.activation(out=w_.t[:], in_=pw.t[:, 0:128], func=AF.Exp), [pw], [w_])
                    c.op("dve", lambda e: e.tensor_tensor(out=w_.t[:], in0=w_.t[:], in1=maskLE, op=ALU.mult), [w_, CON], [w_])
                    pb = PP.next()
                    for k in range(8):
                        c.op("pe", lambda e: e.matmul(pb.t[:, 0:128], lhsT=wq[b].t[:, k, :], rhs=hsl(k), start=(k == 0), stop=(k == 7)),
                             [wq[b], HT[q_]], [pb])
                    for k in range(8):
                        c.op("pe", lambda e: e.matmul(pb.t[:, 128:256], lhsT=wk[b].t[:, k, :], rhs=hsl(k), start=(k == 0), stop=(k == 7),
                                                      skip_group_check=True), [wk[b], HT[q_]], [pb])
                    qc = qTc.next(); kc = kTc.next()
                    c.op("act", lambda e: e.activation(out=qc.t[:], in_=pb.t[:, 0:128], func=AF.Copy, scale=128.0 ** -0.5), [pb], [qc])
                    c.op("dve", lambda e: e.tensor_copy(out=kc.t[:], in_=pb.t[:, 128:256]), [pb], [kc])
                    pb = PP.next()
                    for k in range(8):
                        c.op("pe", lambda e: e.matmul(pb.t[:, 0:128], lhsT=hsl(k), rhs=wk[b].t[:, k, :], start=(k == 0), stop=(k == 7)),
                             [wk[b], HT[q_]], [pb])
                    for k in range(8):
                        c.op("pe", lambda e: e.matmul(pb.t[:, 128:384], lhsT=hsl(k), rhs=wv[b].t[:, k, :], start=(k == 0), stop=(k == 7),
                                                      skip_group_check=True), [wv[b], HT[q_]], [pb])
                    kw_ = kw.next(); va = Va.next()
                    c.op("act", lambda e: e.activation(out=kw_.t[:], in_=pb.t[:, 0:128], func=AF.Copy, scale=cols.t[:, cc, 8 + h:9 + h]),
                         [pb, cols], [kw_])
                    c.op("dve", lambda e: e.tensor_copy(out=va.t[:, 0:256], in_=pb.t[:, 128:384]), [pb], [va])
                    ps_ = PS2.next()
                    c.op("pe", lambda e: e.matmul(ps_.t[:, 0:128], lhsT=kc.t[:], rhs=qc.t[:], start=True, stop=True), [kc, qc], [ps_])
                    pt = PTt.next()
                    c.op("dve", lambda e: e.tensor_tensor(out=pt.t[:], in0=ps_.t[:, 0:128], in1=w_.t[:], op=ALU.mult), [ps_, w_], [pt])
                    pb = PP.next()
                    for k in range(8):
                        c.op("pe", lambda e: e.matmul(pb.t[:, 0:256], lhsT=hsl(k), rhs=wo_[b].t[:, k, :], start=(k == 0), stop=(k == 7)),
                             [wo_[b], HT[q_]], [pb])
                    s_ = sg.next()
                    c.op("act", lambda e: e.activation(out=s_.t[:], in_=pb.t[:, 0:256], func=AF.Tanh, scale=0.5), [pb], [s_])
                    c.op("dve", lambda e: e.tensor_scalar(out=s_.t[:], in0=s_.t[:], scalar1=0.5, scalar2=0.5, op0=ALU.mult, op1=ALU.add), [s_], [s_])
                    return dict(qc=qc, kw=kw_, va=va, pt=pt, s=s_)

                def mS2(cc, d, prev_y):
                    sl = slice(cc * 128, (cc + 1) * 128)
                    qc, kw_, va, pt, s_ = d["qc"], d["kw"], d["va"], d["pt"], d["s"]
                    if cc > 0:
                        pi = PN.next()
                        c.op("pe", lambda e: e.matmul(pi.t[:, 0:257], lhsT=qc.t[:], rhs=Cb.t[:], start=True, stop=True), [qc, Cb], [pi])
                    c.op("pe", lambda e: e.matmul(PC.t[:, 0:257], lhsT=kw_.t[:], rhs=va.t[:], start=True, stop=True), [kw_, va], [PC])
                    pn = PN.next()
                    c.op("pe", lambda e: e.matmul(pn.t[:, 0:257], lhsT=pt.t[:], rhs=va.t[:], start=True, stop=True), [pt, va], [pn])
                    if prev_y is not None:
                        pcc, py_ = prev_y
                        for j2 in range(2):
                            c.op("pe", lambda e: e.transpose(out=PTr.t[:, j2, :], in_=py_.t[:, j2 * 128:(j2 + 1) * 128], identity=ident), [py_, CON], [PTr])
                        for j2 in range(2):
                            c.op("act", lambda e: e.activation(out=YT[2 * h + j2].t[:, pcc * 128:(pcc + 1) * 128], in_=PTr.t[:, j2, :], func=AF.Copy),
                                 [PTr], [YT[2 * h + j2]])
                    if cc > 0:
                        c.op("dve", lambda e: e.scalar_tensor_tensor(out=Cf.t[:], in0=Cf.t[:], scalar=cols.t[:, cc, 12 + h:13 + h], in1=PC.t[:, 0:257],
                                                                     op0=ALU.mult, op1=ALU.add), [Cf, cols, PC], [Cf])
                    else:
                        c.op("dve", lambda e: e.tensor_copy(out=Cf.t[:], in_=PC.t[:, 0:257]), [PC], [Cf])
                    to_ = tot.next()
                    if cc > 0:
                        ti = tI.next()
                        c.op("act", lambda e: e.activation(out=ti.t[:], in_=pi.t[:, 0:257], func=AF.Copy, scale=cols.t[:, cc, h:h + 1]),
                             [pi, cols], [ti])
                    c.op("act", lambda e: e.activation(out=Cb.t[:], in_=Cf.t[:], func=AF.Copy), [Cf], [Cb])
                    if cc > 0:
                        c.op("dve", lambda e: e.tensor_tensor(out=to_.t[:], in0=ti.t[:], in1=pn.t[:, 0:257], op=ALU.add), [ti, pn], [to_])
                    else:
                        c.op("dve", lambda e: e.tensor_copy(out=to_.t[:], in_=pn.t[:, 0:257]), [pn], [to_])
                    d_ = dn.next()
                    c.op("dve", lambda e: e.tensor_scalar(out=d_.t[:, 0:1], in0=to_.t[:, 256:257], scalar1=-1.0, scalar2=None, op0=ALU.mult), [to_], [d_])
                    c.op("dve", lambda e: e.tensor_tensor(out=d_.t[:, 0:1], in0=d_.t[:, 0:1], in1=to_.t[:, 256:257], op=ALU.max), [to_, d_], [d_])
                    c.op("dve", lambda e: e.tensor_tensor(out=d_.t[:, 0:1], in0=d_.t[:, 0:1], in1=cols.t[:, cc, 4 + h:5 + h], op=ALU.max), [cols, d_], [d_])
                    c.op("dve", lambda e: e.reciprocal(out=d_.t[:, 1:2], in_=d_.t[:, 0:1]), [d_], [d_])
                    y_ = yh.next()
                    c.op("dve", lambda e: e.scalar_tensor_tensor(out=y_.t[:], in0=to_.t[:, 0:256], scalar=d_.t[:, 1:2], in1=s_.t[:],
                                                                 op0=ALU.mult, op1=ALU.mult), [to_, d_, s_], [y_])
                    return (cc, y_)

                def mFlush(prev_y):
                    pcc, py_ = prev_y
                    for j2 in range(2):
                        c.op("pe", lambda e: e.transpose(out=PTr.t[:, j2, :], in_=py_.t[:, j2 * 128:(j2 + 1) * 128], identity=ident), [py_, CON], [PTr])
                    for j2 in range(2):
                        c.op("act", lambda e: e.activation(out=YT[2 * h + j2].t[:, pcc * 128:(pcc + 1) * 128], in_=PTr.t[:, j2, :], func=AF.Copy),
                             [PTr], [YT[2 * h + j2]])

                dd = {}; prev_y = None
                for n in range(16 + 2):
                    if n < 16:
                        dd[n] = mS1(n)
                    if 0 <= n - 2 < 16:
                        prev_y = mS2(n - 2, dd.pop(n - 2), prev_y)
                mFlush(prev_y)
            c.pop()
            return YT

        def mixer_gla(win):
            c.push()
            YT = [c.sb([128, S], BF16, f"YT{k}") for k in range(8)]
            c.push()
            wa = wslot([128, 8, 16], "w8")
            wa2 = wslot([16, 512], "w9")
            load_w(wa, wa.t[:], win[:, 3072:3088])
            c.dma("pool", wa2.t[:], Dm["gla_wa2"][:, :], writes=[wa2], key="w9")
            rm = wslot([128, S], "w10")
            c.dma("pool", rm.t[:], Dm["rmask"][:, :], writes=[rm], key="w10")
            nba = c.sb([128, 4], F32, "nba")
            c.dma("sp", nba.t[:], Dm["gla_ba"][:, :], writes=[nba], key="bi")
            c.op("dve", lambda e: e.tensor_scalar(out=nba.t[:], in0=nba.t[:], scalar1=-1.0, scalar2=None, op0=ALU.mult), [nba], [nba])
            gB = c.sb([128, 256], F32, "gB256")
            c.dma("sp", gB.t[:], Dm["gla_norm_g"][0:1, :].partition_broadcast(128), writes=[gB], key="bf")
            c.op("dve", lambda e: e.tensor_scalar(out=gB.t[:], in0=gB.t[:], scalar1=0.5, scalar2=None, op0=ALU.mult), [gB], [gB])
            eps = c.sb([128, 1], F32, "geps")
            c.op("dve", lambda e: e.memset(eps.t[:], 1e-6), [], [eps])
            alT = c.sb([16, S], BF16, "alT")
            wq = wslot([128, 8, 128], "w0"); wk = wslot([128, 8, 128], "w1")
            wv = wslot([128, 8, 256], "w2"); wr = wslot([128, 8, 256], "w3")
            sp_ = c.sb([128, S], F32, "sp"); bp = c.sb([128, S], F32, "bp")
            qtl = c.sb([128, S], BF16, "qtl"); ktl = c.sb([128, S], BF16, "ktl")
            tE = Rot([c.sb([128, 512], F32, "tE") for _ in range(2)])
            ebl = c.sb([128, 16], F32, "ebl")
            Vc = Rot([c.sb([128, 256], BF16, "Vc") for _ in range(4)])
            AT = Rot([c.sb([128, 128], BF16, "AT") for _ in range(4)])
            khT = Rot([c.sb([128, 128], BF16, "khT") for _ in range(3)])
            kh = Rot([c.sb([128, 128], BF16, "kh") for _ in range(4)])
            NEG1 = c.sb([128, 256], F32, "NEG1")
            c.op("pool", lambda e: e.memset(NEG1.t[:], -1.0), [], [NEG1])
            junk = c.sb([128, 256], F32, "junk")
            sm = Rot([c.sb([128, 4], F32, "gsm") for _ in range(2)])
            er = Rot([c.sb([128, 256], F32, "er") for _ in range(4)])
            yh = Rot([c.sb([128, 256], BF16, "yh") for _ in range(3)])
            Sf = c.sb([128, 256], F32, "Sf"); Sb = c.sb([128, 256], BF16, "Sb")
            PP = Rot([c.ps([128, 512], F32, "pp") for _ in range(2)])
            PS_ = Rot([c.ps([128, 512], F32, "pss") for _ in range(1)])
            PO = Rot([c.ps([128, 512], F32, "pgo") for _ in range(2)])
            PC = c.ps([128, 512], F32, "pc")
            PTr = c.ps([128, 8, 128], BF16, "ptr")
            PT2 = c.ps([128, 8, 128], BF16, "pt2")
            for tq in range(4):
                pb = PP.next()
                proj_fm(pb, wa, lambda k: wa.t[:, k, :], tq)
                c.op("dve", lambda e: e.tensor_copy(out=alT.t[:, tq * 512:(tq + 1) * 512], in_=pb.t[0:16, :]), [pb], [alT])
            for h in range(4):
                load_w(wq, wq.t[:], win[:, h * 128:(h + 1) * 128])
                load_w(wk, wk.t[:], win[:, 512 + h * 128:512 + (h + 1) * 128])
                load_w(wv, wv.t[:], win[:, 1024 + h * 256:1024 + (h + 1) * 256])
                load_w(wr, wr.t[:], win[:, 2048 + h * 256:2048 + (h + 1) * 256])
                for tq in range(4):
                    ts_ = slice(tq * 512, (tq + 1) * 512)
                    pb = PP.next()
                    c.op("pe", lambda e: e.matmul(pb.t[:], lhsT=wa2.t[:, h * 128:(h + 1) * 128], rhs=alT.t[:, ts_], start=True, stop=True),
                         [wa2, alT], [pb])
                    te = tE.next()
                    c.op("act", lambda e: e.activation(out=te.t[:], in_=pb.t[:], func=AF.Exp, bias=nba.t[:, h:h + 1], scale=-1.0), [pb, nba], [te])
                    c.op("act", lambda e: e.activation(out=sp_.t[:, ts_], in_=te.t[:], func=AF.Ln, bias=1.0), [te], [sp_])
                c.op("dve", lambda e: e.tensor_tensor_scan(out=bp.t[:], data0=rm.t[:], data1=sp_.t[:], initial=0.0, op0=ALU.mult, op1=ALU.add),
                     [rm, sp_], [bp])
                c.op("act", lambda e: e.activation(out=ebl.t[:], in_=bp.t[:].rearrange("p (c t) -> p c t", t=128)[:, :, 127], func=AF.Exp,
                                                   scale=-1.0 / 16), [bp], [ebl])
                for tq in range(4):
                    ts_ = slice(tq * 512, (tq + 1) * 512)
                    pb = PP.next()
                    proj_fm(pb, wq, lambda k: wq.t[:, k, :], tq)
                    te = tE.next()
                    c.op("act", lambda e: e.activation(out=te.t[:], in_=bp.t[:, ts_], func=AF.Exp, scale=-1.0 / 16), [bp], [te])
                    c.op("dve", lambda e: e.scalar_tensor_tensor(out=qtl.t[:, ts_], in0=pb.t[:], scalar=128.0 ** -0.5, in1=te.t[:],
                                                                 op0=ALU.mult, op1=ALU.mult), [pb, te], [qtl])
                    pb = PP.next()
                    proj_fm(pb, wk, lambda k: wk.t[:, k, :], tq)
                    te = tE.next()
                    c.op("act", lambda e: e.activation(out=te.t[:], in_=bp.t[:, ts_], func=AF.Exp, scale=1.0 / 16), [bp], [te])
                    c.op("dve", lambda e: e.tensor_tensor(out=ktl.t[:, ts_], in0=pb.t[:], in1=te.t[:], op=ALU.mult), [pb, te], [ktl])
                def gS1(cc):
                    q_, j_ = divmod(cc, 4)
                    sl = slice(cc * 128, (cc + 1) * 128)
                    hsl = lambda k: HT[q_].t[:, k, j_ * 128:(j_ + 1) * 128]
                    kt_ = khT.next(); k_ = kh.next()
                    c.op("dve", lambda e: e.tensor_scalar(out=kt_.t[:], in0=ktl.t[:, sl], scalar1=ebl.t[:, cc:cc + 1], scalar2=None, op0=ALU.mult),
                         [ktl, ebl], [kt_])
                    ps = PS_.next()
                    c.op("pe", lambda e: e.matmul(ps.t[:, 0:128], lhsT=ktl.t[:, sl], rhs=qtl.t[:, sl], start=True, stop=True), [ktl, qtl], [ps])
                    at = AT.next()
                    c.op("dve", lambda e: e.tensor_tensor(out=at.t[:], in0=ps.t[:, 0:128], in1=maskLE, op=ALU.mult), [ps, CON], [at])
                    pb = PP.next()
                    for k in range(8):
                        c.op("pe", lambda e: e.matmul(pb.t[:, 0:256], lhsT=hsl(k), rhs=wv.t[:, k, :], start=(k == 0), stop=(k == 7)),
                             [wv, HT[q_]], [pb])
                    vc = Vc.next()
                    c.op("act", lambda e: e.activation(out=vc.t[:], in_=pb.t[:, 0:256], func=AF.Copy), [pb], [vc])
                    c.op("pe", lambda e: e.transpose(out=PT2.t[:, 0, :], in_=kt_.t[:], identity=ident), [kt_, CON], [PT2])
                    c.op("act", lambda e: e.activation(out=k_.t[:], in_=PT2.t[:, 0, :], func=AF.Copy), [PT2], [k_])
                    pr = PP.next()
                    for k in range(8):
                        c.op("pe", lambda e: e.matmul(pr.t[:, 0:256], lhsT=hsl(k), rhs=wr.t[:, k, :], start=(k == 0), stop=(k == 7)),
                             [wr, HT[q_]], [pr])
                    e_ = er.next()
                    c.op("act", lambda e: e.activation(out=e_.t[:], in_=pr.t[:, 0:256], func=AF.Tanh, scale=0.5), [pr], [e_])
                    c.op("dve", lambda e: e.scalar_tensor_tensor(out=e_.t[:], in0=e_.t[:], scalar=1.0, in1=pr.t[:, 0:256], op0=ALU.add, op1=ALU.mult),
                         [e_, pr], [e_])
                    c.op("pool", lambda e: e.tensor_tensor(out=e_.t[:], in0=e_.t[:], in1=gB.t[:], op=ALU.mult), [e_, gB], [e_])
                    return dict(vc=vc, at=at, e=e_, k=k_)

                def gFlush(prev_y):
                    pcc, py_ = prev_y
                    for j2 in range(2):
                        c.op("pe", lambda e: e.transpose(out=PTr.t[:, j2, :], in_=py_.t[:, j2 * 128:(j2 + 1) * 128], identity=ident), [py_, CON], [PTr])
                    for j2 in range(2):
                        c.op("act", lambda e: e.activation(out=YT[2 * h + j2].t[:, pcc * 128:(pcc + 1) * 128], in_=PTr.t[:, j2, :], func=AF.Copy),
                             [PTr], [YT[2 * h + j2]])

                def gS2(cc, d, prev_y):
                    sl = slice(cc * 128, (cc + 1) * 128)
                    vc, at, e_, k_ = d["vc"], d["at"], d["e"], d["k"]
                    po = PO.next()
                    if cc > 0:
                        c.op("pe", lambda e: e.matmul(po.t[:, 0:256], lhsT=qtl.t[:, sl], rhs=Sb.t[:], start=True, stop=False), [qtl, Sb], [po])
                    c.op("pe", lambda e: e.matmul(PC.t[:, 0:256], lhsT=k_.t[:], rhs=vc.t[:], start=True, stop=True), [k_, vc], [PC])
                    c.op("pe", lambda e: e.matmul(po.t[:, 0:256], lhsT=at.t[:], rhs=vc.t[:], start=(cc == 0), stop=True), [at, vc], [po])
                    if prev_y is not None:
                        gFlush(prev_y)
                    if cc > 0:
                        c.op("dve", lambda e: e.scalar_tensor_tensor(out=Sf.t[:], in0=Sf.t[:], scalar=ebl.t[:, cc:cc + 1], in1=PC.t[:, 0:256],
                                                                     op0=ALU.mult, op1=ALU.add), [Sf, ebl, PC], [Sf])
                    else:
                        c.op("dve", lambda e: e.tensor_copy(out=Sf.t[:], in_=PC.t[:, 0:256]), [PC], [Sf])
                    m_ = sm.next()
                    c.op("act", lambda e: e.activation(out=junk.t[:], in_=po.t[:, 0:256], func=AF.Square, accum_out=m_.t[:, 0:1]), [po], [junk, m_])
                    c.op("act", lambda e: e.activation(out=Sb.t[:], in_=Sf.t[:], func=AF.Copy), [Sf], [Sb])
                    c.op("act", lambda e: e.activation(out=m_.t[:, 1:2], in_=m_.t[:, 0:1], func=AF.Ln, bias=eps.t[:, 0:1], scale=1.0 / 256),
                         [m_, eps], [m_])
                    c.op("act", lambda e: e.activation(out=m_.t[:, 2:3], in_=m_.t[:, 1:2], func=AF.Exp, scale=-0.5), [m_], [m_])
                    y_ = yh.next()
                    c.op("dve", lambda e: e.scalar_tensor_tensor(out=y_.t[:], in0=po.t[:, 0:256], scalar=m_.t[:, 2:3], in1=e_.t[:],
                                                                 op0=ALU.mult, op1=ALU.mult), [po, m_, e_], [y_])
                    return (cc, y_)

                dd = {}; prev_y = None
                for n in range(16 + 2):
                    if n < 16:
                        dd[n] = gS1(n)
                    if 0 <= n - 2 < 16:
                        prev_y = gS2(n - 2, dd.pop(n - 2), prev_y)
                gFlush(prev_y)
            c.pop()
            return YT

        def xattn(layer):
            c.push()
            OT = [c.sb([128, S], BF16, f"OT{k}") for k in range(8)]
            c.push()
            wq = [wslot([128, 8, 256], f"w{j}") for j in range(2)]
            wk = [wslot([128, 8, 256], f"w{2 + j}") for j in range(2)]
            wv = [wslot([128, 8, 256], f"w{4 + j}") for j in range(2)]
            qTs = [c.sb([128, 2, S], BF16, "xqT") for _ in range(2)]
            kTs = [c.sb([128, 2, 256], BF16, "xkT") for _ in range(2)]
            Vas = [c.sb([128, 2, 257], BF16, "xV") for _ in range(2)]
            PT = [Rot([c.sb([128, 512], BF16, "xPT") for _ in range(2)]) for _ in range(2)]
            Ot = Rot([c.sb([128, 256], BF16, "xO") for _ in range(3)])
            rd = Rot([c.sb([128, 1], F32, "xrd") for _ in range(3)])
            PP = Rot([c.ps([128, 512], F32, "pp") for _ in range(2)])
            PS_ = Rot([c.ps([128, 512], F32, "psc") for _ in range(2)])
            PO = Rot([c.ps([128, 512], F32, "pxo") for _ in range(2)])
            PTr = Rot([c.ps([128, 8, 128], BF16, "ptr") for _ in range(2)])
            for Va_ in Vas:
                c.op("dve", lambda e: e.memset(Va_.t[:, :, 256:257], 1.0), [], [Va_])
            wkv = Dm["xa_wkv"][layer]

            def load_head(h):
                b = h % 2
                load_w(wq[b], wq[b].t[:], Dm["xa_wq"][layer][:, h * 256:(h + 1) * 256])
                load_w(wk[b], wk[b].t[:], wkv[:, h * 256:(h + 1) * 256])
                load_w(wv[b], wv[b].t[:], wkv[:, 1024 + h * 256:1024 + (h + 1) * 256])

            def xproj_items(h):
                bb = h % 2
                items = []
                for dc in range(2):
                    def fk(dc=dc):
                        pb = PP.next()
                        for k in range(8):
                            c.op("pe", lambda e: e.matmul(pb.t[:, 0:256], lhsT=wk[bb].t[:, k, dc * 128:(dc + 1) * 128], rhs=memT.t[:, k, :],
                                                          start=(k == 0), stop=(k == 7)), [wk[bb], memT], [pb])
                        c.op("dve", lambda e: e.tensor_copy(out=kTs[bb].t[:, dc, :], in_=pb.t[:, 0:256]), [pb], [kTs[bb]])
                    items.append(fk)
                for mt in range(2):
                    def fv(mt=mt):
                        pb = PP.next()
                        for k in range(8):
                            c.op("pe", lambda e: e.matmul(pb.t[:, 0:256], lhsT=memT.t[:, k, mt * 128:(mt + 1) * 128], rhs=wv[bb].t[:, k, :],
                                                          start=(k == 0), stop=(k == 7)), [wv[bb], memT], [pb])
                        c.op("dve", lambda e: e.tensor_copy(out=Vas[bb].t[:, mt, 0:256], in_=pb.t[:, 0:256]), [pb], [Vas[bb]])
                    items.append(fv)
                for dc in range(2):
                    for tq in range(4):
                        def fq(dc=dc, tq=tq):
                            pb = PP.next()
                            proj_fm(pb, wq[bb], lambda k: wq[bb].t[:, k, dc * 128:(dc + 1) * 128], tq)
                            c.op("act", lambda e: e.activation(out=qTs[bb].t[:, dc, tq * 512:(tq + 1) * 512], in_=pb.t[:], func=AF.Copy, scale=1.0 / 16),
                                 [pb], [qTs[bb]])
                        items.append(fq)
                return items

            load_head(0)
            load_head(1)
            for it in xproj_items(0):
                it()
            for h in range(4):
                b = h % 2
                if 1 <= h and h + 1 < 4:
                    load_head(h + 1)
                nxt = xproj_items(h + 1) if h + 1 < 4 else []
                qT, kT, Va = qTs[b], kTs[b], Vas[b]
                def xP(tq):
                    pts = []
                    for mt in range(2):
                        psc = PS_.next()
                        for dc in range(2):
                            c.op("pe", lambda e: e.matmul(psc.t[:], lhsT=kT.t[:, dc, mt * 128:(mt + 1) * 128],
                                                          rhs=qT.t[:, dc, tq * 512:(tq + 1) * 512], start=(dc == 0), stop=(dc == 1)),
                                 [kT, qT], [psc])
                        pt = PT[mt].next()
                        c.op("act", lambda e: e.activation(out=pt.t[:], in_=psc.t[:], func=AF.Exp), [psc], [pt])
                        pts.append(pt)
                    return pts

                def xV(i, pts):
                    jt = i % 4
                    po = PO.next()
                    for mt in range(2):
                        c.op("pe", lambda e: e.matmul(po.t[:, 0:257], lhsT=pts[mt].t[:, jt * 128:(jt + 1) * 128], rhs=Va.t[:, mt, :],
                                                      start=(mt == 0), stop=(mt == 1)), [pts[mt], Va], [po])
                    r_ = rd.next(); o_ = Ot.next()
                    c.op("dve", lambda e: e.reciprocal(out=r_.t[:], in_=po.t[:, 256:257]), [po], [r_])
                    c.op("act", lambda e: e.activation(out=o_.t[:], in_=po.t[:, 0:256], func=AF.Copy, scale=r_.t[:, 0:1]), [po, r_], [o_])
                    return o_

                def xT(i, o_):
                    ptr = PTr.next()
                    for j2 in range(2):
                        c.op("pe", lambda e: e.transpose(out=ptr.t[:, j2, :], in_=o_.t[:, j2 * 128:(j2 + 1) * 128], identity=ident),
                             [o_, CON], [ptr])
                    for j2 in range(2):
                        c.op("dve", lambda e: e.tensor_copy(out=OT[2 * h + j2].t[:, i * 128:(i + 1) * 128], in_=ptr.t[:, j2, :]),
                             [ptr], [OT[2 * h + j2]])

                ptsl = {0: xP(0)}
                prev = None
                for tq in range(4):
                    if tq + 1 < 4:
                        ptsl[tq + 1] = xP(tq + 1)
                    for jt in range(4):
                        i = tq * 4 + jt
                        o_ = xV(i, ptsl[tq])
                        if prev is not None:
                            xT(*prev)
                        prev = (i, o_)
                        if i >= 2 and nxt:
                            nxt.pop(0)()
                xT(*prev)
                for it in nxt:
                    it()
            c.pop()
            out_proj_ln(OT, Dm["xa_wo"][layer], layer * 3 + 1)
            c.pop()

        def ffn(layer):
            c.push()
            groups = [[0, 1]] + [list(range(g, g + 4)) for g in range(2, NCH, 4)]
            wup = Dm["ffn_up"][layer]
            wdn = Dm["ffn_down"][layer]
            cw = c.sb([128, 44 * 3], F32, "cw"); cb = c.sb([128, 44], F32, "cb")
            c.dma("sp", cw.t[:], Dm["ffn_conv"][layer], writes=[cw], key="cw")
            c.dma("sp", cb.t[:], Dm["ffn_conv_b"][layer], writes=[cb], key="cb")
            act = [c.sb([128, S], BF16, f"act{j}") for j in range(4)]
            wd = [wslot([128, 4, D], f"w{j}") for j in range(2)]
            PD = Rot([c.ps([128, 512], F32, "pd") for _ in range(4)])
            c.push()
            wg = [wslot([128, 8, 128], f"w{2 + j}") for j in range(2)]
            wv = [wslot([128, 8, 128], f"w{4 + j}") for j in range(2)]
            ug = c.sb([128, S + 2], F32, "ug"); uv = c.sb([128, S + 2], F32, "uv")
            cg = c.sb([128, S], F32, "cg"); cv = c.sb([128, S], F32, "cv")
            gg = c.sb([128, S], F32, "gg")
            PU = Rot([c.ps([128, 512], F32, "pu") for _ in range(4)])
            c.op("dve", lambda e: e.memset(ug.t[:, 0:2], 0.0), [], [ug])
            c.op("dve", lambda e: e.memset(uv.t[:, 0:2], 0.0), [], [uv])

            def load_up(j):
                b = j % 2
                load_w(wg[b], wg[b].t[:], wup[:, j * 128:(j + 1) * 128])
                load_w(wv[b], wv[b].t[:], wup[:, DFF + j * 128:DFF + (j + 1) * 128])

            def conv_part(u, w, ch, dst):
                c.op("dve", lambda e: e.scalar_tensor_tensor(out=dst.t[:], in0=u.t[:, 1:S + 1], scalar=cw.t[:, ch * 3 + 1:ch * 3 + 2],
                                                             in1=dst.t[:], op0=ALU.mult, op1=ALU.add), [u, cw, dst], [dst])
                c.op("dve", lambda e: e.scalar_tensor_tensor(out=dst.t[:], in0=u.t[:, 0:S], scalar=cw.t[:, ch * 3:ch * 3 + 1],
                                                             in1=dst.t[:], op0=ALU.mult, op1=ALU.add), [u, cw, dst], [dst])

            def down_tile(i, gi, grp, wdb):
                for half in range(2):
                    pd = PD.next()
                    for jj in range(len(grp)):
                        c.op("pe", lambda e: e.matmul(pd.t[:], lhsT=act[jj].t[:, i * 128:(i + 1) * 128],
                                                      rhs=wdb.t[:, jj, half * 512:(half + 1) * 512],
                                                      start=(jj == 0), stop=(jj == len(grp) - 1)), [act[jj], wdb], [pd])
                    hs = H[i].t[:, half * 512:(half + 1) * 512]
                    if gi == 0:
                        c.op("dve", lambda e: e.scalar_tensor_tensor(out=hs, in0=hs, scalar=ALPHA, in1=pd.t[:], op0=ALU.mult, op1=ALU.add),
                             [H[i], pd], [H[i]])
                    else:
                        c.op("dve", lambda e: e.tensor_tensor(out=hs, in0=hs, in1=pd.t[:], op=ALU.add), [H[i], pd], [H[i]])

            load_up(0)
            last = len(groups) - 1
            for gi, grp in enumerate(groups):
                wdb = wd[gi % 2]
                load_w(wdb, wdb.t[:, 0:len(grp), :], wdn[grp[0] * 128:(grp[-1] + 1) * 128, :])
                for jj, j in enumerate(grp):
                    b = j % 2
                    if j + 1 < NCH:
                        load_up(j + 1)
                    for (w_, u_, cdst, ch) in ((wg[b], ug, cg, j), (wv[b], uv, cv, NCH + j)):
                        for tq in range(4):
                            pb = PU.next()
                            proj_fm(pb, w_, lambda k: w_.t[:, k, :], tq)
                            c.op("act", lambda e: e.activation(out=u_.t[:, 2 + tq * 512:2 + (tq + 1) * 512], in_=pb.t[:], func=AF.Copy),
                                 [pb], [u_])
                            c.op("act", lambda e: e.activation(out=cdst.t[:, tq * 512:(tq + 1) * 512], in_=pb.t[:], func=AF.Identity,
                                                               bias=cb.t[:, ch:ch + 1], scale=cw.t[:, ch * 3 + 2:ch * 3 + 3]),
                                 [pb, cb, cw], [cdst])
                        conv_part(u_, w_, ch, cdst)
                    c.op("act", lambda e: e.activation(out=gg.t[:], in_=cg.t[:], func=AF.Gelu_apprx_tanh), [cg], [gg])
                    c.op("pool", lambda e: e.tensor_tensor(out=act[jj].t[:], in0=gg.t[:], in1=cv.t[:], op=ALU.mult), [gg, cv], [act[jj]])
                if gi < last:
                    for i in range(NT):
                        down_tile(i, gi, grp, wdb)
            c.pop()
            R, gB, bB = ln_res(layer * 3 + 2)
            grp = groups[last]; wdb = wd[last % 2]
            hbs = {}
            for n in range(NT + 2):
                if n < NT: down_tile(n, last, grp, wdb)
                if 0 <= n - 1 < NT: hbs[n - 1] = ln_tile(n - 1, H[n - 1], gB, bB, R)
                if 0 <= n - 2 < NT: ht_from_hb(n - 2, hbs[n - 2], R["pT"])
            c.pop()

        c.push()
        hbR = Rot([c.sb([128, D], BF16, "hb") for _ in range(2)])
        pTR = Rot([c.ps([128, 8, 128], BF16, "pT") for _ in range(2)])
        for i in range(NT):
            to_ht(i, hbR, pTR)
        mf = c.sb([128, D], F32, "mf")
        for mt in range(2):
            c.dma("sp", mf.t[:], Dm["mem"][mt * 128:(mt + 1) * 128, :], writes=[mf], key="mem")
            hb = hbR.next()
            c.op("act", lambda e: e.activation(out=hb.t[:], in_=mf.t[:], func=AF.Copy), [mf], [hb])
            pT = pTR.next()
            for k in range(8):
                c.op("pe", lambda e: e.transpose(out=pT.t[:, k, :], in_=hb.t[:, k * 128:(k + 1) * 128], identity=ident), [hb, CON], [pT])
            c.op("dve", lambda e: e.tensor_copy(out=memT.t[:, :, mt * 128:(mt + 1) * 128], in_=pT.t[:]), [pT], [memT])
        c.pop()

        def finish():
            for i in range(NT):
                c.dma("sp", out[i * 128:(i + 1) * 128, :], H[i].t[:], reads=[H[i]], key="out")
            c.barrier()

        done = False
        for layer in range(first, nlayers):
            kind = layer % 4
            if kind == 0:
                YT = mixer_sb(Dm["sb_win"])
            elif kind == 1:
                YT = mixer_fox(Dm["fox_win"], Dm["fox_bf"])
            elif kind == 2:
                YT = mixer_mlstm(Dm["ml_win"])
            else:
                YT = mixer_gla(Dm["gla_win"])
            out_proj_ln(YT, Dm["mix_wo"][layer], layer * 3 + 0)
            c.pop()
            if stop == (layer, "a"):
                break
            xattn(layer)
            if stop == (layer, "b"):
                break
            ffn(layer)
        finish()
    return nc


_NC_CACHE = {}


def prep_inputs(inputs):
    con, con4, rmask = make_consts()
    shared = {
        "ln_g": np.ascontiguousarray(inputs["ln_g"].reshape(12, D)),
        "ln_b": np.ascontiguousarray(inputs["ln_b"].reshape(12, D)),
        "mix_wo": inputs["mix_wo"], "sb_win": inputs["sb_win"][0], "fox_win": inputs["fox_win"][0],
        "fox_bf": inputs["fox_bf"].reshape(1, 16),
        "ml_win": inputs["ml_win"][0], "ml_bi": inputs["ml_bi"].reshape(4, 1), "ml_bf": inputs["ml_bf"].reshape(4, 1),
        "gla_win": inputs["gla_win"][0], "gla_wa2": inputs["gla_wa2"][0],
        "gla_ba": np.ascontiguousarray(inputs["gla_ba"].reshape(4, 128).T),
        "gla_norm_g": inputs["gla_norm_g"].reshape(1, 256),
        "xa_wq": inputs["xa_wq"], "xa_wkv": inputs["xa_wkv"], "xa_wo": inputs["xa_wo"],
        "ffn_up": inputs["ffn_up"],
        "ffn_conv": np.ascontiguousarray(inputs["ffn_conv"].reshape(4, 3, 44, 128).transpose(0, 3, 2, 1).reshape(4, 128, 132)),
        "ffn_conv_b": np.ascontiguousarray(inputs["ffn_conv_b"].reshape(4, 44, 128).transpose(0, 2, 1)),
        "ffn_down": inputs["ffn_down"],
        "con": con, "con4": con4, "rmask": rmask,
    }
    shared = {k: np.ascontiguousarray(v, dtype=np.float32) for k, v in shared.items()}
    return shared


def kernel(**inputs):
    inputs = {k: np.asarray(v) for k, v in inputs.items()}
    shared = prep_inputs(inputs)
    if "nc" not in _NC_CACHE:
        _NC_CACHE["nc"] = build()
    nc = _NC_CACHE["nc"]
    in_maps = []
    for b in range(8):
        m = dict(shared)
        m["x"] = np.ascontiguousarray(inputs["x"][b], dtype=np.float32)
        m["mem"] = np.ascontiguousarray(inputs["mem"][b], dtype=np.float32)
        in_maps.append(m)
    res = run_bass_kernel_spmd(nc, in_maps, core_ids=list(range(8)))
    return np.stack([np.asarray(r["out"], dtype=np.float32) for r in res.results], axis=0)
```

```python
import numpy as np
import concourse.bass as bass
import concourse.mybir as mybir
from concourse.bass_utils import run_bass_kernel_spmd
from contextlib import ExitStack

F32 = mybir.dt.float32
BF16 = mybir.dt.bfloat16
AF = mybir.ActivationFunctionType
ALU = mybir.AluOpType
AX = mybir.AxisListType

S = 2048
D = 1024
NT = 16
ALPHA = 8.0 ** 0.25
LN_EPS = 1e-5
DFF = 2816
NCH = 22


class Eng:
    def __init__(s, name, h, sem):
        s.name, s.h, s.sem, s.cnt, s.seen = name, h, sem, 0, {}


class DSem:
    def __init__(s, key, sem):
        s.key, s.sem, s.cnt = key, sem, 0


class TT:
    def __init__(s, t, name):
        s.t, s.name, s.w, s.r = t, name, None, {}


class Ctx:
    def __init__(s, nc, es):
        s.nc = nc
        s.stacks = [es]
        s.E = {}
        for name, h in (("pe", nc.tensor), ("act", nc.scalar), ("dve", nc.vector),
                        ("pool", nc.gpsimd), ("sp", nc.sync)):
            s.E[name] = Eng(name, h, es.enter_context(nc.semaphore("s_" + name)))
        s.dsems = {}
        s.nalloc = 0
        s.same_eng_sync = True
        for key in ["con", "x", "cf", "bf", "bi", "lng", "lnb", "mem", "cw", "cb", "out"] + [f"w{j}" for j in range(11)]:
            s.dsem(key)
        for E in s.E.values():
            nc.gpsimd.sem_clear(E.sem)
        for ds in s.dsems.values():
            nc.gpsimd.sem_clear(ds.sem)
        nc.all_engine_barrier()

    def push(s):
        st = ExitStack()
        st.__enter__()
        s.stacks.append(st)

    def pop(s):
        s.barrier()
        st = s.stacks.pop()
        st.__exit__(None, None, None)

    def sb(s, shape, dt, name="t"):
        s.nalloc += 1
        name = f"{name}_{s.nalloc}"
        return TT(s.stacks[-1].enter_context(s.nc.sbuf_tensor(name, list(shape), dt)), name)

    def ps(s, shape, dt, name="p"):
        s.nalloc += 1
        name = f"{name}_{s.nalloc}"
        t = TT(s.stacks[-1].enter_context(s.nc.psum_tensor(name, list(shape), dt)), name)
        t.psum = True
        return t

    def dsem(s, key):
        if key not in s.dsems:
            s.dsems[key] = DSem("dma_" + key, s.stacks[0].enter_context(s.nc.semaphore("d_" + key)))
        return s.dsems[key]

    def _wait(s, E, key, sem, val):
        if E.seen.get(key, 0) >= val:
            return
        E.h.wait_ge(sem, val)
        E.seen[key] = val

    def _waits(s, E, reads, writes):
        deps = []
        for t in reads:
            if t.w: deps.append(t.w)
            if getattr(t, "psum", False):
                deps.extend(t.r.values())
        for t in writes:
            if t.w: deps.append(t.w)
            deps.extend(t.r.values())
        for (key, sem, val, ds) in deps:
            if key == E.name and (E.name == "pe" or not s.same_eng_sync):
                continue
            if ds is not None:
                val = max(val, ds.cnt)
            s._wait(E, key, sem, val)

    def op(s, eng, fn, reads=(), writes=()):
        E = s.E[eng]
        s._waits(E, reads, writes)
        ins = fn(E.h)
        E.cnt += 1
        ins.then_inc(E.sem, 1)
        tok = (E.name, E.sem, E.cnt, None)
        for t in reads: t.r[E.name] = tok
        for t in writes:
            t.w = tok; t.r = {}
        return ins

    def dma(s, q, out, in_, reads=(), writes=(), key="g"):
        E = s.E[q]
        s._waits(E, reads, writes)
        ds = s.dsem(key)
        ins = E.h.dma_start(out=out, in_=in_)
        ds.cnt += 16
        ins.then_inc(ds.sem, 16)
        tok = (ds.key, ds.sem, ds.cnt, ds)
        for t in reads: t.r[ds.key] = tok
        for t in writes:
            t.w = tok; t.r = {}
        return ins

    def barrier(s):
        engs = list(s.E.values())
        for E in engs:
            for F in engs:
                if F.cnt > 0:
                    s._wait(E, F.name, F.sem, F.cnt)
            for ds in s.dsems.values():
                if ds.cnt > 0:
                    s._wait(E, ds.key, ds.sem, ds.cnt)


class Rot:
    def __init__(s, items):
        s.items, s.i = list(items), 0

    def next(s):
        x = s.items[s.i % len(s.items)]
        s.i += 1
        return x


C_ID, C_LE, C_LT, C_UN, C_GT, NCON = 0, 128, 256, 2304, 2432, 2560


def make_consts():
    con = np.zeros((128, NCON), np.float32)
    p = np.arange(128)[:, None]
    f = np.arange(128)[None, :]
    con[:, C_ID:C_ID + 128] = (p == f)
    con[:, C_LE:C_LE + 128] = (p <= f)
    t = np.arange(512)[None, :]
    for o in range(4):
        con[:, C_LT + 512 * o:C_LT + 512 * (o + 1)] = ((o * 128 + p) < t)
    con[:, C_UN:C_UN + 128] = -1.0 * (p >= f)
    con[:, C_GT:C_GT + 128] = (p > f)
    con4 = np.zeros((4, 4 * 128 + 4 + 128), np.float32)
    for h in range(4):
        con4[h, h * 128:(h + 1) * 128] = 1.0
        con4[h, 512 + h] = 1.0
    con4[:, 516:644] = 1.0
    rmask = np.ones((128, S), np.float32)
    rmask[:, ::128] = 0.0
    return con, con4, rmask


def build(nlayers=4, stop=None, first=0):
    nc = bass.Bass("TRN2", target_bir_lowering=False)
    Dm = {}

    def din(name, shape):
        Dm[name] = nc.dram_tensor(name, list(shape), F32, kind="ExternalInput").ap()

    din("x", [S, D]); din("mem", [256, D]); din("ln_g", [12, D]); din("ln_b", [12, D])
    din("mix_wo", [4, D, D]); din("sb_win", [D, 3072]); din("fox_win", [D, 3088]); din("fox_bf", [1, 16])
    din("ml_win", [D, 3080]); din("ml_bi", [4, 1]); din("ml_bf", [4, 1])
    din("gla_win", [D, 3088]); din("gla_wa2", [16, 512]); din("gla_ba", [128, 4]); din("gla_norm_g", [1, 256])
    din("xa_wq", [4, D, D]); din("xa_wkv", [4, D, 2 * D]); din("xa_wo", [4, D, D])
    din("ffn_up", [4, D, 2 * DFF]); din("ffn_conv", [4, 128, 44 * 3]); din("ffn_conv_b", [4, 128, 44])
    din("ffn_down", [4, DFF, D])
    din("con", [128, NCON]); din("con4", [4, 644]); din("rmask", [128, S])
    out = nc.dram_tensor("out", [S, D], F32, kind="ExternalOutput").ap()

    with ExitStack() as es:
        c = Ctx(nc, es)
        H = [c.sb([128, D], F32, f"H{i}") for i in range(NT)]
        HT = [c.sb([128, 8, 512], BF16, f"HT{q}") for q in range(4)]
        memT = c.sb([128, 8, 256], BF16, "memT")
        CON = c.sb([128, NCON], BF16, "CON")
        ONEN = c.sb([128, 128], BF16, "ONEN")
        ident = CON.t[:, C_ID:C_ID + 128]
        maskLE = CON.t[:, C_LE:C_LE + 128]
        uneg = CON.t[:, C_UN:C_UN + 128]

        def maskLT(o):
            return CON.t[:, C_LT + 512 * o:C_LT + 512 * (o + 1)]

        c.dma("pool", CON.t[:], Dm["con"][:, :], writes=[CON], key="con")
        c.op("dve", lambda e: e.memset(ONEN.t[:], -1.0), [], [ONEN])
        for i in range(NT):
            c.dma("sp", H[i].t[:], Dm["x"][i * 128:(i + 1) * 128, :], writes=[H[i]], key="x")

        wcount = [0]

        def load_w(slot, ap_dst, src):
            wcount[0] += 1
            c.dma("pool", ap_dst, src.rearrange("(kc p) n -> p kc n", p=128), writes=[slot], key=slot.key)

        def wslot(shape, key, name="w"):
            t = c.sb(shape, BF16, name)
            t.key = key
            return t

        def to_ht(i, hb_rot, pT_rot):
            hb = hb_rot.next()
            c.op("act", lambda e: e.activation(out=hb.t[:], in_=H[i].t[:], func=AF.Copy), [H[i]], [hb])
            pT = pT_rot.next()
            for k in range(8):
                c.op("pe", lambda e: e.transpose(out=pT.t[:, k, :], in_=hb.t[:, k * 128:(k + 1) * 128], identity=ident),
                     [hb, CON], [pT])
            q, j = divmod(i, 4)
            c.op("dve", lambda e: e.tensor_copy(out=HT[q].t[:, :, j * 128:(j + 1) * 128], in_=pT.t[:]), [pT], [HT[q]])

        def ln_tile(i, Sx, gB, bB, R):
            st = R["st"].next(); mv = R["mv"].next(); sm = R["sm"].next(); T1 = R["T1"].next()
            for hh in range(2):
                c.op("dve", lambda e: e.bn_stats(out=st.t[:, hh, :], in_=Sx.t[:, hh * 512:(hh + 1) * 512]), [Sx], [st])
            c.op("dve", lambda e: e.bn_aggr(out=mv.t[:], in_=st.t[:]), [st], [mv])
            c.op("act", lambda e: e.activation(out=sm.t[:, 0:1], in_=mv.t[:, 1:2], func=AF.Ln, bias=R["eps"].t[:, 0:1]), [mv, R["eps"]], [sm])
            c.op("act", lambda e: e.activation(out=sm.t[:, 1:2], in_=sm.t[:, 0:1], func=AF.Exp, scale=-0.5), [sm], [sm])
            c.op("dve", lambda e: e.tensor_scalar(out=sm.t[:, 2:3], in0=mv.t[:, 0:1], scalar1=sm.t[:, 1:2], scalar2=-1.0,
                                                  op0=ALU.mult, op1=ALU.mult), [mv, sm], [sm])
            c.op("act", lambda e: e.activation(out=T1.t[:], in_=Sx.t[:], func=AF.Identity, bias=sm.t[:, 2:3], scale=sm.t[:, 1:2]),
                 [Sx, sm], [T1])
            c.op("pool", lambda e: e.tensor_tensor(out=T1.t[:], in0=T1.t[:], in1=gB.t[:], op=ALU.mult), [T1, gB], [T1])
            c.op("pool", lambda e: e.tensor_tensor(out=H[i].t[:], in0=T1.t[:], in1=bB.t[:], op=ALU.add), [T1, bB], [H[i]])
            hb = R["hb"].next()
            c.op("act", lambda e: e.activation(out=hb.t[:], in_=H[i].t[:], func=AF.Copy), [H[i]], [hb])
            return hb

        def ht_from_hb(i, hb, pT_rot):
            pT = pT_rot.next()
            for k in range(8):
                c.op("pe", lambda e: e.transpose(out=pT.t[:, k, :], in_=hb.t[:, k * 128:(k + 1) * 128], identity=ident),
                     [hb, CON], [pT])
            q, j = divmod(i, 4)
            c.op("dve", lambda e: e.tensor_copy(out=HT[q].t[:, :, j * 128:(j + 1) * 128], in_=pT.t[:]), [pT], [HT[q]])

        def ln_res(lnidx):
            R = {}
            R["st"] = Rot([c.sb([128, 2, 6], F32, "st") for _ in range(2)])
            R["mv"] = Rot([c.sb([128, 2], F32, "mv") for _ in range(2)])
            R["sm"] = Rot([c.sb([128, 4], F32, "sm") for _ in range(2)])
            R["T1"] = Rot([c.sb([128, D], F32, "T1") for _ in range(2)])
            R["hb"] = Rot([c.sb([128, D], BF16, "hb") for _ in range(3)])
            R["pT"] = Rot([c.ps([128, 8, 128], BF16, "pT") for _ in range(2)])
            R["eps"] = c.sb([128, 1], F32, "eps")
            c.op("dve", lambda e: e.memset(R["eps"].t[:], LN_EPS), [], [R["eps"]])
            gB = c.sb([128, D], F32, "gB"); bB = c.sb([128, D], F32, "bB")
            c.dma("sp", gB.t[:], Dm["ln_g"][lnidx:lnidx + 1, :].partition_broadcast(128), writes=[gB], key="lng")
            c.dma("sp", bB.t[:], Dm["ln_b"][lnidx:lnidx + 1, :].partition_broadcast(128), writes=[bB], key="lnb")
            return R, gB, bB

        def out_proj_ln(KT, w_dram, lnidx):
            c.push()
            wo = [wslot([128, 8, 512], f"w{j}") for j in range(2)]
            for j in range(2):
                load_w(wo[j], wo[j].t[:], w_dram[:, j * 512:(j + 1) * 512])
            R, gB, bB = ln_res(lnidx)
            PB = Rot([c.ps([128, 512], F32, "po") for _ in range(4)])
            SX = Rot([c.sb([128, D], F32, "SX") for _ in range(3)])
            sxs = {}; hbs = {}

            def sA(i):
                Sx = SX.next(); sxs[i] = Sx
                for j in range(2):
                    pb = PB.next()
                    for k in range(8):
                        c.op("pe", lambda e: e.matmul(pb.t[:], lhsT=KT[k].t[:, i * 128:(i + 1) * 128], rhs=wo[j].t[:, k, :],
                                                      start=(k == 0), stop=(k == 7)), [KT[k], wo[j]], [pb])
                    c.op("dve", lambda e: e.scalar_tensor_tensor(out=Sx.t[:, j * 512:(j + 1) * 512], in0=H[i].t[:, j * 512:(j + 1) * 512],
                                                                 scalar=ALPHA, in1=pb.t[:], op0=ALU.mult, op1=ALU.add),
                         [H[i], pb], [Sx])

            for n in range(NT + 2):
                if n < NT: sA(n)
                if 0 <= n - 1 < NT: hbs[n - 1] = ln_tile(n - 1, sxs[n - 1], gB, bB, R)
                if 0 <= n - 2 < NT: ht_from_hb(n - 2, hbs[n - 2], R["pT"])
            c.pop()

        def proj_fm(pb, w, wap_fn, tq):
            for k in range(8):
                c.op("pe", lambda e: e.matmul(pb_ap(pb, wap_fn(k)), lhsT=wap_fn(k), rhs=HT[tq].t[:, k, :], start=(k == 0), stop=(k == 7)),
                     [w, HT[tq]], [pb])

        def pb_ap(pb, lhsT):
            m = lhsT.shape[-1]
            return pb.t[0:m, :]

        def proj_tm(pb, out_ap, w, wap_fn, i):
            q, j = divmod(i, 4)
            for k in range(8):
                c.op("pe", lambda e: e.matmul(out_ap, lhsT=HT[q].t[:, k, j * 128:(j + 1) * 128], rhs=wap_fn(k), start=(k == 0), stop=(k == 7)),
                     [w, HT[q]], [pb])

        def mixer_sb(win):
            c.push()
            YT = [c.sb([128, S], BF16, f"YT{k}") for k in range(8)]
            c.push()
            wq = [wslot([128, 8, 128], f"w{j}") for j in range(2)]
            wk = [wslot([128, 8, 128], f"w{2 + j}") for j in range(2)]
            wv = [wslot([128, 8, 128], f"w{4 + j}") for j in range(2)]
            qT = [c.sb([128, 2, S], BF16, "qTz") for _ in range(2)]
            for q__ in qT:
                c.op("pool", lambda e: e.memset(q__.t[:], 0.0), [], [q__])
            kT = [c.sb([128, S], BF16, "kT") for _ in range(2)]
            V = [c.sb([128, NT, 128], BF16, "V") for _ in range(2)]
            Yp = c.sb([128, NT, 128], BF16, "Yp")
            SPt = Rot([c.sb([128, 512], F32, "SP") for _ in range(3)])
            LmP = Rot([c.sb([128, 512], BF16, "Lm") for _ in range(5)])
            Ssum = Rot([c.sb([128, 512], BF16, "Ss") for _ in range(5)])
            WT = Rot([c.sb([128, 512], BF16, "WT") for _ in range(4)])
            PZ = Rot([c.ps([128, 512], F32, "pz") for _ in range(4)])
            PY = Rot([c.ps([128, 512], F32, "py") for _ in range(2)])
            PP = Rot([c.ps([128, 512], F32, "pp") for _ in range(1)])
            PTr = Rot([c.ps([128, 8, 128], BF16, "ptr") for _ in range(1)])
            for t_ in SPt.items:
                c.op("dve", lambda e: e.memset(t_.t[:], 0.0), [], [t_])

            def load_pair(p):
                b = p % 2
                load_w(wq[b], wq[b].t[:], win[:, p * 128:(p + 1) * 128])
                load_w(wk[b], wk[b].t[:], win[:, 1024 + p * 128:1024 + (p + 1) * 128])
                load_w(wv[b], wv[b].t[:], win[:, 2048 + p * 128:2048 + (p + 1) * 128])

            def proj_items(p):
                bb = p % 2
                items = []
                for tq in range(4):
                    def fq(tq=tq):
                        pb = PP.next()
                        proj_fm(pb, wq[bb], lambda k: wq[bb].t[:, k, :], tq)
                        for hq in range(2):
                            c.op("dve", lambda e: e.tensor_scalar(out=qT[bb].t[64 * hq:64 * hq + 64, hq, tq * 512:(tq + 1) * 512],
                                                                  in0=pb.t[64 * hq:64 * hq + 64, :], scalar1=0.125, scalar2=None, op0=ALU.mult),
                                 [pb], [qT[bb]])

                    def fk(tq=tq):
                        pb = PP.next()
                        proj_fm(pb, wk[bb], lambda k: wk[bb].t[:, k, :], tq)
                        c.op("dve", lambda e: e.tensor_copy(out=kT[bb].t[:, tq * 512:(tq + 1) * 512], in_=pb.t[:]), [pb], [kT[bb]])
                    items += [fq, fk]
                for i4 in range(4):
                    def fv(i4=i4):
                        pb = PP.next()
                        for jj in range(4):
                            proj_tm(pb, pb.t[:, jj * 128:(jj + 1) * 128], wv[bb], lambda k: wv[bb].t[:, k, :], i4 * 4 + jj)
                        c.op("dve", lambda e: e.tensor_copy(out=V[bb].t[:, i4 * 4:(i4 + 1) * 4, :],
                                                            in_=pb.t[:].rearrange("p (a b) -> p a b", a=4)), [pb], [V[bb]])
                    items.append(fv)
                return items

            load_pair(0)
            load_pair(1)
            for it in proj_items(0):
                it()
            for p in range(8):
                b = p % 2
                if p + 2 < 8:
                    load_pair(p + 2)
                nxt = proj_items(p + 1) if p + 1 < 8 else []
                steps = []
                for hh in range(2):
                    for g in range(4):
                        grp = {"py": None, "first": True, "ss": None}
                        for idx, kb in enumerate(range(4 * g + 3, -1, -1)):
                            steps.append(dict(hh=hh, g=g, kb=kb, grp=grp))

                def stA(st):
                    hh, g, kb, grp = st["hh"], st["g"], st["kb"], st["grp"]
                    lo, hi = 64 * hh, 64 * hh + 64
                    o = kb - 4 * g
                    pz = PZ.next(); st["pz"] = pz
                    c0 = max(o, 0) * 128
                    c.op("pe", lambda e: e.matmul(pz.t[:, c0:512], lhsT=kT[b].t[:, kb * 128:(kb + 1) * 128],
                                                  rhs=qT[b].t[:, hh, g * 512 + c0:(g + 1) * 512], start=True, stop=False, skip_group_check=True),
                         [kT[b], qT[b]], [pz])
                    SP_ = SPt.next(); Lm = LmP.next()
                    c.op("act", lambda e: e.activation(out=SP_.t[:, c0:512], in_=pz.t[:, c0:512], func=AF.Exp), [pz], [SP_])
                    c.op("act", lambda e: e.activation(out=SP_.t[:, c0:512], in_=SP_.t[:, c0:512], func=AF.Ln, bias=1.0), [SP_], [SP_])
                    if o >= 0:
                        c.op("dve", lambda e: e.tensor_tensor(out=Lm.t[:], in0=SP_.t[:], in1=maskLT(o), op=ALU.mult), [SP_, CON], [Lm])
                    else:
                        c.op("dve", lambda e: e.tensor_copy(out=Lm.t[:], in_=SP_.t[:]), [SP_], [Lm])
                    st["Lm"] = Lm
                    st["ss_prev"] = grp["ss"]
                    if kb > 0:
                        if grp["ss"] is None:
                            grp["ss"] = Lm
                        else:
                            ss_new = Ssum.next()
                            sp0 = grp["ss"]
                            c.op("pool", lambda e: e.tensor_tensor(out=ss_new.t[:], in0=sp0.t[:], in1=Lm.t[:], op=ALU.add), [sp0, Lm], [ss_new])
                            grp["ss"] = ss_new

                def stB(st):
                    g, kb = st["g"], st["kb"]
                    o = kb - 4 * g
                    pz, Lm, ss_prev = st["pz"], st["Lm"], st["ss_prev"]
                    c0 = max(o, 0) * 128
                    c.op("pe", lambda e: e.matmul(pz.t[:, c0:512], lhsT=uneg, rhs=Lm.t[:, c0:512], start=False, stop=(ss_prev is None), skip_group_check=True),
                         [CON, Lm], [pz])
                    if ss_prev is not None:
                        c.op("pe", lambda e: e.matmul(pz.t[:, c0:512], lhsT=ONEN.t[:], rhs=ss_prev.t[:, c0:512], start=False, stop=True, skip_group_check=True),
                             [ONEN, ss_prev], [pz])
                    W_ = WT.next(); st["W"] = W_
                    c.op("act", lambda e: e.activation(out=W_.t[:, c0:512], in_=pz.t[:, c0:512], func=AF.Exp), [pz], [W_])
                    if o >= 0:
                        c.op("dve", lambda e: e.tensor_tensor(out=W_.t[:, c0:512], in0=W_.t[:, c0:512], in1=maskLT(o)[:, c0:512], op=ALU.mult), [W_, CON], [W_])

                def stC(st):
                    hh, g, kb, grp = st["hh"], st["g"], st["kb"], st["grp"]
                    lo, hi = 64 * hh, 64 * hh + 64
                    if grp["py"] is None:
                        grp["py"] = PY.next()
                    py = grp["py"]; W_ = st["W"]
                    for jq in range(4):
                        if 4 * g + jq < kb:
                            continue
                        c.op("pe", lambda e: e.matmul(py.t[:, jq * 64:(jq + 1) * 64], lhsT=W_.t[:, jq * 128:(jq + 1) * 128],
                                                      rhs=V[b].t[:, kb, lo:hi], start=grp["first"], stop=(kb == 0 and jq == 3),
                                                      skip_group_check=True), [W_, V[b]], [py])
                        grp["first"] = False
                    if kb == 0:
                        c.op("dve", lambda e: e.tensor_copy(out=Yp.t[:, 4 * g:4 * g + 4, lo:hi],
                                                            in_=py.t[:, 0:256].rearrange("p (a b) -> p a b", a=4)), [py], [Yp])

                ns = len(steps)
                for n in range(ns + 3):
                    if n < ns: stA(steps[n])
                    if 0 <= n - 2 < ns: stB(steps[n - 2])
                    if 0 <= n - 3 < ns: stC(steps[n - 3])
                    if n % 6 == 3 and nxt:
                        nxt.pop(0)()
                for it in nxt:
                    it()
                for i2 in range(2):
                    ptr = PTr.next()
                    for jj in range(8):
                        i = i2 * 8 + jj
                        c.op("pe", lambda e: e.transpose(out=ptr.t[:, jj, :], in_=Yp.t[:, i, :], identity=ident), [Yp, CON], [ptr])
                    c.op("dve", lambda e: e.tensor_copy(out=YT[p].t[:, i2 * 1024:(i2 + 1) * 1024],
                                                        in_=ptr.t[:].rearrange("p a b -> p (a b)")), [ptr], [YT[p]])
            c.pop()
            return YT


        def mixer_fox(win, bf_dram):
            c.push()
            YT = [c.sb([128, S], BF16, f"YT{k}") for k in range(8)]
            c.push()
            wq = [wslot([128, 8, 128], f"w{j}") for j in range(2)]
            wk = [wslot([128, 8, 128], f"w{2 + j}") for j in range(2)]
            wv = [wslot([128, 8, 128], f"w{4 + j}") for j in range(2)]
            wf = wslot([128, 8, 16], "w6")
            qT = [c.sb([128, 2, S], BF16, "qTz") for _ in range(2)]
            for q__ in qT:
                c.op("pool", lambda e: e.memset(q__.t[:], 0.0), [], [q__])
            kT = [c.sb([128, S], BF16, "kT") for _ in range(2)]
            V = [c.sb([128, NT, 2, 65], BF16, "V") for _ in range(2)]
            Yp = c.sb([128, NT, 128], BF16, "Yp")
            WT = Rot([c.sb([128, 512], BF16, "WT") for _ in range(4)])
            Bh = Rot([c.sb([128, 8, 16], F32, "Bh") for _ in range(4)])
            RD = Rot([c.sb([128, 4, 1], F32, "rd") for _ in range(2)])
            lfc = c.sb([128, 256], F32, "lfc"); tmp = c.sb([128, 256], F32, "ftmp")
            T2 = c.sb([128, 256], F32, "T2"); PSc = c.sb([128, 256], F32, "PSc")
            ugt = c.sb([128, 128], F32, "ugt"); onef = c.sb([128, 128], F32, "onef")
            bfB = c.sb([128, 16], F32, "bfB")
            PZ = Rot([c.ps([128, 512], F32, "pz") for _ in range(4)])
            PY = Rot([c.ps([128, 512], F32, "py") for _ in range(2)])
            PP = Rot([c.ps([128, 512], F32, "pp") for _ in range(1)])
            PTr = Rot([c.ps([128, 8, 128], BF16, "ptr") for _ in range(1)])
            for b in range(2):
                c.op("dve", lambda e: e.memset(V[b].t[:, :, :, 64:65], 1.0), [], [V[b]])
            c.op("dve", lambda e: e.memset(onef.t[:], 1.0), [], [onef])
            c.dma("sp", ugt.t[:], Dm["con"][:, C_GT:C_GT + 128], writes=[ugt], key="cf")
            c.dma("sp", bfB.t[:], bf_dram[0:1, :].partition_broadcast(128), writes=[bfB], key="bf")
            load_w(wf, wf.t[:], win[:, 3072:3088])

            def load_pair(p):
                b = p % 2
                load_w(wq[b], wq[b].t[:], win[:, p * 128:(p + 1) * 128])
                load_w(wk[b], wk[b].t[:], win[:, 1024 + p * 128:1024 + (p + 1) * 128])
                load_w(wv[b], wv[b].t[:], win[:, 2048 + p * 128:2048 + (p + 1) * 128])

            load_pair(0)
            for i in range(NT):
                pb = PP.next()
                proj_tm(pb, pb.t[:, 0:16], wf, lambda k: wf.t[:, k, :], i)
                c.op("dve", lambda e: e.tensor_tensor(out=lfc.t[:, i * 16:(i + 1) * 16], in0=pb.t[:, 0:16], in1=bfB.t[:], op=ALU.add),
                     [pb, bfB], [lfc])
            c.op("act", lambda e: e.activation(out=tmp.t[:], in_=lfc.t[:], func=AF.Exp, scale=-1.0), [lfc], [tmp])
            c.op("act", lambda e: e.activation(out=tmp.t[:], in_=tmp.t[:], func=AF.Ln, bias=1.0), [tmp], [tmp])
            c.op("dve", lambda e: e.tensor_scalar(out=lfc.t[:], in0=tmp.t[:], scalar1=-1.0, scalar2=None, op0=ALU.mult), [tmp], [lfc])
            pb = PP.next()
            c.op("pe", lambda e: e.matmul(pb.t[:, 0:256], lhsT=onef.t[:], rhs=lfc.t[:], start=True, stop=True), [onef, lfc], [pb])
            c.op("dve", lambda e: e.tensor_copy(out=PSc.t[:, 0:16], in_=pb.t[:, 0:16]), [pb], [PSc])
            for jb in range(1, 16):
                c.op("dve", lambda e: e.tensor_tensor(out=PSc.t[:, jb * 16:(jb + 1) * 16], in0=PSc.t[:, (jb - 1) * 16:jb * 16],
                                                      in1=pb.t[:, jb * 16:(jb + 1) * 16], op=ALU.add), [pb, PSc], [PSc])
            pb = PP.next()
            c.op("pe", lambda e: e.matmul(pb.t[:, 0:256], lhsT=ugt.t[:], rhs=lfc.t[:], start=True, stop=True), [ugt, lfc], [pb])
            c.op("dve", lambda e: e.tensor_tensor(out=T2.t[:], in0=pb.t[:, 0:256], in1=PSc.t[:], op=ALU.subtract), [pb, PSc], [T2])
            T2v = T2.t[:].rearrange("p (a b) -> p a b", a=16)

            def proj_items(p):
                bb = p % 2
                items = []
                for tq in range(4):
                    def fq(tq=tq):
                        pb = PP.next()
                        proj_fm(pb, wq[bb], lambda k: wq[bb].t[:, k, :], tq)
                        for hq in range(2):
                            c.op("dve", lambda e: e.tensor_scalar(out=qT[bb].t[64 * hq:64 * hq + 64, hq, tq * 512:(tq + 1) * 512],
                                                                  in0=pb.t[64 * hq:64 * hq + 64, :], scalar1=0.125, scalar2=None, op0=ALU.mult),
                                 [pb], [qT[bb]])

                    def fk(tq=tq):
                        pb = PP.next()
                        proj_fm(pb, wk[bb], lambda k: wk[bb].t[:, k, :], tq)
                        c.op("dve", lambda e: e.tensor_copy(out=kT[bb].t[:, tq * 512:(tq + 1) * 512], in_=pb.t[:]), [pb], [kT[bb]])
                    items += [fq, fk]
                for i4 in range(4):
                    def fv(i4=i4):
                        pb = PP.next()
                        for jj in range(4):
                            proj_tm(pb, pb.t[:, jj * 128:(jj + 1) * 128], wv[bb], lambda k: wv[bb].t[:, k, :], i4 * 4 + jj)
                        for hh in range(2):
                            c.op("act", lambda e: e.activation(out=V[bb].t[:, i4 * 4:(i4 + 1) * 4, hh, 0:64],
                                                               in_=pb.t[:].rearrange("p (a h d) -> p a h d", a=4, h=2)[:, :, hh, :], func=AF.Copy),
                                 [pb], [V[bb]])
                    items.append(fv)
                return items

            PSm = c.sb([128, 8, 16], F32, "PSm")
            PSv = PSc.t[:].rearrange("p (q two h) -> p q two h", two=2, h=16)
            c.op("dve", lambda e: e.tensor_tensor(out=PSm.t[:], in0=PSv[:, :, 0, :], in1=PSv[:, :, 1, :], op=ALU.add), [PSc], [PSm])
            c.op("dve", lambda e: e.tensor_scalar(out=PSm.t[:], in0=PSm.t[:], scalar1=0.5, scalar2=None, op0=ALU.mult), [PSm], [PSm])
            load_pair(1)
            for it in proj_items(0):
                it()
            for p in range(8):
                b = p % 2
                if p + 2 < 8:
                    load_pair(p + 2)
                nxt = proj_items(p + 1) if p + 1 < 8 else []
                steps = []
                for hh in range(2):
                    h = 2 * p + hh
                    B_ = Bh.next()
                    for pr in range(8):
                        c.op("dve", lambda e: e.tensor_scalar(out=B_.t[:, pr, :], in0=T2v[:, :, h], scalar1=PSm.t[:, pr, h:h + 1],
                                                              scalar2=None, op0=ALU.add), [T2, PSm], [B_])
                    for g in range(4):
                        grp = {"py": None, "first": True}
                        for kb in range(4 * g + 4):
                            steps.append(dict(hh=hh, g=g, kb=kb, grp=grp, B=B_))

                def fA(st):
                    hh, g, kb = st["hh"], st["g"], st["kb"]
                    lo, hi = 64 * hh, 64 * hh + 64
                    pz = PZ.next(); st["pz"] = pz
                    c.op("pe", lambda e: e.matmul(pz.t[:], lhsT=kT[b].t[:, kb * 128:(kb + 1) * 128],
                                                  rhs=qT[b].t[:, hh, g * 512:(g + 1) * 512], start=True, stop=True), [kT[b], qT[b]], [pz])

                def fB(st):
                    g, kb, pz, B_ = st["g"], st["kb"], st["pz"], st["B"]
                    W_ = WT.next(); st["W"] = W_
                    for p2 in range(2):
                        if 4 * g + 2 * p2 + 1 < kb:
                            continue
                        pr = 2 * g + p2
                        c.op("act", lambda e: e.activation(out=W_.t[:, p2 * 256:(p2 + 1) * 256], in_=pz.t[:, p2 * 256:(p2 + 1) * 256],
                                                           func=AF.Exp, bias=B_.t[:, pr, kb:kb + 1]), [pz, B_], [W_])
                    for jq in range(4):
                        Q = 4 * g + jq
                        if Q == kb:
                            c.op("pool", lambda e: e.tensor_tensor(out=W_.t[:, jq * 128:(jq + 1) * 128], in0=W_.t[:, jq * 128:(jq + 1) * 128],
                                                                   in1=maskLE, op=ALU.mult), [W_, CON], [W_])

                def fC(st):
                    hh, g, kb, grp, W_ = st["hh"], st["g"], st["kb"], st["grp"], st["W"]
                    lo, hi = 64 * hh, 64 * hh + 64
                    if grp["py"] is None:
                        grp["py"] = PY.next()
                    py = grp["py"]
                    for jq in range(4):
                        Q = 4 * g + jq
                        if Q < kb:
                            continue
                        c.op("pe", lambda e: e.matmul(py.t[:, jq * 65:jq * 65 + 65], lhsT=W_.t[:, jq * 128:(jq + 1) * 128],
                                                      rhs=V[b].t[:, kb, hh, :], start=grp["first"], stop=(kb == 4 * g + 3 and jq == 3),
                                                      skip_group_check=True), [W_, V[b]], [py])
                        grp["first"] = False
                    if kb == 4 * g + 3:
                        rd = RD.next()
                        pyv = py.t[:, 0:260].rearrange("p (a b) -> p a b", a=4)
                        c.op("dve", lambda e: e.reciprocal(out=rd.t[:], in_=pyv[:, :, 64:65]), [py], [rd])
                        for jq in range(4):
                            c.op("dve", lambda e: e.tensor_scalar(out=Yp.t[:, 4 * g + jq, lo:hi], in0=py.t[:, jq * 65:jq * 65 + 64],
                                                                  scalar1=rd.t[:, jq, :], scalar2=None, op0=ALU.mult), [py, rd], [Yp])

                ns = len(steps)
                for n in range(ns + 3):
                    if n < ns: fA(steps[n])
                    if 0 <= n - 2 < ns: fB(steps[n - 2])
                    if 0 <= n - 3 < ns: fC(steps[n - 3])
                    if n % 6 == 3 and nxt:
                        nxt.pop(0)()
                for it in nxt:
                    it()
                for i2 in range(2):
                    ptr = PTr.next()
                    for jj in range(8):
                        i = i2 * 8 + jj
                        c.op("pe", lambda e: e.transpose(out=ptr.t[:, jj, :], in_=Yp.t[:, i, :], identity=ident), [Yp, CON], [ptr])
                    c.op("dve", lambda e: e.tensor_copy(out=YT[p].t[:, i2 * 1024:(i2 + 1) * 1024],
                                                        in_=ptr.t[:].rearrange("p a b -> p (a b)")), [ptr], [YT[p]])
            c.pop()
            return YT


        def mixer_mlstm(win):
            c.push()
            YT = [c.sb([128, S], BF16, f"YT{k}") for k in range(8)]
            aT = c.sb([4, S], F32, "aT"); nG = c.sb([4, S], F32, "nG")
            cols = c.sb([128, 16, 16], F32, "cols")
            c4 = c.sb([4, 644], F32, "c4")
            c.dma("sp", c4.t[:], Dm["con4"][:, :], writes=[c4], key="cf")
            I4 = c4.t[:, 512:516]; ones4 = c4.t[:, 516:644]
            c.push()
            wi = wslot([128, 8, 4], "w6"); wf = wslot([128, 8, 4], "w7")
            load_w(wi, wi.t[:], win[:, 3072:3076]); load_w(wf, wf.t[:], win[:, 3076:3080])
            bi = c.sb([4, 1], F32, "bi"); bfv = c.sb([4, 1], F32, "bfv")
            c.dma("sp", bi.t[:], Dm["ml_bi"][:, :], writes=[bi], key="bi")
            c.dma("sp", bfv.t[:], Dm["ml_bf"][:, :], writes=[bfv], key="bf")
            c.op("dve", lambda e: e.tensor_scalar(out=bfv.t[:], in0=bfv.t[:], scalar1=-1.0, scalar2=None, op0=ALU.mult), [bfv], [bfv])
            iT = c.sb([4, S], F32, "iT"); t4 = c.sb([4, S], F32, "t4"); Fp = c.sb([4, S], F32, "Fp"); G_ = c.sb([4, S], F32, "G")
            GP = c.sb([4, 16, 3], F32, "GP"); Dg = c.sb([4, 16, 12], F32, "Dg")
            PP = Rot([c.ps([128, 512], F32, "pp") for _ in range(2)])
            PCo = c.ps([128, 512], F32, "pco")
            for tq in range(4):
                pb = PP.next()
                proj_fm(pb, wi, lambda k: wi.t[:, k, :], tq)
                c.op("act", lambda e: e.activation(out=iT.t[:, tq * 512:(tq + 1) * 512], in_=pb.t[0:4, :], func=AF.Identity, bias=bi.t[:, 0:1]),
                     [pb, bi], [iT])
                pb = PP.next()
                proj_fm(pb, wf, lambda k: wf.t[:, k, :], tq)
                c.op("act", lambda e: e.activation(out=t4.t[:, tq * 512:(tq + 1) * 512], in_=pb.t[0:4, :], func=AF.Exp, bias=bfv.t[:, 0:1], scale=-1.0),
                     [pb, bfv], [t4])
            c.op("act", lambda e: e.activation(out=t4.t[:], in_=t4.t[:], func=AF.Ln, bias=1.0), [t4], [t4])
            c.op("dve", lambda e: e.tensor_tensor_scan(out=Fp.t[:], data0=t4.t[:], data1=t4.t[:], initial=0.0, op0=ALU.add, op1=ALU.max),
                 [t4], [Fp])
            c.op("dve", lambda e: e.tensor_tensor(out=aT.t[:], in0=iT.t[:], in1=Fp.t[:], op=ALU.add), [iT, Fp], [aT])
            c.op("dve", lambda e: e.tensor_tensor_scan(out=G_.t[:], data0=aT.t[:], data1=aT.t[:], initial=0.0, op0=ALU.max, op1=ALU.max),
                 [aT], [G_])
            c.op("dve", lambda e: e.tensor_scalar(out=nG.t[:], in0=G_.t[:], scalar1=-1.0, scalar2=None, op0=ALU.mult), [G_], [nG])
            c.op("dve", lambda e: e.tensor_tensor(out=iT.t[:], in0=Fp.t[:], in1=G_.t[:], op=ALU.subtract), [Fp, G_], [iT])
            nM = iT
            Gend = G_.t[:].rearrange("p (c t) -> p c t", t=128)[:, :, 127]
            c.op("dve", lambda e: e.memset(GP.t[:], 0.0), [], [GP])
            c.op("dve", lambda e: e.tensor_copy(out=GP.t[:, 1:16, 0], in_=G_.t[:].rearrange("p (c t) -> p c t", t=128)[:, 0:15, 127]), [G_], [GP])
            c.op("dve", lambda e: e.tensor_scalar(out=GP.t[:, :, 1], in0=Gend, scalar1=-1.0, scalar2=None, op0=ALU.mult), [G_], [GP])
            c.op("dve", lambda e: e.tensor_tensor(out=GP.t[:, :, 2], in0=GP.t[:, :, 0], in1=GP.t[:, :, 1], op=ALU.add), [GP], [GP])
            for cc in range(16):
                for j in range(3):
                    c.op("dve", lambda e: e.tensor_scalar(out=Dg.t[:, cc, j * 4:(j + 1) * 4], in0=I4, scalar1=GP.t[:, cc, j:j + 1], scalar2=None,
                                                          op0=ALU.mult), [c4, GP], [Dg])
            for cc in range(16):
                sl = slice(cc * 128, (cc + 1) * 128)
                o0 = cc * 16
                mmx = lambda oc, l, r, st, sp_: c.op("pe", lambda e: e.matmul(PCo.t[:, o0 + oc:o0 + oc + 4], lhsT=l, rhs=r, start=st, stop=sp_,
                                                                              skip_group_check=True), [nG, aT, nM, c4, Dg], [PCo])
                mmx(0, nG.t[:, sl], I4, True, False); mmx(0, ones4, Dg.t[:, cc, 0:4], False, True)
                mmx(4, nM.t[:, sl], I4, True, True)
                mmx(8, aT.t[:, sl], I4, True, False); mmx(8, ones4, Dg.t[:, cc, 4:8], False, True)
                mmx(12, ones4, Dg.t[:, cc, 8:12], True, True)
            c.op("act", lambda e: e.activation(out=cols.t[:].rearrange("p a b -> p (a b)"), in_=PCo.t[:, 0:256], func=AF.Exp), [PCo], [cols])
            c.pop()
            c.push()
            wq = [wslot([128, 8, 128], f"w{j}") for j in range(2)]
            wk = [wslot([128, 8, 128], f"w{2 + j}") for j in range(2)]
            wv = [wslot([128, 8, 256], f"w{4 + j}") for j in range(2)]
            wo_ = [wslot([128, 8, 256], f"w{6 + j}") for j in range(2)]
            qTc = Rot([c.sb([128, 128], BF16, "qTc") for _ in range(4)])
            kTc = Rot([c.sb([128, 128], BF16, "kTc") for _ in range(2)])
            kw = Rot([c.sb([128, 128], BF16, "kw") for _ in range(4)])
            Va = Rot([c.sb([128, 257], BF16, "Va") for _ in range(4)])
            Wt = Rot([c.sb([128, 128], F32, "Wt") for _ in range(3)])
            PTt = Rot([c.sb([128, 128], BF16, "PTt") for _ in range(4)])
            ONEF = c.sb([128, 256], F32, "ONEF")
            c.op("pool", lambda e: e.memset(ONEF.t[:], -1.0), [], [ONEF])
            tI = Rot([c.sb([128, 257], F32, "tI") for _ in range(2)])
            tot = Rot([c.sb([128, 257], F32, "tot") for _ in range(2)])
            sg = Rot([c.sb([128, 256], F32, "sg") for _ in range(4)])
            yh = Rot([c.sb([128, 256], BF16, "yh") for _ in range(3)])
            dn = Rot([c.sb([128, 2], F32, "dn") for _ in range(2)])
            Cf = c.sb([128, 257], F32, "Cf"); Cb = c.sb([128, 257], BF16, "Cb")
            PP = Rot([c.ps([128, 512], F32, "pp") for _ in range(2)])
            PW = Rot([c.ps([128, 512], F32, "pw") for _ in range(1)])
            PS2 = Rot([c.ps([128, 512], F32, "ps2") for _ in range(1)])
            PN = Rot([c.ps([128, 512], F32, "pn") for _ in range(2)])
            PC = c.ps([128, 512], F32, "pc")
            PTr = c.ps([128, 8, 128], BF16, "ptr")
            for v_ in Va.items:
                c.op("dve", lambda e: e.memset(v_.t[:, 256:257], 1.0), [], [v_])

            def load_head(h):
                b = h % 2
                load_w(wq[b], wq[b].t[:], win[:, h * 128:(h + 1) * 128])
                load_w(wk[b], wk[b].t[:], win[:, 512 + h * 128:512 + (h + 1) * 128])
                load_w(wv[b], wv[b].t[:], win[:, 1024 + h * 256:1024 + (h + 1) * 256])
                load_w(wo_[b], wo_[b].t[:], win[:, 2048 + h * 256:2048 + (h + 1) * 256])

            import os
            load_head(0)
            for h in range(int(os.environ.get('ML_HEADS', '4'))):
                b = h % 2
                if h + 1 < 4:
                    load_head(h + 1)
                Esel = c4.t[:, h * 128:(h + 1) * 128]
                def mS1(cc):
                    q_, j_ = divmod(cc, 4)
                    sl = slice(cc * 128, (cc + 1) * 128)
                    hsl = lambda k: HT[q_].t[:, k, j_ * 128:(j_ + 1) * 128]
                    pw = PW.next()
                    c.op("pe", lambda e: e.matmul(pw.t[:, 0:128], lhsT=aT.t[:, sl], rhs=Esel, start=True, stop=False), [aT, c4], [pw])
                    c.op("pe", lambda e: e.matmul(pw.t[:, 0:128], lhsT=Esel, rhs=nG.t[:, sl], start=False, stop=True), [nG, c4], [pw])
                    w_ = Wt.next()
                    c.op("act", lambda e: e.activation(out=w_.t[:], in_=pw.t[:, 0:128], func=AF.Exp), [pw], [w_])
                    c.op("dve", lambda e: e.tensor_tensor(out=w_.t[:], in0=w_.t[:], in1=maskLE, op=ALU.mult), [w_, CON], [w_])
                    pb = PP.next()
                    for k in range(8):
                        c.op("pe", lambda e: e.matmul(pb.t[:, 0:128], lhsT=wq[b].t[:, k, :], rhs=hsl(k), start=(k == 0), stop=(k == 7)),
                             [wq[b], HT[q_]], [pb])
                    for k in range(8):
                        c.op("pe", lambda e: e.matmul(pb.t[:, 128:256], lhsT=wk[b].t[:, k, :], rhs=hsl(k), start=(k == 0), stop=(k == 7),
                                                      skip_group_check=True), [wk[b], HT[q_]], [pb])
                    qc = qTc.next(); kc = kTc.next()
                    c.op("act", lambda e: e.activation(out=qc.t[:], in_=pb.t[:, 0:128], func=AF.Copy, scale=128.0 ** -0.5), [pb], [qc])
                    c.op("dve", lambda e: e.tensor_copy(out=kc.t[:], in_=pb.t[:, 128:256]), [pb], [kc])
                    pb = PP.next()
                    for k in range(8):
                        c.op("pe", lambda e: e.matmul(pb.t[:, 0:128], lhsT=hsl(k), rhs=wk[b].t[:, k, :], start=(k == 0), stop=(k == 7)),
                             [wk[b], HT[q_]], [pb])
                    for k in range(8):
                        c.op("pe", lambda e: e.matmul(pb.t[:, 128:384], lhsT=hsl(k), rhs=wv[b].t[:, k, :], start=(k == 0), stop=(k == 7),
                                                      skip_group_check=True), [wv[b], HT[q_]], [pb])
                    kw_ = kw.next(); va = Va.next()
                    c.op("act", lambda e: e.activation(out=kw_.t[:], in_=pb.t[:, 0:128], func=AF.Copy, scale=cols.t[:, cc, 8 + h:9 + h]),
                         [pb, cols], [kw_])
                    c.op("dve", lambda e: e.tensor_copy(out=va.t[:, 0:256], in_=pb.t[:, 128:384]), [pb], [va])
                    ps_ = PS2.next()
                    c.op("pe", lambda e: e.matmul(ps_.t[:, 0:128], lhsT=kc.t[:], rhs=qc.t[:], start=True, stop=True), [kc, qc], [ps_])
                    pt = PTt.next()
                    c.op("dve", lambda e: e.tensor_tensor(out=pt.t[:], in0=ps_.t[:, 0:128], in1=w_.t[:], op=ALU.mult), [ps_, w_], [pt])
                    pb = PP.next()
                    for k in range(8):
                        c.op("pe", lambda e: e.matmul(pb.t[:, 0:256], lhsT=hsl(k), rhs=wo_[b].t[:, k, :], start=(k == 0), stop=(k == 7)),
                             [wo_[b], HT[q_]], [pb])
                    s_ = sg.next()
                    c.op("act", lambda e: e.activation(out=s_.t[:], in_=pb.t[:, 0:256], func=AF.Tanh, scale=0.5), [pb], [s_])
                    c.op("dve", lambda e: e.tensor_scalar(out=s_.t[:], in0=s_.t[:], scalar1=0.5, scalar2=0.5, op0=ALU.mult, op1=ALU.add), [s_], [s_])
                    return dict(qc=qc, kw=kw_, va=va, pt=pt, s=s_)

                def mS2(cc, d, prev_y):
                    sl = slice(cc * 128, (cc + 1) * 128)
                    qc, kw_, va, pt, s_ = d["qc"], d["kw"], d["va"], d["pt"], d["s"]
                    if cc > 0:
                        pi = PN.next()
                        c.op("pe", lambda e: e.matmul(pi.t[:, 0:257], lhsT=qc.t[:], rhs=Cb.t[:], start=True, stop=True), [qc, Cb], [pi])
                    c.op("pe", lambda e: e.matmul(PC.t[:, 0:257], lhsT=kw_.t[:], rhs=va.t[:], start=True, stop=True), [kw_, va], [PC])
                    pn = PN.next()
                    c.op("pe", lambda e: e.matmul(pn.t[:, 0:257], lhsT=pt.t[:], rhs=va.t[:], start=True, stop=True), [pt, va], [pn])
                    if prev_y is not None:
                        pcc, py_ = prev_y
                        for j2 in range(2):
                            c.op("pe", lambda e: e.transpose(out=PTr.t[:, j2, :], in_=py_.t[:, j2 * 128:(j2 + 1) * 128], identity=ident), [py_, CON], [PTr])
                        for j2 in range(2):
                            c.op("act", lambda e: e.activation(out=YT[2 * h + j2].t[:, pcc * 128:(pcc + 1) * 128], in_=PTr.t[:, j2, :], func=AF.Copy),
                                 [PTr], [YT[2 * h + j2]])
                    if cc > 0:
                        c.op("dve", lambda e: e.scalar_tensor_tensor(out=Cf.t[:], in0=Cf.t[:], scalar=cols.t[:, cc, 12 + h:13 + h], in1=PC.t[:, 0:257],
                                                                     op0=ALU.mult, op1=ALU.add), [Cf, cols, PC], [Cf])
                    else:
                        c.op("dve", lambda e: e.tensor_copy(out=Cf.t[:], in_=PC.t[:, 0:257]), [PC], [Cf])
                    to_ = tot.next()
                    if cc > 0:
                        ti = tI.next()
                        c.op("act", lambda e: e.activation(out=ti.t[:], in_=pi.t[:, 0:257], func=AF.Copy, scale=cols.t[:, cc, h:h + 1]),
                             [pi, cols], [ti])
                    c.op("act", lambda e: e.activation(out=Cb.t[:], in_=Cf.t[:], func=AF.Copy), [Cf], [Cb])
                    if cc > 0:
                        c.op("dve", lambda e: e.tensor_tensor(out=to_.t[:], in0=ti.t[:], in1=pn.t[:, 0:257], op=ALU.add), [ti, pn], [to_])
                    else:
                        c.op("dve", lambda e: e.tensor_copy(out=to_.t[:], in_=pn.t[:, 0:257]), [pn], [to_])
                    d_ = dn.next()
                    c.op("dve", lambda e: e.tensor_scalar(out=d_.t[:, 0:1], in0=to_.t[:, 256:257], scalar1=-1.0, scalar2=None, op0=ALU.mult), [to_], [d_])
                    c.op("dve", lambda e: e.tensor_tensor(out=d_.t[:, 0:1], in0=d_.t[:, 0:1], in1=to_.t[:, 256:257], op=ALU.max), [to_, d_], [d_])
                    c.op("dve", lambda e: e.tensor_tensor(out=d_.t[:, 0:1], in0=d_.t[:, 0:1], in1=cols.t[:, cc, 4 + h:5 + h], op=ALU.max), [cols, d_], [d_])
                    c.op("dve", lambda e: e.reciprocal(out=d_.t[:, 1:2], in_=d_.t[:, 0:1]), [d_], [d_])
                    y_ = yh.next()
                    c.op("dve", lambda e: e.scalar_tensor_tensor(out=y_.t[:], in0=to_.t[:, 0:256], scalar=d_.t[:, 1:2], in1=s_.t[:],
                                                                 op0=ALU.mult, op1=ALU.mult), [to_, d_, s_], [y_])
                    return (cc, y_)

                def mFlush(prev_y):
                    pcc, py_ = prev_y
                    for j2 in range(2):
                        c.op("pe", lambda e: e.transpose(out=PTr.t[:, j2, :], in_=py_.t[:, j2 * 128:(j2 + 1) * 128], identity=ident), [py_, CON], [PTr])
                    for j2 in range(2):
                        c.op("act", lambda e: e.activation(out=YT[2 * h + j2].t[:, pcc * 128:(pcc + 1) * 128], in_=PTr.t[:, j2, :], func=AF.Copy),
                             [PTr], [YT[2 * h + j2]])

                dd = {}; prev_y = None
                for n in range(16 + 2):
                    if n < 16:
                        dd[n] = mS1(n)
                    if 0 <= n - 2 < 16:
                        prev_y = mS2(n - 2, dd.pop(n - 2), prev_y)
                mFlush(prev_y)
            c.pop()
            return YT

        def mixer_gla(win):
            c.push()
            YT = [c.sb([128, S], BF16, f"YT{k}") for k in range(8)]
            c.push()
            wa = wslot([128, 8, 16], "w8")
            wa2 = wslot([16, 512], "w9")
            load_w(wa, wa.t[:], win[:, 3072:3088])
            c.dma("pool", wa2.t[:], Dm["gla_wa2"][:, :], writes=[wa2], key="w9")
            rm = wslot([128, S], "w10")
            c.dma("pool", rm.t[:], Dm["rmask"][:, :], writes=[rm], key="w10")
            nba = c.sb([128, 4], F32, "nba")
            c.dma("sp", nba.t[:], Dm["gla_ba"][:, :], writes=[nba], key="bi")
            c.op("dve", lambda e: e.tensor_scalar(out=nba.t[:], in0=nba.t[:], scalar1=-1.0, scalar2=None, op0=ALU.mult), [nba], [nba])
            gB = c.sb([128, 256], F32, "gB256")
            c.dma("sp", gB.t[:], Dm["gla_norm_g"][0:1, :].partition_broadcast(128), writes=[gB], key="bf")
            c.op("dve", lambda e: e.tensor_scalar(out=gB.t[:], in0=gB.t[:], scalar1=0.5, scalar2=None, op0=ALU.mult), [gB], [gB])
            eps = c.sb([128, 1], F32, "geps")
            c.op("dve", lambda e: e.memset(eps.t[:], 1e-6), [], [eps])
            alT = c.sb([16, S], BF16, "alT")
            wq = wslot([128, 8, 128], "w0"); wk = wslot([128, 8, 128], "w1")
            wv = wslot([128, 8, 256], "w2"); wr = wslot([128, 8, 256], "w3")
            sp_ = c.sb([128, S], F32, "sp"); bp = c.sb([128, S], F32, "bp")
            qtl = c.sb([128, S], BF16, "qtl"); ktl = c.sb([128, S], BF16, "ktl")
            tE = Rot([c.sb([128, 512], F32, "tE") for _ in range(2)])
            ebl = c.sb([128, 16], F32, "ebl")
            Vc = Rot([c.sb([128, 256], BF16, "Vc") for _ in range(4)])
            AT = Rot([c.sb([128, 128], BF16, "AT") for _ in range(4)])
            khT = Rot([c.sb([128, 128], BF16, "khT") for _ in range(3)])
            kh = Rot([c.sb([128, 128], BF16, "kh") for _ in range(4)])
            NEG1 = c.sb([128, 256], F32, "NEG1")
            c.op("pool", lambda e: e.memset(NEG1.t[:], -1.0), [], [NEG1])
            junk = c.sb([128, 256], F32, "junk")
            sm = Rot([c.sb([128, 4], F32, "gsm") for _ in range(2)])
            er = Rot([c.sb([128, 256], F32, "er") for _ in range(4)])
            yh = Rot([c.sb([128, 256], BF16, "yh") for _ in range(3)])
            Sf = c.sb([128, 256], F32, "Sf"); Sb = c.sb([128, 256], BF16, "Sb")
            PP = Rot([c.ps([128, 512], F32, "pp") for _ in range(2)])
            PS_ = Rot([c.ps([128, 512], F32, "pss") for _ in range(1)])
            PO = Rot([c.ps([128, 512], F32, "pgo") for _ in range(2)])
            PC = c.ps([128, 512], F32, "pc")
            PTr = c.ps([128, 8, 128], BF16, "ptr")
            PT2 = c.ps([128, 8, 128], BF16, "pt2")
            for tq in range(4):
                pb = PP.next()
                proj_fm(pb, wa, lambda k: wa.t[:, k, :], tq)
                c.op("dve", lambda e: e.tensor_copy(out=alT.t[:, tq * 512:(tq + 1) * 512], in_=pb.t[0:16, :]), [pb], [alT])
            for h in range(4):
                load_w(wq, wq.t[:], win[:, h * 128:(h + 1) * 128])
                load_w(wk, wk.t[:], win[:, 512 + h * 128:512 + (h + 1) * 128])
                load_w(wv, wv.t[:], win[:, 1024 + h * 256:1024 + (h + 1) * 256])
                load_w(wr, wr.t[:], win[:, 2048 + h * 256:2048 + (h + 1) * 256])
                for tq in range(4):
                    ts_ = slice(tq * 512, (tq + 1) * 512)
                    pb = PP.next()
                    c.op("pe", lambda e: e.matmul(pb.t[:], lhsT=wa2.t[:, h * 128:(h + 1) * 128], rhs=alT.t[:, ts_], start=True, stop=True),
                         [wa2, alT], [pb])
                    te = tE.next()
                    c.op("act", lambda e: e.activation(out=te.t[:], in_=pb.t[:], func=AF.Exp, bias=nba.t[:, h:h + 1], scale=-1.0), [pb, nba], [te])
                    c.op("act", lambda e: e.activation(out=sp_.t[:, ts_], in_=te.t[:], func=AF.Ln, bias=1.0), [te], [sp_])
                c.op("dve", lambda e: e.tensor_tensor_scan(out=bp.t[:], data0=rm.t[:], data1=sp_.t[:], initial=0.0, op0=ALU.mult, op1=ALU.add),
                     [rm, sp_], [bp])
                c.op("act", lambda e: e.activation(out=ebl.t[:], in_=bp.t[:].rearrange("p (c t) -> p c t", t=128)[:, :, 127], func=AF.Exp,
                                                   scale=-1.0 / 16), [bp], [ebl])
                for tq in range(4):
                    ts_ = slice(tq * 512, (tq + 1) * 512)
                    pb = PP.next()
                    proj_fm(pb, wq, lambda k: wq.t[:, k, :], tq)
                    te = tE.next()
                    c.op("act", lambda e: e.activation(out=te.t[:], in_=bp.t[:, ts_], func=AF.Exp, scale=-1.0 / 16), [bp], [te])
                    c.op("dve", lambda e: e.scalar_tensor_tensor(out=qtl.t[:, ts_], in0=pb.t[:], scalar=128.0 ** -0.5, in1=te.t[:],
                                                                 op0=ALU.mult, op1=ALU.mult), [pb, te], [qtl])
                    pb = PP.next()
                    proj_fm(pb, wk, lambda k: wk.t[:, k, :], tq)
                    te = tE.next()
                    c.op("act", lambda e: e.activation(out=te.t[:], in_=bp.t[:, ts_], func=AF.Exp, scale=1.0 / 16), [bp], [te])
                    c.op("dve", lambda e: e.tensor_tensor(out=ktl.t[:, ts_], in0=pb.t[:], in1=te.t[:], op=ALU.mult), [pb, te], [ktl])
                def gS1(cc):
                    q_, j_ = divmod(cc, 4)
                    sl = slice(cc * 128, (cc + 1) * 128)
                    hsl = lambda k: HT[q_].t[:, k, j_ * 128:(j_ + 1) * 128]
                    kt_ = khT.next(); k_ = kh.next()
                    c.op("dve", lambda e: e.tensor_scalar(out=kt_.t[:], in0=ktl.t[:, sl], scalar1=ebl.t[:, cc:cc + 1], scalar2=None, op0=ALU.mult),
                         [ktl, ebl], [kt_])
                    ps = PS_.next()
                    c.op("pe", lambda e: e.matmul(ps.t[:, 0:128], lhsT=ktl.t[:, sl], rhs=qtl.t[:, sl], start=True, stop=True), [ktl, qtl], [ps])
                    at = AT.next()
                    c.op("dve", lambda e: e.tensor_tensor(out=at.t[:], in0=ps.t[:, 0:128], in1=maskLE, op=ALU.mult), [ps, CON], [at])
                    pb = PP.next()
                    for k in range(8):
                        c.op("pe", lambda e: e.matmul(pb.t[:, 0:256], lhsT=hsl(k), rhs=wv.t[:, k, :], start=(k == 0), stop=(k == 7)),
                             [wv, HT[q_]], [pb])
                    vc = Vc.next()
                    c.op("act", lambda e: e.activation(out=vc.t[:], in_=pb.t[:, 0:256], func=AF.Copy), [pb], [vc])
                    c.op("pe", lambda e: e.transpose(out=PT2.t[:, 0, :], in_=kt_.t[:], identity=ident), [kt_, CON], [PT2])
                    c.op("act", lambda e: e.activation(out=k_.t[:], in_=PT2.t[:, 0, :], func=AF.Copy), [PT2], [k_])
                    pr = PP.next()
                    for k in range(8):
                        c.op("pe", lambda e: e.matmul(pr.t[:, 0:256], lhsT=hsl(k), rhs=wr.t[:, k, :], start=(k == 0), stop=(k == 7)),
                             [wr, HT[q_]], [pr])
                    e_ = er.next()
                    c.op("act", lambda e: e.activation(out=e_.t[:], in_=pr.t[:, 0:256], func=AF.Tanh, scale=0.5), [pr], [e_])
                    c.op("dve", lambda e: e.scalar_tensor_tensor(out=e_.t[:], in0=e_.t[:], scalar=1.0, in1=pr.t[:, 0:256], op0=ALU.add, op1=ALU.mult),
                         [e_, pr], [e_])
                    c.op("pool", lambda e: e.tensor_tensor(out=e_.t[:], in0=e_.t[:], in1=gB.t[:], op=ALU.mult), [e_, gB], [e_])
                    return dict(vc=vc, at=at, e=e_, k=k_)

                def gFlush(prev_y):
                    pcc, py_ = prev_y
                    for j2 in range(2):
                        c.op("pe", lambda e: e.transpose(out=PTr.t[:, j2, :], in_=py_.t[:, j2 * 128:(j2 + 1) * 128], identity=ident), [py_, CON], [PTr])
                    for j2 in range(2):
                        c.op("act", lambda e: e.activation(out=YT[2 * h + j2].t[:, pcc * 128:(pcc + 1) * 128], in_=PTr.t[:, j2, :], func=AF.Copy),
                             [PTr], [YT[2 * h + j2]])

                def gS2(cc, d, prev_y):
                    sl = slice(cc * 128, (cc + 1) * 128)
                    vc, at, e_, k_ = d["vc"], d["at"], d["e"], d["k"]
                    po = PO.next()
                    if cc > 0:
                        c.op("pe", lambda e: e.matmul(po.t[:, 0:256], lhsT=qtl.t[:, sl], rhs=Sb.t[:], start=True, stop=False), [qtl, Sb], [po])
                    c.op("pe", lambda e: e.matmul(PC.t[:, 0:256], lhsT=k_.t[:], rhs=vc.t[:], start=True, stop=True), [k_, vc], [PC])
                    c.op("pe", lambda e: e.matmul(po.t[:, 0:256], lhsT=at.t[:], rhs=vc.t[:], start=(cc == 0), stop=True), [at, vc], [po])
                    if prev_y is not None:
                        gFlush(prev_y)
                    if cc > 0:
                        c.op("dve", lambda e: e.scalar_tensor_tensor(out=Sf.t[:], in0=Sf.t[:], scalar=ebl.t[:, cc:cc + 1], in1=PC.t[:, 0:256],
                                                                     op0=ALU.mult, op1=ALU.add), [Sf, ebl, PC], [Sf])
                    else:
                        c.op("dve", lambda e: e.tensor_copy(out=Sf.t[:], in_=PC.t[:, 0:256]), [PC], [Sf])
                    m_ = sm.next()
                    c.op("act", lambda e: e.activation(out=junk.t[:], in_=po.t[:, 0:256], func=AF.Square, accum_out=m_.t[:, 0:1]), [po], [junk, m_])
                    c.op("act", lambda e: e.activation(out=Sb.t[:], in_=Sf.t[:], func=AF.Copy), [Sf], [Sb])
                    c.op("act", lambda e: e.activation(out=m_.t[:, 1:2], in_=m_.t[:, 0:1], func=AF.Ln, bias=eps.t[:, 0:1], scale=1.0 / 256),
                         [m_, eps], [m_])
                    c.op("act", lambda e: e.activation(out=m_.t[:, 2:3], in_=m_.t[:, 1:2], func=AF.Exp, scale=-0.5), [m_], [m_])
                    y_ = yh.next()
                    c.op("dve", lambda e: e.scalar_tensor_tensor(out=y_.t[:], in0=po.t[:, 0:256], scalar=m_.t[:, 2:3], in1=e_.t[:],
                                                                 op0=ALU.mult, op1=ALU.mult), [po, m_, e_], [y_])
                    return (cc, y_)

                dd = {}; prev_y = None
                for n in range(16 + 2):
                    if n < 16:
                        dd[n] = gS1(n)
                    if 0 <= n - 2 < 16:
                        prev_y = gS2(n - 2, dd.pop(n - 2), prev_y)
                gFlush(prev_y)
            c.pop()
            return YT

        def xattn(layer):
            c.push()
            OT = [c.sb([128, S], BF16, f"OT{k}") for k in range(8)]
            c.push()
            wq = [wslot([128, 8, 256], f"w{j}") for j in range(2)]
            wk = [wslot([128, 8, 256], f"w{2 + j}") for j in range(2)]
            wv = [wslot([128, 8, 256], f"w{4 + j}") for j in range(2)]
            qTs = [c.sb([128, 2, S], BF16, "xqT") for _ in range(2)]
            kTs = [c.sb([128, 2, 256], BF16, "xkT") for _ in range(2)]
            Vas = [c.sb([128, 2, 257], BF16, "xV") for _ in range(2)]
            PT = [Rot([c.sb([128, 512], BF16, "xPT") for _ in range(2)]) for _ in range(2)]
            Ot = Rot([c.sb([128, 256], BF16, "xO") for _ in range(3)])
            rd = Rot([c.sb([128, 1], F32, "xrd") for _ in range(3)])
            PP = Rot([c.ps([128, 512], F32, "pp") for _ in range(2)])
            PS_ = Rot([c.ps([128, 512], F32, "psc") for _ in range(2)])
            PO = Rot([c.ps([128, 512], F32, "pxo") for _ in range(2)])
            PTr = Rot([c.ps([128, 8, 128], BF16, "ptr") for _ in range(2)])
            for Va_ in Vas:
                c.op("dve", lambda e: e.memset(Va_.t[:, :, 256:257], 1.0), [], [Va_])
            wkv = Dm["xa_wkv"][layer]

            def load_head(h):
                b = h % 2
                load_w(wq[b], wq[b].t[:], Dm["xa_wq"][layer][:, h * 256:(h + 1) * 256])
                load_w(wk[b], wk[b].t[:], wkv[:, h * 256:(h + 1) * 256])
                load_w(wv[b], wv[b].t[:], wkv[:, 1024 + h * 256:1024 + (h + 1) * 256])

            def xproj_items(h):
                bb = h % 2
                items = []
                for dc in range(2):
                    def fk(dc=dc):
                        pb = PP.next()
                        for k in range(8):
                            c.op("pe", lambda e: e.matmul(pb.t[:, 0:256], lhsT=wk[bb].t[:, k, dc * 128:(dc + 1) * 128], rhs=memT.t[:, k, :],
                                                          start=(k == 0), stop=(k == 7)), [wk[bb], memT], [pb])
                        c.op("dve", lambda e: e.tensor_copy(out=kTs[bb].t[:, dc, :], in_=pb.t[:, 0:256]), [pb], [kTs[bb]])
                    items.append(fk)
                for mt in range(2):
                    def fv(mt=mt):
                        pb = PP.next()
                        for k in range(8):
                            c.op("pe", lambda e: e.matmul(pb.t[:, 0:256], lhsT=memT.t[:, k, mt * 128:(mt + 1) * 128], rhs=wv[bb].t[:, k, :],
                                                          start=(k == 0), stop=(k == 7)), [wv[bb], memT], [pb])
                        c.op("dve", lambda e: e.tensor_copy(out=Vas[bb].t[:, mt, 0:256], in_=pb.t[:, 0:256]), [pb], [Vas[bb]])
                    items.append(fv)
                for dc in range(2):
                    for tq in range(4):
                        def fq(dc=dc, tq=tq):
                            pb = PP.next()
                            proj_fm(pb, wq[bb], lambda k: wq[bb].t[:, k, dc * 128:(dc + 1) * 128], tq)
                            c.op("act", lambda e: e.activation(out=qTs[bb].t[:, dc, tq * 512:(tq + 1) * 512], in_=pb.t[:], func=AF.Copy, scale=1.0 / 16),
                                 [pb], [qTs[bb]])
                        items.append(fq)
                return items

            load_head(0)
            load_head(1)
            for it in xproj_items(0):
                it()
            for h in range(4):
                b = h % 2
                if 1 <= h and h + 1 < 4:
                    load_head(h + 1)
                nxt = xproj_items(h + 1) if h + 1 < 4 else []
                qT, kT, Va = qTs[b], kTs[b], Vas[b]
                def xP(tq):
                    pts = []
                    for mt in range(2):
                        psc = PS_.next()
                        for dc in range(2):
                            c.op("pe", lambda e: e.matmul(psc.t[:], lhsT=kT.t[:, dc, mt * 128:(mt + 1) * 128],
                                                          rhs=qT.t[:, dc, tq * 512:(tq + 1) * 512], start=(dc == 0), stop=(dc == 1)),
                                 [kT, qT], [psc])
                        pt = PT[mt].next()
                        c.op("act", lambda e: e.activation(out=pt.t[:], in_=psc.t[:], func=AF.Exp), [psc], [pt])
                        pts.append(pt)
                    return pts

                def xV(i, pts):
                    jt = i % 4
                    po = PO.next()
                    for mt in range(2):
                        c.op("pe", lambda e: e.matmul(po.t[:, 0:257], lhsT=pts[mt].t[:, jt * 128:(jt + 1) * 128], rhs=Va.t[:, mt, :],
                                                      start=(mt == 0), stop=(mt == 1)), [pts[mt], Va], [po])
                    r_ = rd.next(); o_ = Ot.next()
                    c.op("dve", lambda e: e.reciprocal(out=r_.t[:], in_=po.t[:, 256:257]), [po], [r_])
                    c.op("act", lambda e: e.activation(out=o_.t[:], in_=po.t[:, 0:256], func=AF.Copy, scale=r_.t[:, 0:1]), [po, r_], [o_])
                    return o_

                def xT(i, o_):
                    ptr = PTr.next()
                    for j2 in range(2):
                        c.op("pe", lambda e: e.transpose(out=ptr.t[:, j2, :], in_=o_.t[:, j2 * 128:(j2 + 1) * 128], identity=ident),
                             [o_, CON], [ptr])
                    for j2 in range(2):
                        c.op("dve", lambda e: e.tensor_copy(out=OT[2 * h + j2].t[:, i * 128:(i + 1) * 128], in_=ptr.t[:, j2, :]),
                             [ptr], [OT[2 * h + j2]])

                ptsl = {0: xP(0)}
                prev = None
                for tq in range(4):
                    if tq + 1 < 4:
                        ptsl[tq + 1] = xP(tq + 1)
                    for jt in range(4):
                        i = tq * 4 + jt
                        o_ = xV(i, ptsl[tq])
                        if prev is not None:
                            xT(*prev)
                        prev = (i, o_)
                        if i >= 2 and nxt:
                            nxt.pop(0)()
                xT(*prev)
                for it in nxt:
                    it()
            c.pop()
            out_proj_ln(OT, Dm["xa_wo"][layer], layer * 3 + 1)
            c.pop()

        def ffn(layer):
            c.push()
            groups = [[0, 1]] + [list(range(g, g + 4)) for g in range(2, NCH, 4)]
            wup = Dm["ffn_up"][layer]
            wdn = Dm["ffn_down"][layer]
            cw = c.sb([128, 44 * 3], F32, "cw"); cb = c.sb([128, 44], F32, "cb")
            c.dma("sp", cw.t[:], Dm["ffn_conv"][layer], writes=[cw], key="cw")
            c.dma("sp", cb.t[:], Dm["ffn_conv_b"][layer], writes=[cb], key="cb")
            act = [c.sb([128, S], BF16, f"act{j}") for j in range(4)]
            wd = [wslot([128, 4, D], f"w{j}") for j in range(2)]
            PD = Rot([c.ps([128, 512], F32, "pd") for _ in range(4)])
            c.push()
            wg = [wslot([128, 8, 128], f"w{2 + j}") for j in range(2)]
            wv = [wslot([128, 8, 128], f"w{4 + j}") for j in range(2)]
            ug = c.sb([128, S + 2], F32, "ug"); uv = c.sb([128, S + 2], F32, "uv")
            cg = c.sb([128, S], F32, "cg"); cv = c.sb([128, S], F32, "cv")
            gg = c.sb([128, S], F32, "gg")
            PU = Rot([c.ps([128, 512], F32, "pu") for _ in range(4)])
            c.op("dve", lambda e: e.memset(ug.t[:, 0:2], 0.0), [], [ug])
            c.op("dve", lambda e: e.memset(uv.t[:, 0:2], 0.0), [], [uv])

            def load_up(j):
                b = j % 2
                load_w(wg[b], wg[b].t[:], wup[:, j * 128:(j + 1) * 128])
                load_w(wv[b], wv[b].t[:], wup[:, DFF + j * 128:DFF + (j + 1) * 128])

            def conv_part(u, w, ch, dst):
                c.op("dve", lambda e: e.scalar_tensor_tensor(out=dst.t[:], in0=u.t[:, 1:S + 1], scalar=cw.t[:, ch * 3 + 1:ch * 3 + 2],
                                                             in1=dst.t[:], op0=ALU.mult, op1=ALU.add), [u, cw, dst], [dst])
                c.op("dve", lambda e: e.scalar_tensor_tensor(out=dst.t[:], in0=u.t[:, 0:S], scalar=cw.t[:, ch * 3:ch * 3 + 1],
                                                             in1=dst.t[:], op0=ALU.mult, op1=ALU.add), [u, cw, dst], [dst])

            def down_tile(i, gi, grp, wdb):
                for half in range(2):
                    pd = PD.next()
                    for jj in range(len(grp)):
                        c.op("pe", lambda e: e.matmul(pd.t[:], lhsT=act[jj].t[:, i * 128:(i + 1) * 128],
                                                      rhs=wdb.t[:, jj, half * 512:(half + 1) * 512],
                                                      start=(jj == 0), stop=(jj == len(grp) - 1)), [act[jj], wdb], [pd])
                    hs = H[i].t[:, half * 512:(half + 1) * 512]
                    if gi == 0:
                        c.op("dve", lambda e: e.scalar_tensor_tensor(out=hs, in0=hs, scalar=ALPHA, in1=pd.t[:], op0=ALU.mult, op1=ALU.add),
                             [H[i], pd], [H[i]])
                    else:
                        c.op("dve", lambda e: e.tensor_tensor(out=hs, in0=hs, in1=pd.t[:], op=ALU.add), [H[i], pd], [H[i]])

            load_up(0)
            last = len(groups) - 1
            for gi, grp in enumerate(groups):
                wdb = wd[gi % 2]
                load_w(wdb, wdb.t[:, 0:len(grp), :], wdn[grp[0] * 128:(grp[-1] + 1) * 128, :])
                for jj, j in enumerate(grp):
                    b = j % 2
                    if j + 1 < NCH:
                        load_up(j + 1)
                    for (w_, u_, cdst, ch) in ((wg[b], ug, cg, j), (wv[b], uv, cv, NCH + j)):
                        for tq in range(4):
                            pb = PU.next()
                            proj_fm(pb, w_, lambda k: w_.t[:, k, :], tq)
                            c.op("act", lambda e: e.activation(out=u_.t[:, 2 + tq * 512:2 + (tq + 1) * 512], in_=pb.t[:], func=AF.Copy),
                                 [pb], [u_])
                            c.op("act", lambda e: e.activation(out=cdst.t[:, tq * 512:(tq + 1) * 512], in_=pb.t[:], func=AF.Identity,
                                                               bias=cb.t[:, ch:ch + 1], scale=cw.t[:, ch * 3 + 2:ch * 3 + 3]),
                                 [pb, cb, cw], [cdst])
                        conv_part(u_, w_, ch, cdst)
                    c.op("act", lambda e: e.activation(out=gg.t[:], in_=cg.t[:], func=AF.Gelu_apprx_tanh), [cg], [gg])
                    c.op("pool", lambda e: e.tensor_tensor(out=act[jj].t[:], in0=gg.t[:], in1=cv.t[:], op=ALU.mult), [gg, cv], [act[jj]])
                if gi < last:
                    for i in range(NT):
                        down_tile(i, gi, grp, wdb)
            c.pop()
            R, gB, bB = ln_res(layer * 3 + 2)
            grp = groups[last]; wdb = wd[last % 2]
            hbs = {}
            for n in range(NT + 2):
                if n < NT: down_tile(n, last, grp, wdb)
                if 0 <= n - 1 < NT: hbs[n - 1] = ln_tile(n - 1, H[n - 1], gB, bB, R)
                if 0 <= n - 2 < NT: ht_from_hb(n - 2, hbs[n - 2], R["pT"])
            c.pop()

        c.push()
        hbR = Rot([c.sb([128, D], BF16, "hb") for _ in range(2)])
        pTR = Rot([c.ps([128, 8, 128], BF16, "pT") for _ in range(2)])
        for i in range(NT):
            to_ht(i, hbR, pTR)
        mf = c.sb([128, D], F32, "mf")
        for mt in range(2):
            c.dma("sp", mf.t[:], Dm["mem"][mt * 128:(mt + 1) * 128, :], writes=[mf], key="mem")
            hb = hbR.next()
            c.op("act", lambda e: e.activation(out=hb.t[:], in_=mf.t[:], func=AF.Copy), [mf], [hb])
            pT = pTR.next()
            for k in range(8):
                c.op("pe", lambda e: e.transpose(out=pT.t[:, k, :], in_=hb.t[:, k * 128:(k + 1) * 128], identity=ident), [hb, CON], [pT])
            c.op("dve", lambda e: e.tensor_copy(out=memT.t[:, :, mt * 128:(mt + 1) * 128], in_=pT.t[:]), [pT], [memT])
        c.pop()

        def finish():
            for i in range(NT):
                c.dma("sp", out[i * 128:(i + 1) * 128, :], H[i].t[:], reads=[H[i]], key="out")
            c.barrier()

        done = False
        for layer in range(first, nlayers):
            kind = layer % 4
            if kind == 0:
                YT = mixer_sb(Dm["sb_win"])
            elif kind == 1:
                YT = mixer_fox(Dm["fox_win"], Dm["fox_bf"])
            elif kind == 2:
                YT = mixer_mlstm(Dm["ml_win"])
            else:
                YT = mixer_gla(Dm["gla_win"])
            out_proj_ln(YT, Dm["mix_wo"][layer], layer * 3 + 0)
            c.pop()
            if stop == (layer, "a"):
                break
            xattn(layer)
            if stop == (layer, "b"):
                break
            ffn(layer)
        finish()
    return nc


_NC_CACHE = {}


def prep_inputs(inputs):
    con, con4, rmask = make_consts()
    shared = {
        "ln_g": np.ascontiguousarray(inputs["ln_g"].reshape(12, D)),
        "ln_b": np.ascontiguousarray(inputs["ln_b"].reshape(12, D)),
        "mix_wo": inputs["mix_wo"], "sb_win": inputs["sb_win"][0], "fox_win": inputs["fox_win"][0],
        "fox_bf": inputs["fox_bf"].reshape(1, 16),
        "ml_win": inputs["ml_win"][0], "ml_bi": inputs["ml_bi"].reshape(4, 1), "ml_bf": inputs["ml_bf"].reshape(4, 1),
        "gla_win": inputs["gla_win"][0], "gla_wa2": inputs["gla_wa2"][0],
        "gla_ba": np.ascontiguousarray(inputs["gla_ba"].reshape(4, 128).T),
        "gla_norm_g": inputs["gla_norm_g"].reshape(1, 256),
        "xa_wq": inputs["xa_wq"], "xa_wkv": inputs["xa_wkv"], "xa_wo": inputs["xa_wo"],
        "ffn_up": inputs["ffn_up"],
        "ffn_conv": np.ascontiguousarray(inputs["ffn_conv"].reshape(4, 3, 44, 128).transpose(0, 3, 2, 1).reshape(4, 128, 132)),
        "ffn_conv_b": np.ascontiguousarray(inputs["ffn_conv_b"].reshape(4, 44, 128).transpose(0, 2, 1)),
        "ffn_down": inputs["ffn_down"],
        "con": con, "con4": con4, "rmask": rmask,
    }
    shared = {k: np.ascontiguousarray(v, dtype=np.float32) for k, v in shared.items()}
    return shared


def kernel(**inputs):
    inputs = {k: np.asarray(v) for k, v in inputs.items()}
    shared = prep_inputs(inputs)
    if "nc" not in _NC_CACHE:
        _NC_CACHE["nc"] = build()
    nc = _NC_CACHE["nc"]
    in_maps = []
    for b in range(8):
        m = dict(shared)
        m["x"] = np.ascontiguousarray(inputs["x"][b], dtype=np.float32)
        m["mem"] = np.ascontiguousarray(inputs["mem"][b], dtype=np.float32)
        in_maps.append(m)
    res = run_bass_kernel_spmd(nc, in_maps, core_ids=list(range(8)))
    return np.stack([np.asarray(r["out"], dtype=np.float32) for r in res.results], axis=0)
```
